# Optimizing a Trainium2 kernel written in Bass

```python
import math
import jax, jax.numpy as jnp
from jax import lax
import numpy as np

D_MODEL = 1024
BATCH = 8
SEQ = 2048
DEPTH = 4

CHUNK = 64
N_MIXERS = 3
N_RWKV = (DEPTH + 2) // 3
N_MLSTM = (DEPTH + 1) // 3
N_RET = DEPTH // 3
DN_ALPHA = (2 * DEPTH) ** 0.25
DN_BETA = (8 * DEPTH) ** -0.25
LN_EPS = 1e-5

RW_HEAD = 64
RW_HEADS = D_MODEL // RW_HEAD
RW_LORA_W = 64
RW_LORA_A = 64
RW_LORA_V = 32
RW_LORA_G = 128
RW_GN_EPS = 64e-5

ML_HEADS = 8
ML_QK = D_MODEL // 2
ML_V = D_MODEL
ML_DK = ML_QK // ML_HEADS
ML_DV = ML_V // ML_HEADS
ML_CONV = 4
ML_IN = 2 * ML_QK + 2 * ML_V + 2 * ML_HEADS

RT_HEADS = 4
RT_QK = D_MODEL
RT_V = 2 * D_MODEL
RT_DK = RT_QK // RT_HEADS
RT_DV = RT_V // RT_HEADS
RT_IN = 2 * RT_QK + 2 * RT_V
RT_ROPE_BASE = 10000.0

MOE_GROUPS = 4
MOE_PER_GROUP = 8
MOE_EXPERTS = MOE_GROUPS * MOE_PER_GROUP
MOE_TOPK = 2
MOE_HIDDEN = 512
MOE_BLOCK = 128

kernel_name = 'hybrid_rwkv7_mlstm_retention_hmoe_deepnorm_adaln'


def layer_norm(x, g, b, eps=LN_EPS):
    xf = x.astype(jnp.float32)
    mu = xf.mean(-1, keepdims=True)
    var = jnp.square(xf - mu).mean(-1, keepdims=True)
    return ((xf - mu) * lax.rsqrt(var + eps) * g + b).astype(x.dtype)


def head_norm(h, g, b, eps):
    B, T, H, d = h.shape
    return layer_norm(h, g.reshape(H, d), b.reshape(H, d), eps).reshape(B, T, H * d)


def token_shift(x):
    return jnp.pad(x, ((0, 0), (1, 0), (0, 0)))[:, :-1]


def causal_conv(x, w, b):
    K, T = w.shape[0], x.shape[1]
    xp = jnp.pad(x, ((0, 0), (K - 1, 0), (0, 0)))
    y = b
    for j in range(K):
        y = y + w[j] * xp[:, j:j + T]
    return y


def to_chunks(t):
    B, T, H = t.shape[:3]
    t = t.reshape(B, T // CHUNK, CHUNK, H, *t.shape[3:])
    return jnp.moveaxis(t, (1, 3), (0, 2))


def from_chunks(t):
    t = jnp.moveaxis(t, (0, 2), (1, 3))
    B, NC, L, H = t.shape[:4]
    return t.reshape(B, NC * L, H, *t.shape[4:])


def ada_modulation(c, w, b):
    mod = jax.nn.silu(c) @ w + b
    shift, scale, gate = jnp.split(mod[:, None, :], 3, axis=-1)
    return shift, scale, gate


def rwkv7_scan(r, w, k, v, a, b):
    B, T, H, N = r.shape
    seq = tuple(jnp.moveaxis(t.astype(jnp.float32), 1, 0) for t in (r, w, k, v, a, b))

    def step(S, inp):
        rt, wt, kt, vt, at, bt = inp
        sa = jnp.einsum('bhij,bhj->bhi', S, at)
        S = S * wt[:, :, None, :] + sa[..., None] * bt[:, :, None, :] + vt[..., None] * kt[:, :, None, :]
        return S, jnp.einsum('bhij,bhj->bhi', S, rt)

    _, y = lax.scan(step, jnp.zeros((B, H, N, N), jnp.float32), seq)
    return jnp.moveaxis(y, 0, 1).astype(r.dtype)


def rwkv7_mixer(h, v_first, mu, w_rkv, w0, w1, w2, a0, a1, a2, g1, g2, k_k, k_a, r_k, gn_g, gn_b, w_out, vmix):
    B, T, D = h.shape
    xx = token_shift(h) - h
    xs = h[None] + xx[None] * mu[:, None, None, :]
    r, k, v = jnp.einsum('nbtd,nde->nbte', xs[:3], w_rkv)
    xv, xw, xa, xg = xs[2], xs[3], xs[4], xs[5]
    w_log = -jax.nn.softplus(-(w0 + jnp.tanh(xw @ w1) @ w2)) - 0.5
    decay = jnp.exp(-jnp.exp(w_log.astype(jnp.float32)))
    if vmix is None:
        v_first = v
    else:
        v0, v1, v2 = vmix
        v = v + (v_first - v) * jax.nn.sigmoid(v0 + (xv @ v1) @ v2)
    a = jax.nn.sigmoid(a0 + (xa @ a1) @ a2)
    g = jax.nn.sigmoid(xg @ g1) @ g2
    heads = lambda t: t.reshape(B, T, RW_HEADS, RW_HEAD)
    kk = heads(k * k_k)
    kk = kk / jnp.maximum(jnp.sqrt(jnp.sum(kk * kk, -1, keepdims=True)), 1e-12)
    k = k * (1 + (a - 1) * k_a)
    rh, kh, vh, ah = heads(r), heads(k), heads(v), heads(a)
    y = rwkv7_scan(rh, heads(decay), kh, vh, -kk, kk * ah)
    y = head_norm(y, gn_g, gn_b, RW_GN_EPS)
    bonus = jnp.sum(rh * kh * r_k, -1, keepdims=True) * vh
    y = y + bonus.reshape(B, T, D)
    return (y * g) @ w_out, v_first


def mlstm_chunkwise(q, k, v, log_i, log_f):
    B, T, H, DK = q.shape
    DV = v.shape[-1]
    qc, kc, vc = (to_chunks(t.astype(jnp.float32)) for t in (q, k, v))
    ic, fc = to_chunks(log_i), to_chunks(log_f)
    causal = jnp.tril(jnp.ones((CHUNK, CHUNK), bool))

    def step(carry, inp):
        C, n, m = carry
        qb, kb, vb, ib, fb = inp
        bcum = jnp.cumsum(fb, -1)
        dmat = jnp.where(causal, bcum[..., :, None] - bcum[..., None, :] + ib[..., None, :], -jnp.inf)
        inter = bcum + m[..., None]
        m_s = jnp.maximum(dmat.max(-1), inter)
        s = jnp.einsum('bhsd,bhjd->bhsj', qb, kb) * jnp.exp(dmat - m_s[..., None])
        w_inter = jnp.exp(inter - m_s)
        num = jnp.einsum('bhsj,bhjv->bhsv', s, vb) + w_inter[..., None] * jnp.einsum('bhvd,bhsd->bhsv', C, qb)
        den = s.sum(-1) + w_inter * jnp.einsum('bhd,bhsd->bhs', n, qb)
        hb = num / jnp.maximum(jnp.abs(den), jnp.exp(-m_s))[..., None]
        b_end = bcum[..., -1]
        to_end = b_end[..., None] - bcum + ib
        m_new = jnp.maximum(b_end + m, to_end.max(-1))
        w_end = jnp.exp(to_end - m_new[..., None])
        carry_decay = jnp.exp(b_end + m - m_new)
        C = carry_decay[..., None, None] * C + jnp.einsum('bhj,bhjv,bhjd->bhvd', w_end, vb, kb)
        n = carry_decay[..., None] * n + jnp.einsum('bhj,bhjd->bhd', w_end, kb)
        return (C, n, m_new), hb

    init = (jnp.zeros((B, H, DV, DK), jnp.float32), jnp.zeros((B, H, DK), jnp.float32), jnp.zeros((B, H), jnp.float32))
    _, y = lax.scan(step, init, (qc, kc, vc, ic, fc))
    return from_chunks(y).astype(v.dtype)


def mlstm_mixer(h, w_in, b_gate, conv_w, conv_b, gn_g, gn_b, w_out):
    B, T, D = h.shape
    H = ML_HEADS
    proj = h @ w_in
    qk, v, o, gates = jnp.split(proj, [2 * ML_QK, 2 * ML_QK + ML_V, 2 * ML_QK + 2 * ML_V], axis=-1)
    qk = jax.nn.silu(causal_conv(qk, conv_w, conv_b))
    q, k = jnp.split(qk, 2, axis=-1)
    gates = (gates + b_gate).astype(jnp.float32)
    log_i = gates[..., :H]
    log_f = jax.nn.log_sigmoid(gates[..., H:])
    y = mlstm_chunkwise(q.reshape(B, T, H, ML_DK) * ML_DK ** -0.5, k.reshape(B, T, H, ML_DK),
                        v.reshape(B, T, H, ML_DV), log_i, log_f)
    y = head_norm(y, gn_g, gn_b, 1e-6) * jax.nn.sigmoid(o)
    return y @ w_out


def rotary(x, base):
    T, dk = x.shape[1], x.shape[-1]
    half = dk // 2
    inv = base ** -(jnp.arange(half, dtype=jnp.float32) / (half - 1))
    ang = jnp.arange(T, dtype=jnp.float32)[:, None] * inv[None, :]
    cos, sin = jnp.cos(ang)[None, :, None, :], jnp.sin(ang)[None, :, None, :]
    x1, x2 = x[..., :half], x[..., half:]
    return jnp.concatenate([x1 * cos - x2 * sin, x1 * sin + x2 * cos], -1).astype(x.dtype)


def retention_chunkwise(q, k, v):
    B, T, H, DK = q.shape
    DV = v.shape[-1]
    log_g = jnp.log1p(-jnp.exp2(-5.0 - jnp.arange(H, dtype=jnp.float32)))
    pos = jnp.arange(CHUNK, dtype=jnp.float32)
    rel = pos[:, None] - pos[None, :]
    intra = jnp.where(rel >= 0, jnp.exp(log_g[:, None, None] * jnp.maximum(rel, 0.0)), 0.0)
    q_dec = jnp.exp(log_g[:, None] * (pos + 1.0))
    k_dec = jnp.exp(log_g[:, None] * (CHUNK - 1.0 - pos))
    chunk_dec = jnp.exp(log_g * CHUNK)
    qc, kc, vc = (to_chunks(t.astype(jnp.float32)) for t in (q, k, v))
    inner = jnp.einsum('cbhsj,cbhjv->cbhsv', jnp.einsum('cbhsd,cbhjd->cbhsj', qc, kc) * intra, vc)

    def step(R, inp):
        qb, kb, vb = inp
        cross = jnp.einsum('bhsd,bhdv->bhsv', qb, R) * q_dec[:, :, None]
        R = chunk_dec[:, None, None] * R + jnp.einsum('bhjd,hj,bhjv->bhdv', kb, k_dec, vb)
        return R, cross

    _, cross = lax.scan(step, jnp.zeros((B, H, DK, DV), jnp.float32), (qc, kc, vc))
    return from_chunks(inner + cross).astype(v.dtype)


def retention_mixer(h, w_in, gn_g, gn_b, w_out):
    B, T, D = h.shape
    H = RT_HEADS
    proj = h @ w_in
    q, k, v, g = jnp.split(proj, [RT_QK, 2 * RT_QK, 2 * RT_QK + RT_V], axis=-1)
    q = rotary(q.reshape(B, T, H, RT_DK), RT_ROPE_BASE)
    k = rotary(k.reshape(B, T, H, RT_DK), RT_ROPE_BASE) * RT_DK ** -0.5
    y = retention_chunkwise(q, k, v.reshape(B, T, H, RT_DV))
    y = head_norm(y, gn_g, gn_b, 1e-6)
    return (jax.nn.silu(g) * y) @ w_out


def hier_moe(h, w_grp, b_grp, w_exp, b_exp, w1, w3, w2):
    B, T, D = h.shape
    N = B * T
    hf = h.reshape(N, D)
    rows = jnp.arange(N)
    g_logit = (hf @ w_grp + b_grp).astype(jnp.float32)
    g_prob = jax.nn.softmax(g_logit, axis=-1)
    g_idx = jnp.argmax(g_logit, axis=-1).astype(jnp.int32)
    g_gate = g_prob[rows, g_idx]
    e_logit = (hf @ w_exp + b_exp).astype(jnp.float32).reshape(N, MOE_GROUPS, MOE_PER_GROUP)
    e_prob = jax.nn.softmax(e_logit[rows, g_idx], axis=-1)
    top_p, top_i = lax.top_k(e_prob, MOE_TOPK)
    weight = g_gate[:, None] * top_p / top_p.sum(-1, keepdims=True)
    expert = g_idx[:, None] * MOE_PER_GROUP + top_i.astype(jnp.int32)

    A = N * MOE_TOPK
    e_flat = expert.reshape(A)
    w_flat = weight.reshape(A)
    tok_flat = jnp.repeat(jnp.arange(N, dtype=jnp.int32), MOE_TOPK)
    order = jnp.argsort(e_flat)
    e_sorted, tok_sorted, w_sorted = e_flat[order], tok_flat[order], w_flat[order]
    counts = jnp.zeros((MOE_EXPERTS,), jnp.int32).at[e_flat].add(1)
    padded = (counts + MOE_BLOCK - 1) // MOE_BLOCK * MOE_BLOCK
    starts = jnp.cumsum(counts) - counts
    pends = jnp.cumsum(padded)
    pstarts = pends - padded
    dest = pstarts[e_sorted] + (jnp.arange(A, dtype=jnp.int32) - starts[e_sorted])
    P = A + MOE_EXPERTS * MOE_BLOCK
    n_blocks = P // MOE_BLOCK
    slot_tok = jnp.zeros((P,), jnp.int32).at[dest].set(tok_sorted)
    slot_w = jnp.zeros((P,), h.dtype).at[dest].set(w_sorted.astype(h.dtype))
    blk_start = jnp.arange(n_blocks, dtype=jnp.int32) * MOE_BLOCK
    blk_e = jnp.minimum(jnp.searchsorted(pends, blk_start, side='right'), MOE_EXPERTS - 1).astype(jnp.int32)
    xs = hf[slot_tok].reshape(n_blocks, MOE_BLOCK, D)

    def expert_block(args):
        xb, e = args
        return (jax.nn.silu(xb @ w1[e]) * (xb @ w3[e])) @ w2[e]

    ys = lax.map(expert_block, (xs, blk_e)).reshape(P, D)
    out = jnp.zeros((N, D), h.dtype).at[slot_tok].add(ys * slot_w[:, None])
    return out.reshape(B, T, D)


def setup_inputs(seed: int = 0) -> dict:
    key = jax.random.key(seed)
    ks = iter(jax.random.split(key, 64))
    f32 = jnp.float32
    D = D_MODEL

    def nrm(shape, scale):
        return jax.random.normal(next(ks), shape, f32) * scale

    NA, NB, NC = N_RWKV, N_MLSTM, N_RET
    inp = {}
    inp['x'] = nrm((BATCH, SEQ, D), 1.0)
    inp['c'] = nrm((BATCH, D), 1.0)
    inp['ada_w'] = nrm((DEPTH, 2, D, 3 * D), 0.5 * D ** -0.5)
    inp['ada_b'] = nrm((DEPTH, 2, 3 * D), 0.02)
    inp['ln_g'] = 1.0 + nrm((DEPTH, 2, D), 0.05)
    inp['ln_b'] = nrm((DEPTH, 2, D), 0.02)
    inp['rw_mu'] = jax.random.uniform(next(ks), (NA, 6, D), f32, 0.0, 1.0)
    inp['rw_w_rkv'] = nrm((NA, 3, D, D), D ** -0.5)
    inp['rw_w0'] = jnp.linspace(-5.0, -1.0, D, dtype=f32) + nrm((NA, D), 0.1)
    inp['rw_w1'] = nrm((NA, D, RW_LORA_W), D ** -0.5)
    inp['rw_w2'] = nrm((NA, RW_LORA_W, D), 0.1 * RW_LORA_W ** -0.5)
    inp['rw_a0'] = nrm((NA, D), 0.1)
    inp['rw_a1'] = nrm((NA, D, RW_LORA_A), D ** -0.5)
    inp['rw_a2'] = nrm((NA, RW_LORA_A, D), RW_LORA_A ** -0.5)
    inp['rw_v0'] = 1.0 + nrm((NA - 1, D), 0.1)
    inp['rw_v1'] = nrm((NA - 1, D, RW_LORA_V), D ** -0.5)
    inp['rw_v2'] = nrm((NA - 1, RW_LORA_V, D), RW_LORA_V ** -0.5)
    inp['rw_g1'] = nrm((NA, D, RW_LORA_G), D ** -0.5)
    inp['rw_g2'] = nrm((NA, RW_LORA_G, D), RW_LORA_G ** -0.5)
    inp['rw_kk'] = 0.85 + nrm((NA, D), 0.05)
    inp['rw_ka'] = 1.0 + nrm((NA, D), 0.05)
    inp['rw_rk'] = nrm((NA, RW_HEADS, RW_HEAD), 0.1)
    inp['rw_gn_g'] = 1.0 + nrm((NA, D), 0.05)
    inp['rw_gn_b'] = nrm((NA, D), 0.02)
    inp['rw_w_out'] = nrm((NA, D, D), DN_BETA * D ** -0.5)
    inp['ml_w_in'] = nrm((NB, D, ML_IN), D ** -0.5)
    inp['ml_b_gate'] = jnp.concatenate([nrm((NB, ML_HEADS), 0.1),
                                        jnp.linspace(3.0, 6.0, ML_HEADS, dtype=f32) + nrm((NB, ML_HEADS), 0.1)], -1)
    inp['ml_conv_w'] = nrm((NB, ML_CONV, 2 * ML_QK), ML_CONV ** -0.5)
    inp['ml_conv_b'] = nrm((NB, 2 * ML_QK), 0.02)
    inp['ml_gn_g'] = 1.0 + nrm((NB, ML_V), 0.05)
    inp['ml_gn_b'] = nrm((NB, ML_V), 0.02)
    inp['ml_w_out'] = nrm((NB, ML_V, D), DN_BETA * ML_V ** -0.5)
    inp['rt_w_in'] = nrm((NC, D, RT_IN), D ** -0.5)
    inp['rt_gn_g'] = 1.0 + nrm((NC, RT_V), 0.05)
    inp['rt_gn_b'] = nrm((NC, RT_V), 0.02)
    inp['rt_w_out'] = nrm((NC, RT_V, D), DN_BETA * RT_V ** -0.5)
    inp['moe_w_grp'] = nrm((DEPTH, D, MOE_GROUPS), D ** -0.5)
    inp['moe_b_grp'] = nrm((DEPTH, MOE_GROUPS), 0.01)
    inp['moe_w_exp'] = nrm((DEPTH, D, MOE_EXPERTS), D ** -0.5)
    inp['moe_b_exp'] = nrm((DEPTH, MOE_EXPERTS), 0.01)
    inp['moe_w1'] = nrm((DEPTH, MOE_EXPERTS, D, MOE_HIDDEN), D ** -0.5)
    inp['moe_w3'] = nrm((DEPTH, MOE_EXPERTS, D, MOE_HIDDEN), D ** -0.5)
    inp['moe_w2'] = nrm((DEPTH, MOE_EXPERTS, MOE_HIDDEN, D), DN_BETA * MOE_HIDDEN ** -0.5)
    return inp


def reference(x, c, ada_w, ada_b, ln_g, ln_b,
              rw_mu, rw_w_rkv, rw_w0, rw_w1, rw_w2, rw_a0, rw_a1, rw_a2, rw_v0, rw_v1, rw_v2,
              rw_g1, rw_g2, rw_kk, rw_ka, rw_rk, rw_gn_g, rw_gn_b, rw_w_out,
              ml_w_in, ml_b_gate, ml_conv_w, ml_conv_b, ml_gn_g, ml_gn_b, ml_w_out,
              rt_w_in, rt_gn_g, rt_gn_b, rt_w_out,
              moe_w_grp, moe_b_grp, moe_w_exp, moe_b_exp, moe_w1, moe_w3, moe_w2):
    v_first = None
    for i in range(DEPTH):
        kind, j = i % N_MIXERS, i // N_MIXERS
        shift, scale, gate = ada_modulation(c, ada_w[i, 0], ada_b[i, 0])
        h = x * (1 + scale) + shift
        if kind == 0:
            vmix = None if j == 0 else (rw_v0[j - 1], rw_v1[j - 1], rw_v2[j - 1])
            y, v_first = rwkv7_mixer(h, v_first, rw_mu[j], rw_w_rkv[j], rw_w0[j], rw_w1[j], rw_w2[j],
                                     rw_a0[j], rw_a1[j], rw_a2[j], rw_g1[j], rw_g2[j], rw_kk[j], rw_ka[j],
                                     rw_rk[j], rw_gn_g[j], rw_gn_b[j], rw_w_out[j], vmix)
        elif kind == 1:
            y = mlstm_mixer(h, ml_w_in[j], ml_b_gate[j], ml_conv_w[j], ml_conv_b[j],
                            ml_gn_g[j], ml_gn_b[j], ml_w_out[j])
        else:
            y = retention_mixer(h, rt_w_in[j], rt_gn_g[j], rt_gn_b[j], rt_w_out[j])
        x = layer_norm(DN_ALPHA * x + (1 + gate) * y, ln_g[i, 0], ln_b[i, 0])
        shift, scale, gate = ada_modulation(c, ada_w[i, 1], ada_b[i, 1])
        y = hier_moe(x * (1 + scale) + shift, moe_w_grp[i], moe_b_grp[i], moe_w_exp[i], moe_b_exp[i],
                     moe_w1[i], moe_w3[i], moe_w2[i])
        x = layer_norm(DN_ALPHA * x + (1 + gate) * y, ln_g[i, 1], ln_b[i, 1])
    return x
```

```python
import math
from contextlib import ExitStack
import numpy as np
import concourse.bass as bass
import concourse.mybir as mybir
from concourse.bass_utils import run_bass_kernel_spmd

F32 = mybir.dt.float32
BF16 = mybir.dt.bfloat16
AF = mybir.ActivationFunctionType
ALU = mybir.AluOpType
AX = mybir.AxisListType

D = 1024
T = 2048
NT = T // 128
KC = D // 128
DEPTH = 4
DN_ALPHA = (2 * DEPTH) ** 0.25
LN_EPS = 1e-5
NE = 32
HID = 512
BIG = 1.0e4


class Tk:
    __slots__ = ("name", "wr", "rd", "dsem")

    def __init__(self, name):
        self.name = name
        self.wr = None
        self.rd = {}
        self.dsem = None


class Sync:
    def __init__(self, nc, es):
        self.nc = nc
        self.es = es
        self.eng = {"pe": nc.tensor, "dve": nc.vector, "act": nc.scalar, "pool": nc.gpsimd, "sp": nc.sync}
        self.sem = {}
        self.cnt = {}
        for k in ("pe", "dve", "act", "pool"):
            self.sem[k] = es.enter_context(nc.semaphore("s_" + k))
            self.cnt[k] = 0
        self.free = {"sw": [], "hw": []}
        self.owners = []
        self.nd = 0
        self.seen = {e: {} for e in self.eng}
        self.nwait = 0
        self.ninst = 0

    def _dsem(self, tk, q):
        kind = "sw" if q == "pool" else "hw"
        if tk.dsem is None:
            tk.dsem = {}
        if kind not in tk.dsem:
            if self.free[kind]:
                tk.dsem[kind] = self.free[kind].pop()
            else:
                key = "d%s%d" % (kind, self.nd)
                self.nd += 1
                self.sem[key] = self.es.enter_context(self.nc.semaphore(key))
                self.cnt[key] = 0
                tk.dsem[kind] = key
            self.owners.append((tk, kind))
        return tk.dsem[kind]

    def _wait(self, e, ev):
        key, val = ev
        if self.seen[e].get(key, 0) >= val:
            return
        self.seen[e][key] = val
        self.eng[e].wait_ge(self.sem[key], val)
        self.nwait += 1

    def _deps(self, e, reads, writes):
        evs = {}

        def add(ev):
            if ev is None:
                return
            k, v = ev
            if e == "pe" and k == "pe":
                return
            if evs.get(k, 0) < v:
                evs[k] = v

        for t in reads:
            add(t.wr)
        for t in writes:
            add(t.wr)
            for k, v in t.rd.items():
                add((k, v))
        for k, v in evs.items():
            self._wait(e, (k, v))

    def _post(self, ev, reads, writes):
        k, v = ev
        for t in reads:
            if t.rd.get(k, 0) < v:
                t.rd[k] = v
        for t in writes:
            t.wr = ev
            t.rd = {}

    def op(self, e, fn, reads=(), writes=(), signal=True):
        self._deps(e, reads, writes)
        ins = fn(self.eng[e])
        self.ninst += 1
        ev = (e, self.cnt[e] + 1)
        if signal:
            ins.then_inc(self.sem[e], 1)
            self.cnt[e] += 1
        self._post(ev, reads, writes)
        return ins

    def dma(self, q, out, in_, reads=(), writes=(), **kw):
        self._deps(q, reads, writes)
        key = self._dsem(writes[0], q)
        ins = self.eng[q].dma_start(out=out, in_=in_, **kw)
        ins.then_inc(self.sem[key], 16)
        self.cnt[key] += 16
        self.ninst += 1
        self._post((key, self.cnt[key]), reads, writes)
        return ins

    def barrier(self):
        for e in self.eng:
            for key, val in self.cnt.items():
                if val > 0:
                    self._wait(e, (key, val))
        for tk, kind in self.owners:
            self.free[kind].append(tk.dsem.pop(kind))
        self.owners = []

    def finish(self, tks):
        self.barrier()


class Prog:
    def __init__(self, cfg):
        self.cfg = cfg
        self.nc = bass.Bass("TRN2", target_bir_lowering=False)
        self.es = ExitStack()
        self.S = Sync(self.nc, self.es)
        self.dram = {}
        self.psn = 0

    def din(self, name, shape, dt=F32):
        ap = self.nc.dram_tensor(name, list(shape), dt, kind="ExternalInput").ap()
        self.dram[name] = ap
        return ap

    def dout(self, name, shape, dt=F32):
        ap = self.nc.dram_tensor(name, list(shape), dt, kind="ExternalOutput").ap()
        self.dram[name] = ap
        return ap

    def dscratch(self, name, shape, dt=F32):
        kind = "ExternalOutput" if self.cfg.get("dbg_scratch") else "Internal"
        ap = self.nc.dram_tensor(name, list(shape), dt, kind=kind).ap()
        self.dram[name] = ap
        return ap

    def dump(self, name, ap, tks, dt=F32):
        if name not in self.cfg.get("dumps", ()):
            return
        d = self.dout("dump_" + name, list(ap.shape), dt)
        self.S.dma("sp", d, ap, reads=list(tks), writes=[Tk("dump_" + name)])

    def sb(self, name, shape, dt=F32, es=None):
        self.psn += 1
        return (es or self.es).enter_context(self.nc.sbuf_tensor("sb%d_%s" % (self.psn, name), list(shape), dt))

    def ps(self, name, shape, dt=F32, es=None):
        self.psn += 1
        return (es or self.es).enter_context(self.nc.psum_tensor("ps%d_%s" % (self.psn, name), list(shape), dt))


def _layer_norm_tile(P, S, src, src_tk, dst, dst_tk, gB, bB, gb_tk, st, st_tk, tmp=None, tmp_tk=None):
    nc = P.nc
    for c in range(2):
        S.op("dve", lambda e, c=c: e.bn_stats(out=st[:, c * 6:(c + 1) * 6], in_=src[:, c * 512:(c + 1) * 512]),
             reads=[src_tk], writes=[st_tk])
    S.op("dve", lambda e: e.bn_aggr(out=st[:, 12:14], in_=st[:, 0:12]), reads=[st_tk], writes=[st_tk])
    S.op("dve", lambda e: e.tensor_scalar_add(out=st[:, 15:16], in0=st[:, 13:14], scalar1=LN_EPS),
         reads=[st_tk], writes=[st_tk])
    S.op("act", lambda e: e.activation(out=st[:, 15:16], in_=st[:, 15:16], func=AF.Ln), reads=[st_tk],
         writes=[st_tk])
    S.op("act", lambda e: e.activation(out=st[:, 14:15], in_=st[:, 15:16], func=AF.Exp, scale=-0.5), reads=[st_tk],
         writes=[st_tk])
    S.op("dve", lambda e: e.tensor_scalar(out=dst, in0=src, scalar1=st[:, 12:13], scalar2=st[:, 14:15],
                                          op0=ALU.subtract, op1=ALU.mult), reads=[src_tk, st_tk], writes=[dst_tk])
    S.op("pool", lambda e: e.tensor_tensor(out=dst, in0=dst, in1=gB, op=ALU.mult), reads=[dst_tk, gb_tk],
         writes=[dst_tk])
    S.op("pool", lambda e: e.tensor_tensor(out=dst, in0=dst, in1=bB, op=ALU.add), reads=[dst_tk, gb_tk],
         writes=[dst_tk])


def build(cfg):
    P = Prog(cfg)
    nc, S, es = P.nc, P.S, P.es
    layers = cfg.get("layers", list(range(DEPTH)))
    subs = cfg.get("subs", (0, 1))

    x_d = P.din("x", [T, D])
    c_d = P.din("c", [1, D])
    ada_w = P.din("ada_w", [DEPTH, 2, D, 3 * D])
    ada_b = P.din("ada_b", [DEPTH, 2, 3 * D])
    ln_g = P.din("ln_g", [DEPTH, 2, D])
    ln_b = P.din("ln_b", [DEPTH, 2, D])
    moe_wr = P.din("moe_wr", [DEPTH, D, 36])
    moe_br = P.din("moe_br", [DEPTH, 36])
    moe_w1 = P.din("moe_w1", [DEPTH, NE, D, HID])
    moe_w3 = P.din("moe_w3", [DEPTH, NE, D, HID])
    moe_w2 = P.din("moe_w2", [DEPTH, NE, HID, D])
    ident_d = P.din("ident", [128, 128])
    P.din("rt_w_in", [1, D, 6 * D])
    P.din("rt_gn_g", [1, 2 * D])
    P.din("rt_gn_b", [1, 2 * D])
    P.din("rt_w_out", [1, 2 * D, D])
    for nm, shp in (("rw_mu", [2, 6, D]), ("rw_w_rkv", [2, 3, D, D]), ("rw_w0", [2, D]), ("rw_w1", [2, D, 64]),
                    ("rw_w2", [2, 64, D]), ("rw_a0", [2, D]), ("rw_a1", [2, D, 64]), ("rw_a2", [2, 64, D]),
                    ("rw_v0", [1, D]), ("rw_v1", [1, D, 32]), ("rw_v2", [1, 32, D]), ("rw_g1", [2, D, 128]),
                    ("rw_g2", [2, 128, D]), ("rw_kk", [2, D]), ("rw_ka", [2, D]), ("rw_rk", [2, D]),
                    ("rw_gn_g", [2, D]), ("rw_gn_b", [2, D]), ("rw_w_out", [2, D, D])):
        P.din(nm, shp)
    for nm, w in (("rw_tri_i", 128), ("rw_tri_e", 128), ("rw_mask4", 512), ("rw_maskl", 128), ("rw_blk", 128),
                  ("rw_sel2", 2)):
        P.din(nm, [128, w])
    P.din("ml_w_in", [1, D, 3 * D + 16])
    P.din("ml_b_gate", [1, 16])
    P.din("ml_conv_w", [1, 4, D])
    P.din("ml_conv_b", [1, D])
    P.din("ml_gn_g", [1, D])
    P.din("ml_gn_b", [1, D])
    P.din("ml_w_out", [1, D, D])
    P.din("tri_incl", [128, 128])
    P.din("ones", [128, 128])
    P.din("rt_cos", [128, T])
    P.din("rt_sin", [128, T])
    P.din("rt_tab", [4, 128, 16, 128])
    out_d = P.dout("out", [T, D])

    X = P.sb("X", [128, NT, D], F32)
    Xtk = [Tk("X%d" % n) for n in range(NT)]
    HT = P.sb("HT", [128, KC, T], BF16)
    HTtk = [Tk("HT%d" % n) for n in range(NT)]
    MOD = P.sb("MOD", [128, 3 * D], F32)
    MODtk = Tk("MOD")
    LNG = P.sb("LNG", [128, D], F32)
    LNB = P.sb("LNB", [128, D], F32)
    LNtk = Tk("LNGB")
    ident = P.sb("ident", [128, 128], F32)
    identtk = Tk("ident")
    csb = P.sb("csb", [128, KC, 128], BF16)
    csbtk = Tk("csb")
    ST = P.sb("ST", [128, 2, 16], F32)
    STtk = [Tk("ST0"), Tk("ST1")]

    S.dma("sp", ident[:], ident_d[:, :], writes=[identtk])
    for n in range(NT):
        S.dma("sp", X[:, n, :], x_d[n * 128:(n + 1) * 128, :], writes=[Xtk[n]])

    with ExitStack() as les:
        c_sb = P.sb("c_sb", [128, KC], F32, les)
        ctk = Tk("c")
        S.dma("sp", c_sb[:], c_d[0, :].rearrange("(k p) -> p k", p=128), writes=[ctk],
              allow_slow_non_contiguous=True)
        S.op("act", lambda e: e.activation(out=c_sb[:], in_=c_sb[:], func=AF.Silu), reads=[ctk], writes=[ctk])
        for k in range(KC):
            S.op("dve", lambda e, k=k: e.tensor_copy(out=csb[:, k, :], in_=c_sb[:, k:k + 1].to_broadcast([128, 128])),
                 reads=[ctk], writes=[csbtk])
        S.finish([csbtk])

    PSB = [P.ps("psb%d" % i, [128, 512], F32) for i in range(8)]
    PStk = [Tk("ps%d" % i) for i in range(8)]

    def ada_mod(i, k):
        with ExitStack() as les:
            wb = [P.sb("adaw%d" % j, [128, KC, 512], BF16, les) for j in range(2)]
            wtk = [Tk("adaw0"), Tk("adaw1")]
            bb = P.sb("adab", [128, 3 * D], F32, les)
            btk = Tk("adab")
            S.dma("sp", bb[:], ada_b[i, k:k + 1, :].to_broadcast([128, 3 * D]), writes=[btk])
            S.dma("sp", LNG[:], ln_g[i, k:k + 1, :].to_broadcast([128, D]), writes=[LNtk])
            S.dma("sp", LNB[:], ln_b[i, k:k + 1, :].to_broadcast([128, D]), writes=[LNtk])
            for nchunk in range(6):
                j = nchunk % 2
                S.dma("pool", wb[j][:], ada_w[i, k, :, nchunk * 512:(nchunk + 1) * 512].rearrange(
                    "(kc p) n -> p kc n", p=128), writes=[wtk[j]])
                pb = nchunk % 2
                for kc in range(KC):
                    S.op("pe", lambda e, kc=kc, j=j, pb=pb: e.matmul(PSB[pb][:], csb[:, kc, :], wb[j][:, kc, :],
                                                                   start=(kc == 0), stop=(kc == KC - 1)),
                         reads=[csbtk, wtk[j]], writes=[PStk[pb]], signal=(kc == KC - 1))
                sl = slice(nchunk * 512, (nchunk + 1) * 512)
                S.op("dve", lambda e, pb=pb, sl=sl: e.tensor_tensor(out=MOD[:, sl], in0=PSB[pb][:], in1=bb[:, sl],
                                                                   op=ALU.add),
                     reads=[PStk[pb], btk], writes=[MODtk])
            S.op("dve", lambda e: e.tensor_scalar_add(out=MOD[:, D:3 * D], in0=MOD[:, D:3 * D], scalar1=1.0),
                 reads=[MODtk], writes=[MODtk])
            P.dump("csb", csb[:], [csbtk], BF16)
            P.dump("wb", wb[1][:], [wtk[1]], BF16)
            P.dump("bb", bb[:], [btk])
            P.dump("MOD", MOD[:], [MODtk])
            S.finish([MODtk])

    def modulate_transpose():
        with ExitStack() as les:
            hb = [P.sb("hmod%d" % j, [128, D], F32, les) for j in range(2)]
            htk = [Tk("hmod0"), Tk("hmod1")]
            for n in range(NT):
                j = n % 2
                S.op("dve", lambda e, n=n, j=j: e.tensor_tensor(out=hb[j][:], in0=X[:, n, :], in1=MOD[:, D:2 * D],
                                                               op=ALU.mult),
                     reads=[Xtk[n], MODtk], writes=[htk[j]])
                S.op("pool", lambda e, j=j: e.tensor_tensor(out=hb[j][:], in0=hb[j][:], in1=MOD[:, 0:D], op=ALU.add),
                     reads=[htk[j], MODtk], writes=[htk[j]])
                for half in range(2):
                    pb = (2 * n + half) % 4
                    for q in range(4):
                        kc = half * 4 + q
                        S.op("pe", lambda e, kc=kc, q=q, pb=pb, j=j: e.transpose(
                            PSB[pb][:, q * 128:(q + 1) * 128], hb[j][:, kc * 128:(kc + 1) * 128], ident[:]),
                             reads=[htk[j], identtk], writes=[PStk[pb]], signal=(q == 3))
                    S.op("act", lambda e, half=half, pb=pb, n=n: e.copy(
                        out=HT[:, half * 4:(half + 1) * 4, n * 128:(n + 1) * 128],
                        in_=PSB[pb][:].rearrange("p (q t) -> p q t", q=4)),
                         reads=[PStk[pb]], writes=[HTtk[n]])
            P.dump("HT", HT[:], HTtk, BF16)
            S.finish(HTtk)

    def deepnorm_from_X():
        for n in range(NT):
            j = n % 2
            _layer_norm_tile(P, S, X[:, n, :], Xtk[n], X[:, n, :], Xtk[n], LNG[:], LNB[:], LNtk, ST[:, j, :], STtk[j])

    def moe(i):
        with ExitStack() as les:
            wr = P.sb("wr", [128, KC, 36], BF16, les)
            wrtk = Tk("wr")
            brb = P.sb("brb", [128, 36], F32, les)
            LG = P.sb("LG", [128, NT, 36], F32, les)
            LGtk = Tk("LG")
            G = P.sb("G", [128, NT, NE], F32, les)
            Gtk = Tk("G")
            t1 = P.sb("t1", [128, NT, NE], F32, les)
            t2 = P.sb("t2", [128, NT, NE], F32, les)
            sm = P.sb("sm", [128, 8, NT], F32, les)
            gtk = Tk("gating")
            W1 = [P.sb("W1_%d" % j, [128, KC, HID], BF16, les) for j in range(2)]
            W3 = [P.sb("W3_%d" % j, [128, KC, HID], BF16, les) for j in range(2)]
            W2 = [P.sb("W2_%d" % j, [128, 4, D], BF16, les) for j in range(2)]
            W1tk = [Tk("W1a"), Tk("W1b")]
            W3tk = [Tk("W3a"), Tk("W3b")]
            W2tk = [Tk("W2a"), Tk("W2b")]
            A = [P.sb("A_%d" % j, [128, 4, 512], BF16, les) for j in range(2)]
            Atk = [Tk("Aa"), Tk("Ab")]
            SL = [P.sb("SL_%d" % j, [128, 512], BF16, les) for j in range(2)]
            SLtk = [Tk("SLa"), Tk("SLb")]

            def load_expert(e):
                j = e % 2
                S.dma("pool", W1[j][:], moe_w1[i, e].rearrange("(kc p) n -> p kc n", p=128), writes=[W1tk[j]])
                S.dma("pool", W3[j][:], moe_w3[i, e].rearrange("(kc p) n -> p kc n", p=128), writes=[W3tk[j]])
                S.dma("pool", W2[j][:], moe_w2[i, e].rearrange("(kc p) n -> p kc n", p=128), writes=[W2tk[j]])

            S.dma("pool", wr[:], moe_wr[i].rearrange("(kc p) n -> p kc n", p=128), writes=[wrtk])
            S.dma("sp", brb[:], moe_br[i:i + 1, :].to_broadcast([128, 36]), writes=[wrtk])
            load_expert(0)

            for n in range(NT):
                pb = n % 2
                for kc in range(KC):
                    S.op("pe", lambda e, kc=kc, n=n, pb=pb: e.matmul(PSB[pb][:, 0:36], HT[:, kc, n * 128:(n + 1) * 128],
                                                                   wr[:, kc, :], start=(kc == 0), stop=(kc == KC - 1)),
                         reads=[HTtk[n], wrtk], writes=[PStk[pb]], signal=(kc == KC - 1))
                S.op("dve", lambda e, n=n, pb=pb: e.tensor_tensor(out=LG[:, n, :], in0=PSB[pb][:, 0:36], in1=brb[:],
                                                                 op=ALU.add),
                     reads=[PStk[pb], wrtk], writes=[LGtk])
            P.dump("LG", LG[:], [LGtk])
            gl = LG[:, :, 0:4]
            el = LG[:, :, 4:36]
            gmax, gsum, m1, m2, s1, s2 = (sm[:, q, :] for q in range(6))
            gm4 = t2[:, :, 0:4]
            pen = t2[:, :, 4:8]
            ex4 = t2[:, :, 8:12]

            def dv(fn, reads=(LGtk,), writes=(gtk,)):
                S.op("dve", fn, reads=list(reads) + [gtk], writes=list(writes))

            bc4 = lambda a: a.unsqueeze(2).to_broadcast([128, NT, 4])
            bc32 = lambda a: a.unsqueeze(2).to_broadcast([128, NT, NE])
            dv(lambda e: e.tensor_reduce(out=gmax, in_=gl, axis=AX.X, op=ALU.max))
            dv(lambda e: e.tensor_tensor(out=gm4, in0=gl, in1=bc4(gmax), op=ALU.is_ge))
            dv(lambda e: e.tensor_tensor(out=ex4, in0=gl, in1=bc4(gmax), op=ALU.subtract))
            S.op("act", lambda e: e.activation(out=ex4, in_=ex4, func=AF.Exp), reads=[gtk], writes=[gtk])
            dv(lambda e: e.tensor_reduce(out=gsum, in_=ex4, axis=AX.X, op=ALU.add))
            dv(lambda e: e.reciprocal(out=gsum, in_=gsum))
            dv(lambda e: e.tensor_scalar(out=pen, in0=gm4, scalar1=BIG, scalar2=-BIG, op0=ALU.mult, op1=ALU.add))
            dv(lambda e: e.tensor_tensor(out=t1[:].rearrange("p t (g e) -> p t g e", g=4),
                                         in0=el.rearrange("p t (g e) -> p t g e", g=4),
                                         in1=pen.unsqueeze(3).to_broadcast([128, NT, 4, 8]), op=ALU.add))
            dv(lambda e: e.tensor_reduce(out=m1, in_=t1[:], axis=AX.X, op=ALU.max))
            dv(lambda e: e.tensor_tensor(out=G[:], in0=t1[:], in1=bc32(m1), op=ALU.is_ge), writes=(gtk, Gtk))
            dv(lambda e: e.scalar_tensor_tensor(out=t1[:], in0=G[:], scalar=-BIG, in1=t1[:], op0=ALU.mult, op1=ALU.add),
               reads=(LGtk, Gtk))
            dv(lambda e: e.tensor_reduce(out=m2, in_=t1[:], axis=AX.X, op=ALU.max))
            dv(lambda e: e.tensor_tensor(out=t2[:], in0=t1[:], in1=bc32(m2), op=ALU.is_ge))
            dv(lambda e: e.tensor_tensor(out=s1, in0=m1, in1=m2, op=ALU.subtract))
            S.op("act", lambda e: e.activation(out=s1, in_=s1, func=AF.Sigmoid), reads=[gtk], writes=[gtk])
            dv(lambda e: e.tensor_scalar(out=s2, in0=s1, scalar1=-1.0, scalar2=1.0, op0=ALU.mult, op1=ALU.add))
            dv(lambda e: e.tensor_tensor(out=s1, in0=s1, in1=gsum, op=ALU.mult))
            dv(lambda e: e.tensor_tensor(out=s2, in0=s2, in1=gsum, op=ALU.mult))
            dv(lambda e: e.tensor_tensor(out=G[:], in0=G[:], in1=bc32(s1), op=ALU.mult), reads=(LGtk, Gtk),
               writes=(gtk, Gtk))
            dv(lambda e: e.tensor_tensor(out=t2[:], in0=t2[:], in1=bc32(s2), op=ALU.mult))
            dv(lambda e: e.tensor_tensor(out=G[:], in0=G[:], in1=t2[:], op=ALU.add), reads=(LGtk, Gtk),
               writes=(gtk, Gtk))
            if cfg.get("dbg_gate"):
                S.dma("sp", P.dram["dbg_gate"].rearrange("(n p) e -> p n e", p=128), G[:], reads=[Gtk],
                      writes=[Tk("dbg_gate")])

            for n in range(NT):
                S.op("act", lambda e, n=n: e.mul(out=X[:, n, :], in_=X[:, n, :], mul=DN_ALPHA), reads=[Xtk[n]],
                     writes=[Xtk[n]])

            NB = T // 512
            items = [(e, b) for e in range(NE) for b in range(NB)]

            def stage1(idx):
                e, b = items[idx]
                j = e % 2
                a = idx % 2
                if b == 1 and e + 1 < NE:
                    load_expert(e + 1)
                if b == 0:
                    S.op("dve", lambda en, j=j: en.tensor_tensor(
                        out=W2[j][:], in0=W2[j][:], in1=MOD[:, 2 * D:3 * D].unsqueeze(1).to_broadcast([128, 4, D]),
                        op=ALU.mult), reads=[W2tk[j], MODtk], writes=[W2tk[j]])
                tsl = slice(b * 512, (b + 1) * 512)
                htks = HTtk[b * 4:(b + 1) * 4]
                for hc in range(4):
                    p1 = (hc % 2) * 2
                    p3 = p1 + 1
                    for kc in range(KC):
                        S.op("pe", lambda en, kc=kc, hc=hc, p1=p1, j=j: en.matmul(
                            PSB[p1][:], W1[j][:, kc, hc * 128:(hc + 1) * 128], HT[:, kc, tsl],
                            start=(kc == 0), stop=(kc == KC - 1)),
                             reads=htks + [W1tk[j]], writes=[PStk[p1]], signal=(kc == KC - 1))
                    for kc in range(KC):
                        S.op("pe", lambda en, kc=kc, hc=hc, p3=p3, j=j: en.matmul(
                            PSB[p3][:], W3[j][:, kc, hc * 128:(hc + 1) * 128], HT[:, kc, tsl],
                            start=(kc == 0), stop=(kc == KC - 1)),
                             reads=htks + [W3tk[j]], writes=[PStk[p3]], signal=(kc == KC - 1))
                    sj = hc % 2
                    S.op("act", lambda en, p1=p1, sj=sj: en.activation(out=SL[sj][:], in_=PSB[p1][:], func=AF.Silu),
                         reads=[PStk[p1]], writes=[SLtk[sj]])
                    S.op("dve", lambda en, p3=p3, sj=sj, hc=hc, a=a: en.tensor_tensor(
                        out=A[a][:, hc, :], in0=PSB[p3][:], in1=SL[sj][:], op=ALU.mult),
                         reads=[PStk[p3], SLtk[sj]], writes=[Atk[a]])

            def stage2(idx):
                e, b = items[idx]
                j = e % 2
                a = idx % 2
                for tt in range(4):
                    n = b * 4 + tt
                    pb = 4 + (tt % 2) * 2
                    for nh in range(2):
                        for hc in range(4):
                            S.op("pe", lambda en, hc=hc, nh=nh, tt=tt, pb=pb: en.matmul(
                                PSB[pb + nh][:], A[a][:, hc, tt * 128:(tt + 1) * 128],
                                W2[j][:, hc, nh * 512:(nh + 1) * 512], start=(hc == 0), stop=(hc == 3)),
                                 reads=[Atk[a], W2tk[j]], writes=[PStk[pb + nh]], signal=(hc == 3))
                    for nh in range(2):
                        S.op("dve", lambda en, nh=nh, pb=pb, n=n, e=e: en.scalar_tensor_tensor(
                            out=X[:, n, nh * 512:(nh + 1) * 512], in0=PSB[pb + nh][:], scalar=G[:, n, e:e + 1],
                            in1=X[:, n, nh * 512:(nh + 1) * 512], op0=ALU.mult, op1=ALU.add),
                             reads=[PStk[pb + nh], Gtk, Xtk[n]], writes=[Xtk[n]])

            ne_run = cfg.get("moe_experts", NE)
            items = [(e, b) for e in range(ne_run) for b in range(NB)]
            stage1(0)
            for idx in range(len(items)):
                if idx + 1 < len(items):
                    stage1(idx + 1)
                stage2(idx)
            S.finish(Xtk + W1tk + W2tk + W3tk + Atk + SLtk + [Gtk, gtk, LGtk, wrtk])

    xs_d = P.dscratch("xs", [T, D])
    Zv = X[:].bitcast(BF16)
    identb = P.sb("identb", [128, 128], BF16)
    S.op("dve", lambda e: e.tensor_copy(out=identb[:], in_=ident[:]), reads=[identtk], writes=[identtk])

    def spill_X():
        tk = Tk("xs")
        for n in range(NT):
            S.dma("sp", xs_d[n * 128:(n + 1) * 128, :], X[:, n, :], reads=[Xtk[n]], writes=[tk])
        S.barrier()

    def mixer_epilogue(w_out_ap, nz):
        with ExitStack() as les:
            WO = P.sb("WO", [128, nz, D], BF16, les)
            WOtk = Tk("WO")
            for c0 in range(0, nz, 8):
                S.dma("pool", WO[:, c0:c0 + 8, :], w_out_ap[c0 * 128:(c0 + 8) * 128, :].rearrange(
                    "(c p) n -> p c n", p=128), writes=[WOtk])
            ZT = [P.sb("ZT%d" % j, [128, nz, 128], BF16, les) for j in range(2)]
            ZTtk = [Tk("ZT0"), Tk("ZT1")]
            XO = [P.sb("XO%d" % j, [128, D], F32, les) for j in range(2)]
            XOtk = [Tk("XO0"), Tk("XO1")]
            for n in range(NT):
                j = n % 2
                S.dma("sp", XO[j][:], xs_d[n * 128:(n + 1) * 128, :], writes=[XOtk[j]])
                for c0 in range(0, nz, 8):
                    pb = (c0 // 8) % 2
                    pv = PSB[pb][:].bitcast(BF16)
                    for c in range(c0, c0 + 8):
                        S.op("pe", lambda e, c=c, c0=c0, pv=pv, n=n: e.transpose(
                            pv[:, (c - c0) * 128:(c - c0 + 1) * 128], Zv[:, n, c * 128:(c + 1) * 128], identb[:]),
                             reads=[Xtk[n], identtk], writes=[PStk[pb]], signal=(c == c0 + 7))
                    S.op("act", lambda e, c0=c0, pv=pv, j=j: e.copy(
                        out=ZT[j][:, c0:c0 + 8, :], in_=pv.rearrange("p (c t) -> p c t", c=8)),
                         reads=[PStk[pb]], writes=[ZTtk[j]])
                for nh in range(2):
                    pb = 2 + (n % 2) * 2 + nh
                    for c in range(nz):
                        S.op("pe", lambda e, c=c, nh=nh, pb=pb, j=j: e.matmul(
                            PSB[pb][:], ZT[j][:, c, :], WO[:, c, nh * 512:(nh + 1) * 512],
                            start=(c == 0), stop=(c == nz - 1)),
                             reads=[ZTtk[j], WOtk], writes=[PStk[pb]], signal=(c == nz - 1))
                    S.op("dve", lambda e, nh=nh, pb=pb, n=n: e.tensor_tensor(
                        out=X[:, n, nh * 512:(nh + 1) * 512], in0=PSB[pb][:],
                        in1=MOD[:, 2 * D + nh * 512:2 * D + (nh + 1) * 512], op=ALU.mult),
                         reads=[PStk[pb], MODtk, Xtk[n]], writes=[Xtk[n]])
                S.op("dve", lambda e, n=n, j=j: e.scalar_tensor_tensor(
                    out=X[:, n, :], in0=XO[j][:], scalar=DN_ALPHA, in1=X[:, n, :], op0=ALU.mult, op1=ALU.add),
                     reads=[XOtk[j], Xtk[n]], writes=[Xtk[n]])
                _layer_norm_tile(P, S, X[:, n, :], Xtk[n], X[:, n, :], Xtk[n], LNG[:], LNB[:], LNtk,
                                 ST[:, j, :], STtk[j])
            S.barrier()

    def head_norm_tile(src_ps, src_tk, width, eps, gB, bB, gbtk, st, sttk, tmp, tmptk):
        S.op("dve", lambda e: e.bn_stats(out=st[:, 0:6], in_=src_ps), reads=[src_tk], writes=[sttk])
        S.op("dve", lambda e: e.bn_aggr(out=st[:, 12:14], in_=st[:, 0:6]), reads=[sttk], writes=[sttk])
        S.op("dve", lambda e: e.tensor_scalar_add(out=st[:, 15:16], in0=st[:, 13:14], scalar1=eps),
             reads=[sttk], writes=[sttk])
        S.op("act", lambda e: e.activation(out=st[:, 15:16], in_=st[:, 15:16], func=AF.Ln), reads=[sttk],
             writes=[sttk])
        S.op("act", lambda e: e.activation(out=st[:, 14:15], in_=st[:, 15:16], func=AF.Exp, scale=-0.5),
             reads=[sttk], writes=[sttk])
        S.op("dve", lambda e: e.tensor_scalar(out=tmp, in0=src_ps, scalar1=st[:, 12:13], scalar2=st[:, 14:15],
                                              op0=ALU.subtract, op1=ALU.mult), reads=[src_tk, sttk],
             writes=[tmptk])
        S.op("pool", lambda e: e.tensor_tensor(out=tmp, in0=tmp, in1=gB, op=ALU.mult), reads=[tmptk, gbtk],
             writes=[tmptk])
        S.op("pool", lambda e: e.tensor_tensor(out=tmp, in0=tmp, in1=bB, op=ALU.add), reads=[tmptk, gbtk],
             writes=[tmptk])

    def retention(j):
        RH, DKh, DVh = 4, 256, 512
        w_in = P.dram["rt_w_in"]
        with ExitStack() as les:
            QT = P.sb("QT", [128, 2, T], BF16, les)
            KT = P.sb("KT", [128, 2, T], BF16, les)
            VH = P.sb("VH", [128, NT, DVh], BF16, les)
            QTtk, KTtk, VHtk = Tk("QT"), Tk("KT"), Tk("VH")
            for h in range(RH):
                with ExitStack() as aes:
                    WQ = P.sb("WQ", [128, KC, DKh], BF16, aes)
                    WK = P.sb("WK", [128, KC, DKh], BF16, aes)
                    WV = P.sb("WV", [128, KC, DVh], BF16, aes)
                    COS = P.sb("COS", [128, T], F32, aes)
                    SIN = P.sb("SIN", [128, T], F32, aes)
                    Wtk, CStk = Tk("Wqkv"), Tk("cs")
                    S.dma("pool", WQ[:], w_in[j, :, h * DKh:(h + 1) * DKh].rearrange("(kc p) n -> p kc n", p=128),
                          writes=[Wtk])
                    S.dma("pool", WK[:], w_in[j, :, D + h * DKh:D + (h + 1) * DKh].rearrange(
                        "(kc p) n -> p kc n", p=128), writes=[Wtk])
                    S.dma("pool", WV[:], w_in[j, :, 2 * D + h * DVh:2 * D + (h + 1) * DVh].rearrange(
                        "(kc p) n -> p kc n", p=128), writes=[Wtk])
                    S.dma("sp", COS[:], P.dram["rt_cos"][:, :], writes=[CStk])
                    S.dma("sp", SIN[:], P.dram["rt_sin"][:, :], writes=[CStk])
                    tA = [P.sb("rtA%d" % q, [128, 512], F32, aes) for q in range(4)]
                    tAtk = [Tk("rtA%d" % q) for q in range(4)]
                    for (Wt, OT, OTtk) in ((WQ, QT, QTtk), (WK, KT, KTtk)):
                        for tb in range(4):
                            tsl = slice(tb * 512, (tb + 1) * 512)
                            htks = HTtk[tb * 4:(tb + 1) * 4]
                            pbs = (4 + (tb % 2) * 2, 5 + (tb % 2) * 2)
                            for dc in range(2):
                                for kc in range(KC):
                                    S.op("pe", lambda e, kc=kc, dc=dc, Wt=Wt: e.matmul(
                                        PSB[pbs[dc]][:], Wt[:, kc, dc * 128:(dc + 1) * 128], HT[:, kc, tsl],
                                        start=(kc == 0), stop=(kc == KC - 1)),
                                         reads=htks + [Wtk], writes=[PStk[pbs[dc]]], signal=(kc == KC - 1))
                            x1, x2 = PSB[pbs[0]][:], PSB[pbs[1]][:]
                            k1, k2 = PStk[pbs[0]], PStk[pbs[1]]
                            S.op("dve", lambda e: e.tensor_tensor(out=tA[0][:], in0=x1, in1=COS[:, tsl], op=ALU.mult),
                                 reads=[k1, CStk], writes=[tAtk[0]])
                            S.op("dve", lambda e: e.tensor_tensor(out=tA[1][:], in0=x2, in1=SIN[:, tsl], op=ALU.mult),
                                 reads=[k2, CStk], writes=[tAtk[1]])
                            S.op("dve", lambda e: e.tensor_tensor(out=tA[2][:], in0=x1, in1=SIN[:, tsl], op=ALU.mult),
                                 reads=[k1, CStk], writes=[tAtk[2]])
                            S.op("dve", lambda e: e.tensor_tensor(out=tA[3][:], in0=x2, in1=COS[:, tsl], op=ALU.mult),
                                 reads=[k2, CStk], writes=[tAtk[3]])
                            S.op("pool", lambda e, OT=OT: e.tensor_tensor(out=OT[:, 0, tsl], in0=tA[0][:], in1=tA[1][:],
                                                                         op=ALU.subtract),
                                 reads=[tAtk[0], tAtk[1]], writes=[OTtk])
                            S.op("pool", lambda e, OT=OT: e.tensor_tensor(out=OT[:, 1, tsl], in0=tA[2][:], in1=tA[3][:],
                                                                         op=ALU.add),
                                 reads=[tAtk[2], tAtk[3]], writes=[OTtk])
                    for n in range(NT):
                        pb = 4 + n % 4
                        for kc in range(KC):
                            S.op("pe", lambda e, kc=kc, n=n, pb=pb: e.matmul(
                                PSB[pb][:], HT[:, kc, n * 128:(n + 1) * 128], WV[:, kc, :],
                                start=(kc == 0), stop=(kc == KC - 1)),
                                 reads=[HTtk[n], Wtk], writes=[PStk[pb]], signal=(kc == KC - 1))
                        S.op("act", lambda e, n=n, pb=pb: e.copy(out=VH[:, n, :], in_=PSB[pb][:]),
                             reads=[PStk[pb]], writes=[VHtk])
                    S.barrier()
                with ExitStack() as bes:
                    TAB = P.sb("TAB", [128, 16, 128], F32, bes)
                    GNG = P.sb("GNG", [128, DVh], F32, bes)
                    GNB = P.sb("GNB", [128, DVh], F32, bes)
                    WG = P.sb("WG", [128, KC, DVh], BF16, bes)
                    TABtk, GNtk, WGtk = Tk("TAB"), Tk("GN"), Tk("WG")
                    S.dma("sp", TAB[:], P.dram["rt_tab"][h], writes=[TABtk])
                    S.dma("sp", GNG[:], P.dram["rt_gn_g"][j:j + 1, h * DVh:(h + 1) * DVh].to_broadcast([128, DVh]),
                          writes=[GNtk])
                    S.dma("sp", GNB[:], P.dram["rt_gn_b"][j:j + 1, h * DVh:(h + 1) * DVh].to_broadcast([128, DVh]),
                          writes=[GNtk])
                    S.dma("pool", WG[:], w_in[j, :, 4 * D + h * DVh:4 * D + (h + 1) * DVh].rearrange(
                        "(kc p) n -> p kc n", p=128), writes=[WGtk])
                    PM = [P.sb("PM%d" % q, [128, 4, 128], BF16, bes) for q in range(2)]
                    PMtk = [Tk("PM0"), Tk("PM1")]
                    SG = [P.sb("SG%d" % q, [128, DVh], F32, bes) for q in range(2)]
                    SGtk = [Tk("SG0"), Tk("SG1")]
                    YN = [P.sb("YN%d" % q, [128, DVh], F32, bes) for q in range(2)]
                    YNtk = [Tk("YN0"), Tk("YN1")]
                    gi = 0
                    for sb in range(NT):
                        ya = 2 + sb % 2
                        for jg in range(0, sb + 1, 4):
                            nj = min(4, sb + 1 - jg)
                            sp = gi % 2
                            gi += 1
                            for q in range(nj):
                                jb = jg + q
                                for dc in range(2):
                                    S.op("pe", lambda e, dc=dc, q=q, jb=jb, sp=sp, sb=sb: e.matmul(
                                        PSB[sp][:, q * 128:(q + 1) * 128], KT[:, dc, jb * 128:(jb + 1) * 128],
                                        QT[:, dc, sb * 128:(sb + 1) * 128], start=(dc == 0), stop=(dc == 1)),
                                         reads=[KTtk, QTtk], writes=[PStk[sp]], signal=(dc == 1 and q == nj - 1))
                            r0 = 15 - sb + jg
                            S.op("dve", lambda e, sp=sp, nj=nj, r0=r0: e.tensor_tensor(
                                out=PM[sp][:, 0:nj, :], in0=PSB[sp][:, 0:nj * 128].rearrange("p (q t) -> p q t", q=nj),
                                in1=TAB[:, r0:r0 + nj, :], op=ALU.mult),
                                 reads=[PStk[sp], TABtk], writes=[PMtk[sp]])
                            for q in range(nj):
                                jb = jg + q
                                S.op("pe", lambda e, q=q, jb=jb, sp=sp, ya=ya, sb=sb: e.matmul(
                                    PSB[ya][:], PM[sp][:, q, :], VH[:, jb, :], start=(jb == 0), stop=(jb == sb)),
                                     reads=[PMtk[sp], VHtk], writes=[PStk[ya]], signal=(jb == sb))
                        gp = 4 + sb % 2
                        for kc in range(KC):
                            S.op("pe", lambda e, kc=kc, sb=sb, gp=gp: e.matmul(
                                PSB[gp][:], HT[:, kc, sb * 128:(sb + 1) * 128], WG[:, kc, :],
                                start=(kc == 0), stop=(kc == KC - 1)),
                                 reads=[HTtk[sb], WGtk], writes=[PStk[gp]], signal=(kc == KC - 1))
                        q2 = sb % 2
                        S.op("act", lambda e, gp=gp, q2=q2: e.activation(out=SG[q2][:], in_=PSB[gp][:], func=AF.Silu),
                             reads=[PStk[gp]], writes=[SGtk[q2]])
                        head_norm_tile(PSB[ya][:], PStk[ya], DVh, 1e-6, GNG[:], GNB[:], GNtk, ST[:, q2, :], STtk[q2],
                                       YN[q2][:], YNtk[q2])
                        S.op("pool", lambda e, q2=q2, sb=sb, h=h: e.tensor_tensor(
                            out=Zv[:, sb, h * DVh:(h + 1) * DVh], in0=YN[q2][:], in1=SG[q2][:], op=ALU.mult),
                             reads=[YNtk[q2], SGtk[q2]], writes=[Xtk[sb]])
                    S.barrier()
        mixer_epilogue(P.dram["rt_w_out"][j], 16)

    fs_d = P.dscratch("fs", [8, T])

    def mlstm(j):
        MH, DKh, DVh = 8, 64, 128
        w_in = P.dram["ml_w_in"]
        with ExitStack() as les:
            QK = P.sb("QK", [128, KC, T], BF16, les)
            QKtk = Tk("QK")
            BJ = P.sb("BJ", [128, NT, MH], F32, les)
            BJtk = Tk("BJ")
            TRI = P.sb("TRI", [128, 128], F32, les)
            ONES = P.sb("ONES", [128, 128], F32, les)
            Ctk = Tk("mlconst")
            S.dma("sp", TRI[:], P.dram["tri_incl"][:, :], writes=[Ctk])
            S.dma("sp", ONES[:], P.dram["ones"][:, :], writes=[Ctk])
            with ExitStack() as aes:
                WQK = P.sb("WQK", [128, KC, D], BF16, aes)
                WGT = P.sb("WGT", [128, KC, 16], BF16, aes)
                CW = P.sb("CW", [128, 4, KC], F32, aes)
                CB = P.sb("CB", [128, KC], F32, aes)
                BG = P.sb("BG", [128, 16], F32, aes)
                Wtk = Tk("mlW")
                for c0 in range(0, KC, 4):
                    S.dma("pool", WQK[:, :, c0 * 128:(c0 + 4) * 128], w_in[j, :, c0 * 128:(c0 + 4) * 128].rearrange(
                        "(kc p) n -> p kc n", p=128), writes=[Wtk])
                S.dma("pool", WGT[:], w_in[j, :, 3 * D:3 * D + 16].rearrange("(kc p) n -> p kc n", p=128),
                      writes=[Wtk])
                for tap in range(4):
                    S.dma("sp", CW[:, tap, :], P.dram["ml_conv_w"][j, tap].rearrange("(c p) -> p c", p=128),
                          writes=[Wtk], allow_slow_non_contiguous=True)
                S.dma("sp", CB[:], P.dram["ml_conv_b"][j].rearrange("(c p) -> p c", p=128), writes=[Wtk],
                      allow_slow_non_contiguous=True)
                S.dma("sp", BG[:], P.dram["ml_b_gate"][j:j + 1, :].to_broadcast([128, 16]), writes=[Wtk])
                RAW = [P.sb("RAW%d" % q, [128, T + 3], F32, aes) for q in range(2)]
                RAWtk = [Tk("RAW0"), Tk("RAW1")]
                ACC = P.sb("ACC", [128, T], F32, aes)
                ACCtk = Tk("ACC")
                for q in range(2):
                    S.op("pool", lambda e, q=q: e.memset(RAW[q][:, 0:3], 0.0), writes=[RAWtk[q]])
                for c in range(KC):
                    rq = c % 2
                    for tb in range(4):
                        pb = 4 + tb
                        tsl = slice(tb * 512, (tb + 1) * 512)
                        for kc in range(KC):
                            S.op("pe", lambda e, kc=kc, c=c, pb=pb, tsl=tsl: e.matmul(
                                PSB[pb][:], WQK[:, kc, c * 128:(c + 1) * 128], HT[:, kc, tsl],
                                start=(kc == 0), stop=(kc == KC - 1)),
                                 reads=HTtk[tb * 4:(tb + 1) * 4] + [Wtk], writes=[PStk[pb]], signal=(kc == KC - 1))
                        S.op("act", lambda e, pb=pb, tb=tb, rq=rq: e.copy(
                            out=RAW[rq][:, 3 + tb * 512:3 + (tb + 1) * 512], in_=PSB[pb][:]),
                             reads=[PStk[pb]], writes=[RAWtk[rq]])
                    S.op("dve", lambda e, c=c, rq=rq: e.tensor_scalar(
                        out=ACC[:], in0=RAW[rq][:, 0:T], scalar1=CW[:, 0, c:c + 1], scalar2=CB[:, c:c + 1],
                        op0=ALU.mult, op1=ALU.add), reads=[RAWtk[rq], Wtk], writes=[ACCtk])
                    for tap in range(1, 4):
                        S.op("dve", lambda e, c=c, rq=rq, tap=tap: e.scalar_tensor_tensor(
                            out=ACC[:], in0=RAW[rq][:, tap:tap + T], scalar=CW[:, tap, c:c + 1], in1=ACC[:],
                            op0=ALU.mult, op1=ALU.add), reads=[RAWtk[rq], Wtk, ACCtk], writes=[ACCtk])
                    S.op("act", lambda e, c=c: e.activation(out=QK[:, c, :], in_=ACC[:], func=AF.Silu),
                         reads=[ACCtk], writes=[QKtk])
                GT = P.sb("GT", [128, NT, 16], F32, aes)
                LF = P.sb("LF", [128, NT, MH], F32, aes)
                FF = P.sb("FF", [128, NT, MH], F32, aes)
                GTtk, LFtk, FFtk = Tk("GT"), Tk("LF"), Tk("FF")
                for n in range(NT):
                    pb = n % 2
                    for kc in range(KC):
                        S.op("pe", lambda e, kc=kc, n=n, pb=pb: e.matmul(
                            PSB[pb][:, 0:16], HT[:, kc, n * 128:(n + 1) * 128], WGT[:, kc, :],
                            start=(kc == 0), stop=(kc == KC - 1)),
                             reads=[HTtk[n], Wtk], writes=[PStk[pb]], signal=(kc == KC - 1))
                    S.op("dve", lambda e, n=n, pb=pb: e.tensor_tensor(out=GT[:, n, :], in0=PSB[pb][:, 0:16], in1=BG[:],
                                                                     op=ALU.add),
                         reads=[PStk[pb], Wtk], writes=[GTtk])
                S.op("act", lambda e: e.activation(out=LF[:], in_=GT[:, :, 8:16], func=AF.Exp, scale=-1.0),
                     reads=[GTtk], writes=[LFtk])
                S.op("act", lambda e: e.activation(out=LF[:], in_=LF[:], func=AF.Ln, bias=1.0),
                     reads=[LFtk], writes=[LFtk])
                S.op("dve", lambda e: e.tensor_scalar_mul(out=LF[:], in0=LF[:], scalar1=-1.0), reads=[LFtk],
                     writes=[LFtk])
                for n in range(NT):
                    pb = 2 + n % 2
                    for m in range(n):
                        S.op("pe", lambda e, m=m, pb=pb: e.matmul(PSB[pb][:, 0:MH], ONES[:], LF[:, m, :],
                                                                 start=(m == 0), stop=False),
                             reads=[LFtk, Ctk], writes=[PStk[pb]], signal=False)
                    S.op("pe", lambda e, n=n, pb=pb: e.matmul(PSB[pb][:, 0:MH], TRI[:], LF[:, n, :],
                                                             start=(n == 0), stop=True),
                         reads=[LFtk, Ctk], writes=[PStk[pb]])
                    S.op("dve", lambda e, n=n, pb=pb: e.tensor_copy(out=FF[:, n, :], in_=PSB[pb][:, 0:MH]),
                         reads=[PStk[pb]], writes=[FFtk])
                S.op("dve", lambda e: e.tensor_tensor(out=BJ[:], in0=GT[:, :, 0:8], in1=FF[:], op=ALU.subtract),
                     reads=[GTtk, FFtk], writes=[BJtk])
                S.op("dve", lambda e: e.tensor_scalar_add(out=BJ[:], in0=BJ[:], scalar1=math.log(DKh ** -0.5)),
                     reads=[BJtk], writes=[BJtk])
                fstk = Tk("fs")
                for hh in range(MH):
                    S.dma("sp", fs_d[hh].rearrange("(n p) -> p n", p=128), FF[:, :, hh], reads=[FFtk], writes=[fstk],
                          allow_slow_non_contiguous=True)
                P.dump("QK", QK[:], [QKtk], BF16)
                P.dump("FF", FF[:], [FFtk])
                S.barrier()
            with ExitStack() as bes:
                GNG = P.sb("GNG", [128, D], F32, bes)
                GNB = P.sb("GNB", [128, D], F32, bes)
                GNtk = Tk("GN")
                S.dma("sp", GNG[:], P.dram["ml_gn_g"][j:j + 1, :].to_broadcast([128, D]), writes=[GNtk])
                S.dma("sp", GNB[:], P.dram["ml_gn_b"][j:j + 1, :].to_broadcast([128, D]), writes=[GNtk])
                FB = [P.sb("FB%d" % q, [128, T], F32, bes) for q in range(2)]
                FBtk = [Tk("FB0"), Tk("FB1")]
                WV = [P.sb("WVh%d" % q, [128, KC, DVh], BF16, bes) for q in range(2)]
                WO = [P.sb("WOh%d" % q, [128, KC, DVh], BF16, bes) for q in range(2)]
                WVtk = [Tk("WV0"), Tk("WV1")]
                V1 = [P.sb("V1%d" % q, [128, NT, DVh + 1], BF16, bes) for q in range(2)]
                V1tk = [Tk("V10"), Tk("V11")]
                EW = [P.sb("EW%d" % q, [128, 4, 128], F32, bes) for q in range(2)]
                EWtk = [Tk("EW0"), Tk("EW1")]
                PM = [P.sb("PMm%d" % q, [128, 4, 128], BF16, bes) for q in range(2)]
                PMtk = [Tk("PM0"), Tk("PM1")]
                SO = [P.sb("SO%d" % q, [128, DVh], F32, bes) for q in range(2)]
                SOtk = [Tk("SO0"), Tk("SO1")]
                HB = [P.sb("HB%d" % q, [128, DVh], F32, bes) for q in range(2)]
                HBtk = [Tk("HB0"), Tk("HB1")]
                DN = [P.sb("DN%d" % q, [128, 2], F32, bes) for q in range(2)]
                DNtk = [Tk("DN0"), Tk("DN1")]
                for q in range(2):
                    S.op("pool", lambda e, q=q: e.memset(V1[q][:, :, DVh:DVh + 1], 1.0), writes=[V1tk[q]])
                gi = 0
                for h in range(MH):
                    hq = h % 2
                    S.dma("sp", FB[hq][:], fs_d[h:h + 1, :].to_broadcast([128, T]), writes=[FBtk[hq]])
                    S.dma("pool", WV[hq][:], w_in[j, :, D + h * DVh:D + (h + 1) * DVh].rearrange(
                        "(kc p) n -> p kc n", p=128), writes=[WVtk[hq]])
                    S.dma("pool", WO[hq][:], w_in[j, :, 2 * D + h * DVh:2 * D + (h + 1) * DVh].rearrange(
                        "(kc p) n -> p kc n", p=128), writes=[WVtk[hq]])
                    for n in range(NT):
                        pb = 4 + n % 2
                        for kc in range(KC):
                            S.op("pe", lambda e, kc=kc, n=n, pb=pb: e.matmul(
                                PSB[pb][:, 0:DVh], HT[:, kc, n * 128:(n + 1) * 128], WV[hq][:, kc, :],
                                start=(kc == 0), stop=(kc == KC - 1)),
                                 reads=[HTtk[n], WVtk[hq]], writes=[PStk[pb]], signal=(kc == KC - 1))
                        S.op("act", lambda e, n=n, pb=pb: e.copy(out=V1[hq][:, n, 0:DVh], in_=PSB[pb][:, 0:DVh]),
                             reads=[PStk[pb]], writes=[V1tk[hq]])
                    prow = slice((h % 2) * 64, (h % 2) * 64 + 64)
                    qc, kcq = h // 2, 4 + h // 2
                    for sb in range(NT):
                        ya = 2 + sb % 2
                        ssl = slice(sb * 128, (sb + 1) * 128)
                        for jg in range(0, sb + 1, 4):
                            nj = min(4, sb + 1 - jg)
                            sp = gi % 2
                            gi += 1
                            for q in range(nj):
                                jb = jg + q
                                S.op("pe", lambda e, q=q, jb=jb, sp=sp: e.matmul(
                                    PSB[sp][:, q * 128:(q + 1) * 128], QK[prow, kcq, jb * 128:(jb + 1) * 128],
                                    QK[prow, qc, ssl], start=True, stop=True),
                                     reads=[QKtk], writes=[PStk[sp]], signal=(q == nj - 1))
                                S.op("act", lambda e, q=q, jb=jb, sp=sp: e.activation(
                                    out=EW[sp][:, q, :], in_=FB[hq][:, ssl], func=AF.Exp, bias=BJ[:, jb, h:h + 1]),
                                     reads=[FBtk[hq], BJtk], writes=[EWtk[sp]])
                                if jb == sb:
                                    S.op("pool", lambda e, q=q, sp=sp: e.tensor_tensor(
                                        out=EW[sp][:, q, :], in0=EW[sp][:, q, :], in1=TRI[:], op=ALU.mult),
                                         reads=[EWtk[sp], Ctk], writes=[EWtk[sp]])
                            S.op("dve", lambda e, sp=sp, nj=nj: e.tensor_tensor(
                                out=PM[sp][:, 0:nj, :], in0=PSB[sp][:, 0:nj * 128].rearrange("p (q t) -> p q t", q=nj),
                                in1=EW[sp][:, 0:nj, :], op=ALU.mult),
                                 reads=[PStk[sp], EWtk[sp]], writes=[PMtk[sp]])
                            for q in range(nj):
                                jb = jg + q
                                S.op("pe", lambda e, q=q, jb=jb, sp=sp, ya=ya: e.matmul(
                                    PSB[ya][:, 0:DVh + 1], PM[sp][:, q, :], V1[hq][:, jb, :],
                                    start=(jb == 0), stop=(jb == sb)),
                                     reads=[PMtk[sp], V1tk[hq]], writes=[PStk[ya]], signal=(jb == sb))
                        gp = 6 + sb % 2
                        for kc in range(KC):
                            S.op("pe", lambda e, kc=kc, gp=gp, ssl=ssl: e.matmul(
                                PSB[gp][:, 0:DVh], HT[:, kc, ssl], WO[hq][:, kc, :],
                                start=(kc == 0), stop=(kc == KC - 1)),
                                 reads=[HTtk[sb], WVtk[hq]], writes=[PStk[gp]], signal=(kc == KC - 1))
                        q2 = sb % 2
                        S.op("act", lambda e, gp=gp, q2=q2: e.activation(out=SO[q2][:], in_=PSB[gp][:, 0:DVh],
                                                                        func=AF.Sigmoid),
                             reads=[PStk[gp]], writes=[SOtk[q2]])
                        S.op("act", lambda e, ya=ya, q2=q2: e.activation(
                            out=DN[q2][:, 0:1], in_=PSB[ya][:, DVh:DVh + 1], func=AF.Abs),
                             reads=[PStk[ya]], writes=[DNtk[q2]])
                        S.op("dve", lambda e, q2=q2: e.tensor_scalar_max(out=DN[q2][:, 0:1], in0=DN[q2][:, 0:1],
                                                                        scalar1=1.0),
                             reads=[DNtk[q2]], writes=[DNtk[q2]])
                        S.op("dve", lambda e, q2=q2: e.reciprocal(out=DN[q2][:, 1:2], in_=DN[q2][:, 0:1]),
                             reads=[DNtk[q2]], writes=[DNtk[q2]])
                        S.op("dve", lambda e, ya=ya, q2=q2: e.tensor_scalar_mul(
                            out=HB[q2][:], in0=PSB[ya][:, 0:DVh], scalar1=DN[q2][:, 1:2]),
                             reads=[PStk[ya], DNtk[q2]], writes=[HBtk[q2]])
                        head_norm_tile(HB[q2][:], HBtk[q2], DVh, 1e-6, GNG[:, h * DVh:(h + 1) * DVh],
                                       GNB[:, h * DVh:(h + 1) * DVh], GNtk, ST[:, q2, :], STtk[q2], HB[q2][:], HBtk[q2])
                        S.op("pool", lambda e, q2=q2, sb=sb, h=h: e.tensor_tensor(
                            out=Zv[:, sb, h * DVh:(h + 1) * DVh], in0=HB[q2][:], in1=SO[q2][:], op=ALU.mult),
                             reads=[HBtk[q2], SOtk[q2]], writes=[Xtk[sb]])
                S.barrier()
        mixer_epilogue(P.dram["ml_w_out"][j], 8)

    RWS = {}
    for nm, shp in (("R", [D, T]), ("K", [D, T]), ("A", [D, T]), ("V", [T, D]), ("VF", [T, D]), ("LW", [T, D]),
                    ("G", [T, D])):
        RWS[nm] = P.dscratch("rw_" + nm, shp)

    def rwkv_stage_a(i, j):
        first = (j == 0)
        dr = P.dram
        with ExitStack() as les:
            XS = P.sb("XS", [128, KC, T], BF16, les)
            XStk = Tk("XS")
            MU = P.sb("MU", [128, 6, KC], F32, les)
            MU1 = P.sb("MU1", [128, 6, KC], F32, les)
            MUtk = Tk("MU")
            for n in range(6):
                S.dma("sp", MU[:, n, :], dr["rw_mu"][j, n].rearrange("(c p) -> p c", p=128), writes=[MUtk],
                      allow_slow_non_contiguous=True)
            S.op("dve", lambda e: e.tensor_scalar(out=MU1[:], in0=MU[:], scalar1=-1.0, scalar2=1.0, op0=ALU.mult,
                                                  op1=ALU.add), reads=[MUtk], writes=[MUtk])
            WB = [P.sb("WBp%d" % q, [128, KC, D], BF16, les) for q in range(2)]
            WBtk = [Tk("WB0"), Tk("WB1")]
            STG = [P.sb("STG%d" % q, [128, 512], F32, les) for q in range(4)]
            STGtk = [Tk("STG%d" % q) for q in range(4)]
            OUTtk = [Tk("rwout%d" % q) for q in range(4)]
            LO = P.sb("LO", [128, T], BF16, les)
            LOtk = Tk("LO")
            L1 = P.sb("L1", [128, KC, 128], BF16, les)
            L2 = P.sb("L2", [128, D], BF16, les)
            Ltk = Tk("Lw")
            BCV = P.sb("BCV", [128, D], F32, les)
            A0 = P.sb("A0", [128, KC], F32, les)
            VFT = P.sb("VFT", [128, 512], F32, les)
            VFTtk = Tk("VFT")
            sctr = [0]

            def make_xs(n):
                for c in range(KC):
                    S.op("dve", lambda e, c=c: e.tensor_scalar_mul(out=XS[:, c, :], in0=HT[:, c, :],
                                                                   scalar1=MU1[:, n, c:c + 1]),
                         reads=HTtk + [MUtk], writes=[XStk])
                    S.op("dve", lambda e, c=c: e.scalar_tensor_tensor(
                        out=XS[:, c, 1:T], in0=HT[:, c, 0:T - 1], scalar=MU[:, n, c:c + 1], in1=XS[:, c, 1:T],
                        op0=ALU.mult, op1=ALU.add), reads=HTtk + [MUtk, XStk], writes=[XStk])

            def stage_out(ps_ap, pstk, dst_ap, func=None, bias=None, pre=None):
                q = sctr[0] % 4
                sctr[0] += 1
                if pre is not None:
                    pre(q)
                elif func is None:
                    S.op("act", lambda e: e.copy(out=STG[q][:], in_=ps_ap), reads=[pstk], writes=[STGtk[q]])
                else:
                    kw = {} if bias is None else {"bias": bias}
                    S.op("act", lambda e: e.activation(out=STG[q][:], in_=ps_ap, func=func, **kw),
                         reads=[pstk, Ltk], writes=[STGtk[q]])
                S.dma("sp", dst_ap, STG[q][:], reads=[STGtk[q]], writes=[OUTtk[q]])

            def proj_fm(Wt, Wtk_, dst, func=None, bias_fn=None, kdim=KC, rhs_fn=None, rtks=None):
                for c in range(KC):
                    for tb in range(4):
                        pb = (c * 4 + tb) % 4
                        tsl = slice(tb * 512, (tb + 1) * 512)
                        if rhs_fn is None:
                            for kc in range(KC):
                                S.op("pe", lambda e, kc=kc: e.matmul(PSB[pb][:], Wt[:, kc, c * 128:(c + 1) * 128],
                                                                     XS[:, kc, tsl], start=(kc == 0),
                                                                     stop=(kc == KC - 1)),
                                     reads=[XStk, Wtk_], writes=[PStk[pb]], signal=(kc == KC - 1))
                        else:
                            rhs_fn(pb, c, tsl)
                        stage_out(PSB[pb][:], PStk[pb], dst[c * 128:(c + 1) * 128, tsl], func,
                                  None if bias_fn is None else bias_fn(c))

            def proj_tm(lhs_fn, ltks, Wt, Wtk_, dst, nk, post=None):
                for n in range(NT):
                    for nh in range(2):
                        pb = 4 + (n * 2 + nh) % 4
                        for kc in range(nk):
                            S.op("pe", lambda e, kc=kc: e.matmul(PSB[pb][:], lhs_fn(kc, n), Wt(kc, nh),
                                                                 start=(kc == 0), stop=(kc == nk - 1)),
                                 reads=ltks + [Wtk_], writes=[PStk[pb]], signal=(kc == nk - 1))
                        dsl = dst[n * 128:(n + 1) * 128, nh * 512:(nh + 1) * 512]
                        if post is None:
                            stage_out(PSB[pb][:], PStk[pb], dsl)
                        else:
                            stage_out(PSB[pb][:], PStk[pb], dsl, pre=lambda q, pb=pb, n=n, nh=nh: post(q, pb, n, nh))

            def load_w(q, ap):
                for c0 in range(0, KC, 4):
                    S.dma("pool", WB[q][:, :, c0 * 128:(c0 + 4) * 128],
                          ap[:, c0 * 128:(c0 + 4) * 128].rearrange("(kc p) n -> p kc n", p=128), writes=[WBtk[q]])

            def lora1(w1_ap, r, func):
                S.dma("pool", L1[:, :, 0:r], w1_ap.rearrange("(kc p) n -> p kc n", p=128), writes=[Ltk])
                for tb in range(4):
                    pb = tb % 4
                    tsl = slice(tb * 512, (tb + 1) * 512)
                    for kc in range(KC):
                        S.op("pe", lambda e, kc=kc: e.matmul(PSB[pb][0:r, :], L1[:, kc, 0:r], XS[:, kc, tsl],
                                                             start=(kc == 0), stop=(kc == KC - 1)),
                             reads=[XStk, Ltk], writes=[PStk[pb]], signal=(kc == KC - 1))
                    if func is None:
                        S.op("act", lambda e: e.copy(out=LO[0:r, tsl], in_=PSB[pb][0:r, :]), reads=[PStk[pb]],
                             writes=[LOtk])
                    else:
                        S.op("act", lambda e: e.activation(out=LO[0:r, tsl], in_=PSB[pb][0:r, :], func=func),
                             reads=[PStk[pb]], writes=[LOtk])

            load_w(0, dr["rw_w_rkv"][j, 0])
            load_w(1, dr["rw_w_rkv"][j, 1])
            make_xs(0)
            proj_fm(WB[0], WBtk[0], RWS["R"])
            make_xs(1)
            proj_fm(WB[1], WBtk[1], RWS["K"])
            load_w(0, dr["rw_w_rkv"][j, 2])
            make_xs(2)
            if first:
                proj_tm(lambda kc, n: XS[:, kc, n * 128:(n + 1) * 128], [XStk],
                        lambda kc, nh: WB[0][:, kc, nh * 512:(nh + 1) * 512], WBtk[0], RWS["VF"], KC)
            else:
                lora1(dr["rw_v1"][j - 1], 32, None)
                S.dma("pool", L2[0:32, :], dr["rw_v2"][j - 1], writes=[Ltk])
                S.dma("sp", BCV[:], dr["rw_v0"][j - 1:j, :].to_broadcast([128, D]), writes=[Ltk])
                SGV = P.sb("SGV", [128, 512], F32, les)
                SGVtk = Tk("SGV")

                def vpost(q, pb, n, nh):
                    csl = slice(nh * 512, (nh + 1) * 512)
                    gp = (pb - 4 + 2) % 4
                    S.op("pe", lambda e: e.matmul(PSB[gp][:], LO[0:32, n * 128:(n + 1) * 128], L2[0:32, csl],
                                                  start=True, stop=True), reads=[LOtk, Ltk], writes=[PStk[gp]])
                    S.op("dve", lambda e: e.tensor_tensor(out=SGV[:], in0=PSB[gp][:], in1=BCV[:, csl], op=ALU.add),
                         reads=[PStk[gp], Ltk], writes=[SGVtk])
                    S.op("act", lambda e: e.activation(out=SGV[:], in_=SGV[:], func=AF.Sigmoid), reads=[SGVtk],
                         writes=[SGVtk])
                    S.dma("sp", VFT[:], RWS["VF"][n * 128:(n + 1) * 128, csl], writes=[VFTtk])
                    S.op("dve", lambda e: e.tensor_tensor(out=VFT[:], in0=VFT[:], in1=PSB[pb][:], op=ALU.subtract),
                         reads=[VFTtk, PStk[pb]], writes=[VFTtk])
                    S.op("dve", lambda e: e.tensor_tensor(out=VFT[:], in0=VFT[:], in1=SGV[:], op=ALU.mult),
                         reads=[VFTtk, SGVtk], writes=[VFTtk])
                    S.op("dve", lambda e: e.tensor_tensor(out=STG[q][:], in0=VFT[:], in1=PSB[pb][:], op=ALU.add),
                         reads=[VFTtk, PStk[pb]], writes=[STGtk[q]])

                proj_tm(lambda kc, n: XS[:, kc, n * 128:(n + 1) * 128], [XStk],
                        lambda kc, nh: WB[0][:, kc, nh * 512:(nh + 1) * 512], WBtk[0], RWS["V"], KC, post=vpost)
            make_xs(3)
            lora1(dr["rw_w1"][j], 64, AF.Tanh)
            S.dma("pool", L2[0:64, :], dr["rw_w2"][j], writes=[Ltk])
            S.dma("sp", BCV[:], dr["rw_w0"][j:j + 1, :].to_broadcast([128, D]), writes=[Ltk])

            def wpost(q, pb, n, nh):
                csl = slice(nh * 512, (nh + 1) * 512)
                S.op("dve", lambda e: e.tensor_tensor(out=STG[q][:], in0=PSB[pb][:], in1=BCV[:, csl], op=ALU.add),
                     reads=[PStk[pb], Ltk], writes=[STGtk[q]])
                S.op("act", lambda e: e.activation(out=STG[q][:], in_=STG[q][:], func=AF.Sigmoid),
                     reads=[STGtk[q]], writes=[STGtk[q]])

            proj_tm(lambda kc, n: LO[0:64, n * 128:(n + 1) * 128], [LOtk],
                    lambda kc, nh: L2[0:64, nh * 512:(nh + 1) * 512], Ltk, RWS["LW"], 1, post=wpost)
            make_xs(4)
            lora1(dr["rw_a1"][j], 64, None)
            S.dma("pool", L2[0:64, :], dr["rw_a2"][j], writes=[Ltk])
            S.dma("sp", A0[:], dr["rw_a0"][j].rearrange("(c p) -> p c", p=128), writes=[Ltk],
                  allow_slow_non_contiguous=True)

            def a_rhs(pb, c, tsl):
                S.op("pe", lambda e: e.matmul(PSB[pb][:], L2[0:64, c * 128:(c + 1) * 128], LO[0:64, tsl],
                                              start=True, stop=True), reads=[LOtk, Ltk], writes=[PStk[pb]])

            proj_fm(None, None, RWS["A"], func=AF.Sigmoid, bias_fn=lambda c: A0[:, c:c + 1], rhs_fn=a_rhs)
            make_xs(5)
            lora1(dr["rw_g1"][j], 128, AF.Sigmoid)
            S.dma("pool", L2[:, :], dr["rw_g2"][j], writes=[Ltk])
            proj_tm(lambda kc, n: LO[:, n * 128:(n + 1) * 128], [LOtk],
                    lambda kc, nh: L2[:, nh * 512:(nh + 1) * 512], Ltk, RWS["G"], 1)
            S.barrier()

    def rwkv_stage_b(i, j):
        first = (j == 0)
        dr = P.dram
        RW_EPS = 64e-5
        with ExitStack() as les:
            HTf = HT[:].bitcast(F32)
            FB_ = [HTf[:, 2 * q:2 * q + 2, :].rearrange("p a b -> p (a b)") for q in range(4)]
            FB_ += [P.sb("rwF%d" % q, [128, T], F32, les)[:] for q in range(3)]
            Rb, Kb, Ab, KPb, B4, B5, B6 = FB_
            Ftk = [Tk("rwFB%d" % q) for q in range(7)]
            Rtk, Ktk, Atk_, KPtk, B4tk, B5tk, B6tk = Ftk
            AR = P.sb("AR", [128, NT, 2, 128], F32, les)
            ARtk = Tk("AR")
            XF = X[:, :, 512:1024]
            VTM, SIGT, BHT, KHT = (XF[:, :, q * 128:(q + 1) * 128] for q in range(4))
            VTMtk, SIGTtk, BHTtk, KHTtk = Tk("VTM"), Tk("SIGT"), Tk("BHT"), Tk("KHT")
            CST = {}
            ctk = Tk("rwconst")
            for nm, w in (("rw_tri_i", 128), ("rw_tri_e", 128), ("rw_mask4", 512), ("rw_maskl", 128),
                          ("rw_blk", 128), ("rw_sel2", 2)):
                CST[nm] = P.sb(nm, [128, w], F32, les)
                S.dma("sp", CST[nm][:], dr[nm][:, :], writes=[ctk])
            PRM = P.sb("PRM", [128, 4, KC], F32, les)
            for q, nm in enumerate(("rw_kk", "rw_ka", "rw_rk")):
                S.dma("sp", PRM[:, q, :], dr[nm][j].rearrange("(c p) -> p c", p=128), writes=[ctk],
                      allow_slow_non_contiguous=True)
            S.op("dve", lambda e: e.tensor_scalar(out=PRM[:, 3, :], in0=PRM[:, 1, :], scalar1=-1.0, scalar2=1.0,
                                                  op0=ALU.mult, op1=ALU.add), reads=[ctk], writes=[ctk])
            GNG = P.sb("rwGNG", [128, D], F32, les)
            GNB = P.sb("rwGNB", [128, D], F32, les)
            S.dma("sp", GNG[:], dr["rw_gn_g"][j:j + 1, :].to_broadcast([128, D]), writes=[ctk])
            S.dma("sp", GNB[:], dr["rw_gn_b"][j:j + 1, :].to_broadcast([128, D]), writes=[ctk])
            PL = P.sb("PL", [128, 32], F32, les)
            PLtk = Tk("PL")
            CBON = P.sb("CBON", [128, NT, 2], F32, les)
            CBtk = Tk("CBON")
            SS = P.sb("SS", [128, 128], F32, les)
            SStk = Tk("SS")
            INN = P.sb("INN", [128, 128], F32, les)
            US = P.sb("US", [128, 128], F32, les)
            YS = P.sb("YS", [128, 128], F32, les)
            INNtk, UStk, YStk = Tk("INN"), Tk("US"), Tk("YS")
            MM = [[P.sb("MM%d%d" % (a, b), [128, 512], F32, les) for b in range(2)] for a in range(2)]
            MMtk = [[Tk("MM%d%d" % (a, b)) for b in range(2)] for a in range(2)]
            NPb_ = [[[P.sb("NP%d%d%d" % (a, b, q), [128, 128], F32, les) for q in range(2)] for b in range(2)]
                    for a in range(2)]
            NNb_ = [[[P.sb("NN%d%d%d" % (a, b, q), [128, 128], F32, les) for q in range(2)] for b in range(2)]
                    for a in range(2)]
            TT = [[P.sb("TT%d%d" % (a, b), [128, 128], F32, les) for b in range(2)] for a in range(2)]
            NPtk = [[[Tk("NP") for q in range(2)] for b in range(2)] for a in range(2)]
            NNtk = [[[Tk("NN") for q in range(2)] for b in range(2)] for a in range(2)]
            TTtk = [[Tk("TT%d%d" % (a, b)) for b in range(2)] for a in range(2)]
            GT_ = [P.sb("rwGT%d" % q, [128, 128], F32, les) for q in range(2)]
            GTtk = [Tk("rwGT0"), Tk("rwGT1")]
            YN = [P.sb("rwYN%d" % q, [128, 64], F32, les) for q in range(2)]
            YNtk = [Tk("rwYN0"), Tk("rwYN1")]
            S.op("pool", lambda e: e.memset(INN[:], 0.0), writes=[INNtk])
            S.op("pool", lambda e: e.memset(US[:], 0.0), writes=[UStk])
            v_src = RWS["VF"] if first else RWS["V"]
            v3 = lambda ap: ap.rearrange("p (n t) -> p n t", t=128)

            for c in range(KC):
                fsl = slice(c * 128, (c + 1) * 128)
                S.dma("sp", Rb, RWS["R"][fsl, :], writes=[Rtk])
                S.dma("sp", Kb, RWS["K"][fsl, :], writes=[Ktk])
                S.dma("sp", Ab, RWS["A"][fsl, :], writes=[Atk_])
                S.dma("sp", SIGT, RWS["LW"][:, fsl].rearrange("(n p) f -> p n f", p=128), writes=[SIGTtk])
                S.dma("sp", VTM, v_src[:, fsl].rearrange("(n p) f -> p n f", p=128), writes=[VTMtk])
                S.op("pool", lambda e: e.memset(SS[:], 0.0), writes=[SStk])
                kkp, kap, rkp, ka1 = (PRM[:, q, c:c + 1] for q in range(4))
                S.op("dve", lambda e: e.tensor_scalar(out=KPb, in0=Ab, scalar1=kap, scalar2=ka1, op0=ALU.mult,
                                                      op1=ALU.add), reads=[Atk_, ctk], writes=[KPtk])
                S.op("dve", lambda e: e.tensor_tensor(out=KPb, in0=KPb, in1=Kb, op=ALU.mult), reads=[KPtk, Ktk],
                     writes=[KPtk])
                S.op("dve", lambda e: e.scalar_tensor_tensor(out=B4, in0=Rb, scalar=rkp, in1=KPb, op0=ALU.mult,
                                                             op1=ALU.mult), reads=[Rtk, KPtk, ctk], writes=[B4tk])
                for n in range(NT):
                    pb = n % 2
                    S.op("pe", lambda e, n=n, pb=pb: e.matmul(PSB[pb][:, 0:2], B4[:, n * 128:(n + 1) * 128],
                                                             CST["rw_sel2"][:], start=True, stop=True),
                         reads=[B4tk, ctk], writes=[PStk[pb]])
                    S.op("act", lambda e, n=n, pb=pb: e.copy(out=CBON[:, n, :], in_=PSB[pb][:, 0:2]),
                         reads=[PStk[pb]], writes=[CBtk])
                S.op("dve", lambda e: e.tensor_scalar_mul(out=Kb, in0=Kb, scalar1=kkp), reads=[Ktk, ctk],
                     writes=[Ktk])
                S.op("dve", lambda e: e.tensor_tensor(out=B4, in0=Kb, in1=Kb, op=ALU.mult), reads=[Ktk, B4tk],
                     writes=[B4tk])
                for tb in range(4):
                    pb = tb % 2
                    tsl = slice(tb * 512, (tb + 1) * 512)
                    S.op("pe", lambda e, pb=pb, tsl=tsl: e.matmul(PSB[pb][:], CST["rw_blk"][:], B4[:, tsl],
                                                                 start=True, stop=True),
                         reads=[B4tk, ctk], writes=[PStk[pb]])
                    S.op("act", lambda e, pb=pb, tsl=tsl: e.activation(out=B5[:, tsl], in_=PSB[pb][:], func=AF.Sqrt),
                         reads=[PStk[pb]], writes=[B5tk])
                S.op("dve", lambda e: e.tensor_scalar_max(out=B5, in0=B5, scalar1=1e-12), reads=[B5tk], writes=[B5tk])
                S.op("dve", lambda e: e.reciprocal(out=B5, in_=B5), reads=[B5tk], writes=[B5tk])
                S.op("dve", lambda e: e.tensor_tensor(out=Kb, in0=Kb, in1=B5, op=ALU.mult), reads=[Ktk, B5tk],
                     writes=[Ktk])
                S.op("dve", lambda e: e.tensor_tensor(out=Ab, in0=Ab, in1=Kb, op=ALU.mult), reads=[Atk_, Ktk],
                     writes=[Atk_])
                for n in range(NT):
                    pb = n % 2
                    nsl = slice(n * 128, (n + 1) * 128)
                    S.op("pe", lambda e, n=n, pb=pb: e.matmul(PSB[pb][:, 0:128], SIGT[:, n, :], CST["rw_tri_i"][:],
                                                             start=True, stop=True),
                         reads=[SIGTtk, ctk], writes=[PStk[pb]], signal=False)
                    S.op("pe", lambda e, n=n, pb=pb: e.matmul(PSB[pb][:, 128:256], SIGT[:, n, :], CST["rw_tri_e"][:],
                                                             start=True, stop=True),
                         reads=[SIGTtk, ctk], writes=[PStk[pb]])
                    S.op("act", lambda e, pb=pb, nsl=nsl: e.activation(out=B4[:, nsl], in_=PSB[pb][:, 0:128],
                                                                      func=AF.Exp),
                         reads=[PStk[pb]], writes=[B4tk])
                    S.op("act", lambda e, pb=pb, nsl=nsl: e.activation(out=B5[:, nsl], in_=PSB[pb][:, 0:128],
                                                                      func=AF.Exp, scale=-1.0),
                         reads=[PStk[pb]], writes=[B5tk])
                    S.op("act", lambda e, pb=pb, nsl=nsl: e.activation(out=B6[:, nsl], in_=PSB[pb][:, 128:256],
                                                                      func=AF.Exp),
                         reads=[PStk[pb]], writes=[B6tk])
                S.op("dve", lambda e: e.tensor_tensor(out=AR[:, :, 1, :], in0=v3(Rb), in1=v3(B4), op=ALU.mult),
                     reads=[Rtk, B4tk], writes=[ARtk])
                S.op("dve", lambda e: e.scalar_tensor_tensor(out=AR[:, :, 0, :], in0=v3(Kb), scalar=-1.0, in1=v3(B6),
                                                             op0=ALU.mult, op1=ALU.mult),
                     reads=[Ktk, B6tk, ARtk], writes=[ARtk])
                S.op("dve", lambda e: e.tensor_tensor(out=KPb, in0=KPb, in1=B5, op=ALU.mult), reads=[KPtk, B5tk],
                     writes=[KPtk])
                S.op("dve", lambda e: e.tensor_tensor(out=Ab, in0=Ab, in1=B5, op=ALU.mult), reads=[Atk_, B5tk],
                     writes=[Atk_])
                ch3 = lambda ap: ap.rearrange("p (c l) -> p c l", l=64)
                S.op("dve", lambda e: e.tensor_copy(out=PL[:], in_=ch3(B4)[:, :, 63]), reads=[B4tk], writes=[PLtk])
                plb = PL[:].unsqueeze(2).to_broadcast([128, 32, 64])
                S.op("dve", lambda e: e.tensor_tensor(out=ch3(B5), in0=ch3(Ab), in1=plb, op=ALU.mult),
                     reads=[Atk_, PLtk, B5tk], writes=[B5tk])
                S.op("dve", lambda e: e.tensor_tensor(out=ch3(B6), in0=ch3(KPb), in1=plb, op=ALU.mult),
                     reads=[KPtk, PLtk, B6tk], writes=[B6tk])
                for n in range(NT):
                    nsl = slice(n * 128, (n + 1) * 128)
                    for (src, stk, dst, dtk, pb) in ((B5, B5tk, BHT, BHTtk, 0), (B6, B6tk, KHT, KHTtk, 1)):
                        S.op("pe", lambda e, src=src, pb=pb, nsl=nsl: e.transpose(PSB[pb][:, 0:128], src[:, nsl],
                                                                                   ident[:]),
                             reads=[stk, identtk], writes=[PStk[pb]])
                        S.op("act", lambda e, dst=dst, pb=pb, n=n: e.copy(out=dst[:, n, :], in_=PSB[pb][:, 0:128]),
                             reads=[PStk[pb]], writes=[dtk])

                def precompute(n):
                    sl_ = n % 2
                    nsl = slice(n * 128, (n + 1) * 128)
                    for h2 in range(2):
                        prow = slice(h2 * 64, h2 * 64 + 64)
                        mb = 2 + h2
                        S.op("pe", lambda e: e.matmul(PSB[mb][:, 0:256], Ab[prow, nsl],
                                                      AR[prow, n, :, :].rearrange("p a t -> p (a t)"),
                                                      start=True, stop=True),
                             reads=[Atk_, ARtk], writes=[PStk[mb]], signal=False)
                        S.op("pe", lambda e: e.matmul(PSB[mb][:, 256:512], KPb[prow, nsl],
                                                      AR[prow, n, :, :].rearrange("p a t -> p (a t)"),
                                                      start=True, stop=True),
                             reads=[KPtk, ARtk], writes=[PStk[mb]])
                        S.op("dve", lambda e: e.tensor_tensor(out=MM[sl_][h2][:], in0=PSB[mb][:],
                                                              in1=CST["rw_mask4"][:], op=ALU.mult),
                             reads=[PStk[mb], ctk], writes=[MMtk[sl_][h2]])
                        ib = 4 + h2
                        S.op("pe", lambda e: e.matmul(PSB[ib][:, 0:128], AR[prow, n, 0, :], Ab[prow, nsl],
                                                      start=True, stop=True),
                             reads=[Atk_, ARtk], writes=[PStk[ib]])
                        NPc, NNc = NPb_[sl_][h2], NNb_[sl_][h2]
                        NPk, NNk = NPtk[sl_][h2], NNtk[sl_][h2]
                        S.op("dve", lambda e: e.tensor_tensor(out=NNc[0][:], in0=PSB[ib][:, 0:128],
                                                              in1=CST["rw_maskl"][:], op=ALU.mult),
                             reads=[PStk[ib], ctk], writes=[NNk[0]])
                        np_ap, np_tk = MM[sl_][h2][:, 0:128], MMtk[sl_][h2]
                        nn_ap, nn_tk = NNc[0][:], NNk[0]
                        XPa, XPk = TT[sl_][h2], TTtk[sl_][h2]
                        S.op("dve", lambda e: e.tensor_tensor(out=XPa[:], in0=np_ap, in1=ident[:], op=ALU.add),
                             reads=[np_tk, identtk], writes=[XPk])
                        for k in range(5):
                            q = (k + 1) % 2
                            S.op("pe", lambda e: e.matmul(PSB[ib][:, 128:256], np_ap, nn_ap, start=True, stop=True),
                                 reads=[np_tk, nn_tk], writes=[PStk[ib]])
                            if k < 4:
                                S.op("pe", lambda e: e.matmul(PSB[ib][:, 256:384], nn_ap, np_ap, start=True, stop=True),
                                     reads=[np_tk, nn_tk], writes=[PStk[ib]])
                            S.op("act", lambda e: e.copy(out=NNc[q][:], in_=PSB[ib][:, 128:256]), reads=[PStk[ib]],
                                 writes=[NNk[q]])
                            if k < 4:
                                S.op("act", lambda e: e.copy(out=NPc[q][:], in_=PSB[ib][:, 256:384]),
                                     reads=[PStk[ib]], writes=[NPk[q]])
                            nn_ap, nn_tk = NNc[q][:], NNk[q]
                            if k < 4:
                                np_ap, np_tk = NPc[q][:], NPk[q]
                            S.op("pe", lambda e: e.matmul(PSB[ib][:, 384:512], nn_ap, XPa[:], start=True, stop=True),
                                 reads=[nn_tk, XPk], writes=[PStk[ib]])
                            S.op("dve", lambda e: e.tensor_tensor(out=XPa[:], in0=PSB[ib][:, 384:512], in1=XPa[:],
                                                                  op=ALU.add),
                                 reads=[PStk[ib], XPk], writes=[XPk])

                def chain(n):
                    sl_ = n % 2
                    b6, b7 = PSB[6], PSB[7]
                    k6, k7 = PStk[6], PStk[7]
                    for par in range(2):
                        tp = slice(par * 64, par * 64 + 64)
                        tc = slice(par * 64, par * 64 + 64)
                        chn = 2 * n + par
                        S.op("pe", lambda e: e.matmul(b6[tp, 0:128], AR[:, n, 0, tc], SS[:], start=True, stop=False),
                             reads=[ARtk, SStk], writes=[k6], signal=False)
                        for h2 in range(2):
                            ic = slice(h2 * 64, h2 * 64 + 64)
                            S.op("pe", lambda e, h2=h2, ic=ic: e.matmul(
                                b6[tp, ic], MM[sl_][h2][:, 256 + par * 64:256 + par * 64 + 64], VTM[:, n, ic],
                                start=False, stop=(h2 == 1)),
                                 reads=[MMtk[sl_][h2], VTMtk], writes=[k6], signal=(h2 == 1))
                        S.op("act", lambda e: e.copy(out=INN[tp, :], in_=b6[tp, 0:128]), reads=[k6], writes=[INNtk])
                        for h2 in range(2):
                            ic = slice(h2 * 64, h2 * 64 + 64)
                            S.op("pe", lambda e, h2=h2, ic=ic: e.matmul(
                                b6[tp, 128 + h2 * 64:128 + h2 * 64 + 64], TT[sl_][h2][:, tc], INN[:, ic],
                                start=True, stop=True),
                                 reads=[TTtk[sl_][h2], INNtk], writes=[k6], signal=(h2 == 1))
                        S.op("dve", lambda e: e.tensor_copy(out=US[tp, :], in_=b6[tp, 128:256]), reads=[k6],
                             writes=[UStk])
                        S.op("pe", lambda e: e.matmul(b6[tp, 256:384], AR[:, n, 1, tc], SS[:], start=True, stop=False),
                             reads=[ARtk, SStk], writes=[k6], signal=False)
                        for h2 in range(2):
                            ic = slice(h2 * 64, h2 * 64 + 64)
                            oc = slice(256 + h2 * 64, 256 + h2 * 64 + 64)
                            S.op("pe", lambda e, h2=h2, ic=ic, oc=oc: e.matmul(
                                b6[tp, oc], MM[sl_][h2][:, 128 + par * 64:128 + par * 64 + 64], US[:, ic],
                                start=False, stop=False),
                                 reads=[MMtk[sl_][h2], UStk], writes=[k6], signal=False)
                            S.op("pe", lambda e, h2=h2, ic=ic, oc=oc: e.matmul(
                                b6[tp, oc], MM[sl_][h2][:, 384 + par * 64:384 + par * 64 + 64], VTM[:, n, ic],
                                start=False, stop=(h2 == 1)),
                                 reads=[MMtk[sl_][h2], VTMtk], writes=[k6], signal=(h2 == 1))
                        S.op("act", lambda e: e.copy(out=YS[tp, :], in_=b6[tp, 256:384]), reads=[k6], writes=[YStk])
                        S.op("pe", lambda e: e.matmul(b7[:, 0:128], BHT[tp, n, :], US[tp, :], start=True, stop=False),
                             reads=[BHTtk, UStk], writes=[k7], signal=False)
                        S.op("pe", lambda e: e.matmul(b7[:, 0:128], KHT[tp, n, :], VTM[tp, n, :], start=False,
                                                      stop=True),
                             reads=[KHTtk, VTMtk], writes=[k7])
                        for h2 in range(2):
                            pr = slice(h2 * 64, h2 * 64 + 64)
                            S.op("dve", lambda e, pr=pr: e.scalar_tensor_tensor(
                                out=SS[pr, pr], in0=SS[pr, pr], scalar=PL[pr, chn:chn + 1], in1=b7[pr, pr],
                                op0=ALU.mult, op1=ALU.add), reads=[SStk, PLtk, k7], writes=[SStk])

                def post(n):
                    gq = n % 2
                    S.dma("sp", GT_[gq][:], RWS["G"][n * 128:(n + 1) * 128, fsl], writes=[GTtk[gq]])
                    for h2 in range(2):
                        ic = slice(h2 * 64, h2 * 64 + 64)
                        gc = slice(c * 128 + h2 * 64, c * 128 + h2 * 64 + 64)
                        head_norm_tile(YS[:, ic], YStk, 64, RW_EPS, GNG[:, gc], GNB[:, gc], ctk, ST[:, h2, :],
                                       STtk[h2], YN[h2][:], YNtk[h2])
                        S.op("dve", lambda e, h2=h2, ic=ic: e.scalar_tensor_tensor(
                            out=YN[h2][:], in0=VTM[:, n, ic], scalar=CBON[:, n, h2:h2 + 1], in1=YN[h2][:],
                            op0=ALU.mult, op1=ALU.add), reads=[VTMtk, CBtk, YNtk[h2]], writes=[YNtk[h2]])
                        S.op("pool", lambda e, h2=h2, ic=ic, gc=gc: e.tensor_tensor(
                            out=Zv[:, n, gc], in0=YN[h2][:], in1=GT_[gq][:, ic], op=ALU.mult),
                             reads=[YNtk[h2], GTtk[gq]], writes=[Xtk[n]])

                precompute(0)
                for n in range(NT):
                    if n + 1 < NT:
                        precompute(n + 1)
                    chain(n)
                    post(n)
                if cfg.get("rw_pairs") and c + 1 >= cfg["rw_pairs"]:
                    break
            S.barrier()
        mixer_epilogue(dr["rw_w_out"][j], 8)

    if cfg.get("dbg_gate"):
        P.dout("dbg_gate", [T, NE])
    for i in layers:
        if 0 in subs:
            kind, jj = i % 3, i // 3
            ada_mod(i, 0)
            modulate_transpose()
            spill_X()
            if kind == 2:
                retention(jj)
            elif kind == 1:
                mlstm(jj)
            else:
                rwkv_stage_a(i, jj)
                if cfg.get("only") == "rwa":
                    break
                rwkv_stage_b(i, jj)
        if 1 in subs:
            ada_mod(i, 1)
            if cfg.get("only") == "ada":
                break
            modulate_transpose()
            moe(i)
            deepnorm_from_X()

    otk = Tk("out")
    for n in range(NT):
        S.dma("sp", out_d[n * 128:(n + 1) * 128, :], X[:, n, :], reads=[Xtk[n]], writes=[otk])
    S.barrier()
    es.close()
    return P


_CONSTS = {}


def _consts():
    if _CONSTS:
        return _CONSTS
    half = 128
    inv = 10000.0 ** -(np.arange(half, dtype=np.float64) / (half - 1))
    ang = (np.arange(T, dtype=np.float32)[:, None] * inv.astype(np.float32)[None, :]).astype(np.float32)
    _CONSTS["rt_cos"] = np.ascontiguousarray(np.cos(ang).T.astype(np.float32))
    _CONSTS["rt_sin"] = np.ascontiguousarray(np.sin(ang).T.astype(np.float32))
    tab = np.zeros((4, 128, 16, 128), np.float64)
    jj = np.arange(128)[:, None]
    ss = np.arange(128)[None, :]
    for h in range(4):
        lg = np.log1p(-2.0 ** (-5.0 - h))
        for r in range(16):
            dl = 15 - r
            ex = 128 * dl + ss - jj
            v = np.exp(lg * np.maximum(ex, 0)) / 16.0
            tab[h, :, r, :] = np.where(ex >= 0, v, 0.0)
    _CONSTS["rt_tab"] = tab.astype(np.float32)
    _CONSTS["tri_incl"] = np.triu(np.ones((128, 128), np.float32))
    _CONSTS["ones"] = np.ones((128, 128), np.float32)
    idx = np.arange(128)
    same = (idx[:, None] // 64) == (idx[None, :] // 64)
    up_i = same & (idx[:, None] <= idx[None, :])
    up_s = same & (idx[:, None] < idx[None, :])
    lo_s = same & (idx[:, None] > idx[None, :])
    cdec = -math.exp(-0.5)
    _CONSTS["rw_tri_i"] = (up_i * cdec).astype(np.float32)
    _CONSTS["rw_tri_e"] = (up_s * cdec).astype(np.float32)
    _CONSTS["rw_mask4"] = np.concatenate([up_s, up_i, up_s, up_i], axis=1).astype(np.float32)
    _CONSTS["rw_maskl"] = lo_s.astype(np.float32)
    _CONSTS["rw_blk"] = same.astype(np.float32)
    sel = np.zeros((128, 2), np.float32)
    sel[:64, 0] = 1.0
    sel[64:, 1] = 1.0
    _CONSTS["rw_sel2"] = sel
    return _CONSTS


def prep_inputs(inputs, b):
    f = lambda a: np.ascontiguousarray(a, dtype=np.float32)
    m = {
        "x": f(inputs["x"][b]),
        "c": f(inputs["c"][b:b + 1]),
        "ada_w": f(inputs["ada_w"]),
        "ada_b": f(inputs["ada_b"]),
        "ln_g": f(inputs["ln_g"]),
        "ln_b": f(inputs["ln_b"]),
        "moe_wr": f(np.concatenate([inputs["moe_w_grp"], inputs["moe_w_exp"]], axis=-1)),
        "moe_br": f(np.concatenate([inputs["moe_b_grp"], inputs["moe_b_exp"]], axis=-1)),
        "moe_w1": f(inputs["moe_w1"]),
        "moe_w3": f(inputs["moe_w3"]),
        "moe_w2": f(inputs["moe_w2"]),
        "ident": np.eye(128, dtype=np.float32),
    }
    for k in ("rt_w_in", "rt_gn_g", "rt_gn_b", "rt_w_out", "ml_w_in", "ml_b_gate", "ml_conv_w", "ml_conv_b",
              "ml_gn_g", "ml_gn_b", "ml_w_out"):
        m[k] = f(inputs[k])
    for k in ("rw_mu", "rw_w_rkv", "rw_w0", "rw_w1", "rw_w2", "rw_a0", "rw_a1", "rw_a2", "rw_v0", "rw_v1", "rw_v2",
              "rw_g1", "rw_g2", "rw_kk", "rw_ka", "rw_gn_g", "rw_gn_b", "rw_w_out"):
        m[k] = f(inputs[k])
    m["rw_rk"] = f(inputs["rw_rk"].reshape(2, D))
    m.update(_consts())
    return m


def kernel(**inputs):
    P = build({})
    in_maps = [prep_inputs(inputs, b) for b in range(8)]
    res = run_bass_kernel_spmd(P.nc, in_maps, core_ids=list(range(8)))
    return np.stack([np.asarray(r["out"]) for r in res.results], axis=0).astype(np.float32)
```

```python
import math
from contextlib import ExitStack
import numpy as np
import concourse.bass as bass
import concourse.mybir as mybir
from concourse.bass_utils import run_bass_kernel_spmd

F32 = mybir.dt.float32
BF16 = mybir.dt.bfloat16
AF = mybir.ActivationFunctionType
ALU = mybir.AluOpType
AX = mybir.AxisListType

D = 1024
T = 2048
NT = T // 128
KC = D // 128
DEPTH = 4
DN_ALPHA = (2 * DEPTH) ** 0.25
LN_EPS = 1e-5
NE = 32
HID = 512
BIG = 1.0e4


class Tk:
    __slots__ = ("name", "wr", "rd", "dsem")

    def __init__(self, name):
        self.name = name
        self.wr = None
        self.rd = {}
        self.dsem = None


class Sync:
    def __init__(self, nc, es):
        self.nc = nc
        self.es = es
        self.eng = {"pe": nc.tensor, "dve": nc.vector, "act": nc.scalar, "pool": nc.gpsimd, "sp": nc.sync}
        self.sem = {}
        self.cnt = {}
        for k in ("pe", "dve", "act", "pool"):
            self.sem[k] = es.enter_context(nc.semaphore("s_" + k))
            self.cnt[k] = 0
        self.free = {"sw": [], "hw": []}
        self.owners = []
        self.nd = 0
        self.seen = {e: {} for e in self.eng}
        self.nwait = 0
        self.ninst = 0

    def _dsem(self, tk, q):
        kind = "sw" if q == "pool" else "hw"
        if tk.dsem is None:
            tk.dsem = {}
        if kind not in tk.dsem:
            if self.free[kind]:
                tk.dsem[kind] = self.free[kind].pop()
            else:
                key = "d%s%d" % (kind, self.nd)
                self.nd += 1
                self.sem[key] = self.es.enter_context(self.nc.semaphore(key))
                self.cnt[key] = 0
                tk.dsem[kind] = key
            self.owners.append((tk, kind))
        return tk.dsem[kind]

    def _wait(self, e, ev):
        key, val = ev
        if self.seen[e].get(key, 0) >= val:
            return
        self.seen[e][key] = val
        self.eng[e].wait_ge(self.sem[key], val)
        self.nwait += 1

    def _deps(self, e, reads, writes):
        evs = {}

        def add(ev):
            if ev is None:
                return
            k, v = ev
            if e == "pe" and k == "pe":
                return
            if evs.get(k, 0) < v:
                evs[k] = v

        for t in reads:
            add(t.wr)
        for t in writes:
            add(t.wr)
            for k, v in t.rd.items():
                add((k, v))
        for k, v in evs.items():
            self._wait(e, (k, v))

    def _post(self, ev, reads, writes):
        k, v = ev
        for t in reads:
            if t.rd.get(k, 0) < v:
                t.rd[k] = v
        for t in writes:
            t.wr = ev
            t.rd = {}

    def op(self, e, fn, reads=(), writes=(), signal=True):
        self._deps(e, reads, writes)
        ins = fn(self.eng[e])
        self.ninst += 1
        ev = (e, self.cnt[e] + 1)
        if signal:
            ins.then_inc(self.sem[e], 1)
            self.cnt[e] += 1
        self._post(ev, reads, writes)
        return ins

    def dma(self, q, out, in_, reads=(), writes=(), **kw):
        self._deps(q, reads, writes)
        key = self._dsem(writes[0], q)
        ins = self.eng[q].dma_start(out=out, in_=in_, **kw)
        ins.then_inc(self.sem[key], 16)
        self.cnt[key] += 16
        self.ninst += 1
        self._post((key, self.cnt[key]), reads, writes)
        return ins

    def barrier(self):
        for e in self.eng:
            for key, val in self.cnt.items():
                if val > 0:
                    self._wait(e, (key, val))
        for tk, kind in self.owners:
            self.free[kind].append(tk.dsem.pop(kind))
        self.owners = []

    def finish(self, tks):
        self.barrier()


class Prog:
    def __init__(self, cfg):
        self.cfg = cfg
        self.nc = bass.Bass("TRN2", target_bir_lowering=False)
        self.es = ExitStack()
        self.S = Sync(self.nc, self.es)
        self.dram = {}
        self.psn = 0

    def din(self, name, shape, dt=F32):
        ap = self.nc.dram_tensor(name, list(shape), dt, kind="ExternalInput").ap()
        self.dram[name] = ap
        return ap

    def dout(self, name, shape, dt=F32):
        ap = self.nc.dram_tensor(name, list(shape), dt, kind="ExternalOutput").ap()
        self.dram[name] = ap
        return ap

    def dscratch(self, name, shape, dt=F32):
        kind = "ExternalOutput" if self.cfg.get("dbg_scratch") else "Internal"
        ap = self.nc.dram_tensor(name, list(shape), dt, kind=kind).ap()
        self.dram[name] = ap
        return ap

    def dump(self, name, ap, tks, dt=F32):
        if name not in self.cfg.get("dumps", ()):
            return
        d = self.dout("dump_" + name, list(ap.shape), dt)
        self.S.dma("sp", d, ap, reads=list(tks), writes=[Tk("dump_" + name)])

    def sb(self, name, shape, dt=F32, es=None):
        self.psn += 1
        return (es or self.es).enter_context(self.nc.sbuf_tensor("sb%d_%s" % (self.psn, name), list(shape), dt))

    def ps(self, name, shape, dt=F32, es=None):
        self.psn += 1
        return (es or self.es).enter_context(self.nc.psum_tensor("ps%d_%s" % (self.psn, name), list(shape), dt))


def _layer_norm_tile(P, S, src, src_tk, dst, dst_tk, gB, bB, gb_tk, st, st_tk, tmp=None, tmp_tk=None):
    nc = P.nc
    for c in range(2):
        S.op("dve", lambda e, c=c: e.bn_stats(out=st[:, c * 6:(c + 1) * 6], in_=src[:, c * 512:(c + 1) * 512]),
             reads=[src_tk], writes=[st_tk])
    S.op("dve", lambda e: e.bn_aggr(out=st[:, 12:14], in_=st[:, 0:12]), reads=[st_tk], writes=[st_tk])
    S.op("dve", lambda e: e.tensor_scalar_add(out=st[:, 15:16], in0=st[:, 13:14], scalar1=LN_EPS),
         reads=[st_tk], writes=[st_tk])
    S.op("act", lambda e: e.activation(out=st[:, 15:16], in_=st[:, 15:16], func=AF.Ln), reads=[st_tk],
         writes=[st_tk])
    S.op("act", lambda e: e.activation(out=st[:, 14:15], in_=st[:, 15:16], func=AF.Exp, scale=-0.5), reads=[st_tk],
         writes=[st_tk])
    S.op("dve", lambda e: e.tensor_scalar(out=dst, in0=src, scalar1=st[:, 12:13], scalar2=st[:, 14:15],
                                          op0=ALU.subtract, op1=ALU.mult), reads=[src_tk, st_tk], writes=[dst_tk])
    S.op("pool", lambda e: e.tensor_tensor(out=dst, in0=dst, in1=gB, op=ALU.mult), reads=[dst_tk, gb_tk],
         writes=[dst_tk])
    S.op("pool", lambda e: e.tensor_tensor(out=dst, in0=dst, in1=bB, op=ALU.add), reads=[dst_tk, gb_tk],
         writes=[dst_tk])


def build(cfg):
    P = Prog(cfg)
    nc, S, es = P.nc, P.S, P.es
    layers = cfg.get("layers", list(range(DEPTH)))
    subs = cfg.get("subs", (0, 1))

    x_d = P.din("x", [T, D])
    c_d = P.din("c", [1, D])
    ada_w = P.din("ada_w", [DEPTH, 2, D, 3 * D])
    ada_b = P.din("ada_b", [DEPTH, 2, 3 * D])
    ln_g = P.din("ln_g", [DEPTH, 2, D])
    ln_b = P.din("ln_b", [DEPTH, 2, D])
    moe_wr = P.din("moe_wr", [DEPTH, D, 36])
    moe_br = P.din("moe_br", [DEPTH, 36])
    moe_w1 = P.din("moe_w1", [DEPTH, NE, D, HID])
    moe_w3 = P.din("moe_w3", [DEPTH, NE, D, HID])
    moe_w2 = P.din("moe_w2", [DEPTH, NE, HID, D])
    ident_d = P.din("ident", [128, 128])
    P.din("rt_w_in", [1, D, 6 * D])
    P.din("rt_gn_g", [1, 2 * D])
    P.din("rt_gn_b", [1, 2 * D])
    P.din("rt_w_out", [1, 2 * D, D])
    for nm, shp in (("rw_mu", [2, 6, D]), ("rw_w_rkv", [2, 3, D, D]), ("rw_w0", [2, D]), ("rw_w1", [2, D, 64]),
                    ("rw_w2", [2, 64, D]), ("rw_a0", [2, D]), ("rw_a1", [2, D, 64]), ("rw_a2", [2, 64, D]),
                    ("rw_v0", [1, D]), ("rw_v1", [1, D, 32]), ("rw_v2", [1, 32, D]), ("rw_g1", [2, D, 128]),
                    ("rw_g2", [2, 128, D]), ("rw_kk", [2, D]), ("rw_ka", [2, D]), ("rw_rk", [2, D]),
                    ("rw_gn_g", [2, D]), ("rw_gn_b", [2, D]), ("rw_w_out", [2, D, D])):
        P.din(nm, shp)
    for nm, w in (("rw_tri_i", 128), ("rw_tri_e", 128), ("rw_mask4", 512), ("rw_maskl", 128), ("rw_blk", 128),
                  ("rw_sel2", 2)):
        P.din(nm, [128, w])
    P.din("ml_w_in", [1, D, 3 * D + 16])
    P.din("ml_b_gate", [1, 16])
    P.din("ml_conv_w", [1, 4, D])
    P.din("ml_conv_b", [1, D])
    P.din("ml_gn_g", [1, D])
    P.din("ml_gn_b", [1, D])
    P.din("ml_w_out", [1, D, D])
    P.din("tri_incl", [128, 128])
    P.din("ones", [128, 128])
    P.din("rt_cos", [128, T])
    P.din("rt_sin", [128, T])
    P.din("rt_tab", [4, 128, 16, 128])
    out_d = P.dout("out", [T, D])

    X = P.sb("X", [128, NT, D], F32)
    Xtk = [Tk("X%d" % n) for n in range(NT)]
    HT = P.sb("HT", [128, KC, T], BF16)
    HTtk = [Tk("HT%d" % n) for n in range(NT)]
    MOD = P.sb("MOD", [128, 3 * D], F32)
    MODtk = Tk("MOD")
    LNG = P.sb("LNG", [128, D], F32)
    LNB = P.sb("LNB", [128, D], F32)
    LNtk = Tk("LNGB")
    ident = P.sb("ident", [128, 128], F32)
    identtk = Tk("ident")
    csb = P.sb("csb", [128, KC, 128], BF16)
    csbtk = Tk("csb")
    ST = P.sb("ST", [128, 2, 16], F32)
    STtk = [Tk("ST0"), Tk("ST1")]

    S.dma("sp", ident[:], ident_d[:, :], writes=[identtk])
    for n in range(NT):
        S.dma("sp", X[:, n, :], x_d[n * 128:(n + 1) * 128, :], writes=[Xtk[n]])

    with ExitStack() as les:
        c_sb = P.sb("c_sb", [128, KC], F32, les)
        ctk = Tk("c")
        S.dma("sp", c_sb[:], c_d[0, :].rearrange("(k p) -> p k", p=128), writes=[ctk],
              allow_slow_non_contiguous=True)
        S.op("act", lambda e: e.activation(out=c_sb[:], in_=c_sb[:], func=AF.Silu), reads=[ctk], writes=[ctk])
        for k in range(KC):
            S.op("dve", lambda e, k=k: e.tensor_copy(out=csb[:, k, :], in_=c_sb[:, k:k + 1].to_broadcast([128, 128])),
                 reads=[ctk], writes=[csbtk])
        S.finish([csbtk])

    PSB = [P.ps("psb%d" % i, [128, 512], F32) for i in range(8)]
    PStk = [Tk("ps%d" % i) for i in range(8)]

    def ada_mod(i, k):
        with ExitStack() as les:
            wb = [P.sb("adaw%d" % j, [128, KC, 512], BF16, les) for j in range(2)]
            wtk = [Tk("adaw0"), Tk("adaw1")]
            bb = P.sb("adab", [128, 3 * D], F32, les)
            btk = Tk("adab")
            S.dma("sp", bb[:], ada_b[i, k:k + 1, :].to_broadcast([128, 3 * D]), writes=[btk])
            S.dma("sp", LNG[:], ln_g[i, k:k + 1, :].to_broadcast([128, D]), writes=[LNtk])
            S.dma("sp", LNB[:], ln_b[i, k:k + 1, :].to_broadcast([128, D]), writes=[LNtk])
            for nchunk in range(6):
                j = nchunk % 2
                S.dma("pool", wb[j][:], ada_w[i, k, :, nchunk * 512:(nchunk + 1) * 512].rearrange(
                    "(kc p) n -> p kc n", p=128), writes=[wtk[j]])
                pb = nchunk % 2
                for kc in range(KC):
                    S.op("pe", lambda e, kc=kc, j=j, pb=pb: e.matmul(PSB[pb][:], csb[:, kc, :], wb[j][:, kc, :],
                                                                   start=(kc == 0), stop=(kc == KC - 1)),
                         reads=[csbtk, wtk[j]], writes=[PStk[pb]], signal=(kc == KC - 1))
                sl = slice(nchunk * 512, (nchunk + 1) * 512)
                S.op("dve", lambda e, pb=pb, sl=sl: e.tensor_tensor(out=MOD[:, sl], in0=PSB[pb][:], in1=bb[:, sl],
                                                                   op=ALU.add),
                     reads=[PStk[pb], btk], writes=[MODtk])
            S.op("dve", lambda e: e.tensor_scalar_add(out=MOD[:, D:3 * D], in0=MOD[:, D:3 * D], scalar1=1.0),
                 reads=[MODtk], writes=[MODtk])
            P.dump("csb", csb[:], [csbtk], BF16)
            P.dump("wb", wb[1][:], [wtk[1]], BF16)
            P.dump("bb", bb[:], [btk])
            P.dump("MOD", MOD[:], [MODtk])
            S.finish([MODtk])

    def modulate_transpose():
        with ExitStack() as les:
            hb = [P.sb("hmod%d" % j, [128, D], F32, les) for j in range(2)]
            htk = [Tk("hmod0"), Tk("hmod1")]
            for n in range(NT):
                j = n % 2
                S.op("dve", lambda e, n=n, j=j: e.tensor_tensor(out=hb[j][:], in0=X[:, n, :], in1=MOD[:, D:2 * D],
                                                               op=ALU.mult),
                     reads=[Xtk[n], MODtk], writes=[htk[j]])
                S.op("pool", lambda e, j=j: e.tensor_tensor(out=hb[j][:], in0=hb[j][:], in1=MOD[:, 0:D], op=ALU.add),
                     reads=[htk[j], MODtk], writes=[htk[j]])
                for half in range(2):
                    pb = (2 * n + half) % 4
                    for q in range(4):
                        kc = half * 4 + q
                        S.op("pe", lambda e, kc=kc, q=q, pb=pb, j=j: e.transpose(
                            PSB[pb][:, q * 128:(q + 1) * 128], hb[j][:, kc * 128:(kc + 1) * 128], ident[:]),
                             reads=[htk[j], identtk], writes=[PStk[pb]], signal=(q == 3))
                    S.op("act", lambda e, half=half, pb=pb, n=n: e.copy(
                        out=HT[:, half * 4:(half + 1) * 4, n * 128:(n + 1) * 128],
                        in_=PSB[pb][:].rearrange("p (q t) -> p q t", q=4)),
                         reads=[PStk[pb]], writes=[HTtk[n]])
            P.dump("HT", HT[:], HTtk, BF16)
            S.finish(HTtk)

    def deepnorm_from_X():
        for n in range(NT):
            j = n % 2
            _layer_norm_tile(P, S, X[:, n, :], Xtk[n], X[:, n, :], Xtk[n], LNG[:], LNB[:], LNtk, ST[:, j, :], STtk[j])

    def moe(i):
        with ExitStack() as les:
            wr = P.sb("wr", [128, KC, 36], BF16, les)
            wrtk = Tk("wr")
            brb = P.sb("brb", [128, 36], F32, les)
            LG = P.sb("LG", [128, NT, 36], F32, les)
            LGtk = Tk("LG")
            G = P.sb("G", [128, NT, NE], F32, les)
            Gtk = Tk("G")
            t1 = P.sb("t1", [128, NT, NE], F32, les)
            t2 = P.sb("t2", [128, NT, NE], F32, les)
            sm = P.sb("sm", [128, 8, NT], F32, les)
            gtk = Tk("gating")
            W1 = [P.sb("W1_%d" % j, [128, KC, HID], BF16, les) for j in range(2)]
            W3 = [P.sb("W3_%d" % j, [128, KC, HID], BF16, les) for j in range(2)]
            W2 = [P.sb("W2_%d" % j, [128, 4, D], BF16, les) for j in range(2)]
            W1tk = [Tk("W1a"), Tk("W1b")]
            W3tk = [Tk("W3a"), Tk("W3b")]
            W2tk = [Tk("W2a"), Tk("W2b")]
            A = [P.sb("A_%d" % j, [128, 4, 512], BF16, les) for j in range(2)]
            Atk = [Tk("Aa"), Tk("Ab")]
            SL = [P.sb("SL_%d" % j, [128, 512], BF16, les) for j in range(2)]
            SLtk = [Tk("SLa"), Tk("SLb")]

            def load_expert(e):
                j = e % 2
                S.dma("pool", W1[j][:], moe_w1[i, e].rearrange("(kc p) n -> p kc n", p=128), writes=[W1tk[j]])
                S.dma("pool", W3[j][:], moe_w3[i, e].rearrange("(kc p) n -> p kc n", p=128), writes=[W3tk[j]])
                S.dma("pool", W2[j][:], moe_w2[i, e].rearrange("(kc p) n -> p kc n", p=128), writes=[W2tk[j]])

            S.dma("pool", wr[:], moe_wr[i].rearrange("(kc p) n -> p kc n", p=128), writes=[wrtk])
            S.dma("sp", brb[:], moe_br[i:i + 1, :].to_broadcast([128, 36]), writes=[wrtk])
            load_expert(0)

            for n in range(NT):
                pb = n % 2
                for kc in range(KC):
                    S.op("pe", lambda e, kc=kc, n=n, pb=pb: e.matmul(PSB[pb][:, 0:36], HT[:, kc, n * 128:(n + 1) * 128],
                                                                   wr[:, kc, :], start=(kc == 0), stop=(kc == KC - 1)),
                         reads=[HTtk[n], wrtk], writes=[PStk[pb]], signal=(kc == KC - 1))
                S.op("dve", lambda e, n=n, pb=pb: e.tensor_tensor(out=LG[:, n, :], in0=PSB[pb][:, 0:36], in1=brb[:],
                                                                 op=ALU.add),
                     reads=[PStk[pb], wrtk], writes=[LGtk])
            P.dump("LG", LG[:], [LGtk])
            gl = LG[:, :, 0:4]
            el = LG[:, :, 4:36]
            gmax, gsum, m1, m2, s1, s2 = (sm[:, q, :] for q in range(6))
            gm4 = t2[:, :, 0:4]
            pen = t2[:, :, 4:8]
            ex4 = t2[:, :, 8:12]

            def dv(fn, reads=(LGtk,), writes=(gtk,)):
                S.op("dve", fn, reads=list(reads) + [gtk], writes=list(writes))

            bc4 = lambda a: a.unsqueeze(2).to_broadcast([128, NT, 4])
            bc32 = lambda a: a.unsqueeze(2).to_broadcast([128, NT, NE])
            dv(lambda e: e.tensor_reduce(out=gmax, in_=gl, axis=AX.X, op=ALU.max))
            dv(lambda e: e.tensor_tensor(out=gm4, in0=gl, in1=bc4(gmax), op=ALU.is_ge))
            dv(lambda e: e.tensor_tensor(out=ex4, in0=gl, in1=bc4(gmax), op=ALU.subtract))
            S.op("act", lambda e: e.activation(out=ex4, in_=ex4, func=AF.Exp), reads=[gtk], writes=[gtk])
            dv(lambda e: e.tensor_reduce(out=gsum, in_=ex4, axis=AX.X, op=ALU.add))
            dv(lambda e: e.reciprocal(out=gsum, in_=gsum))
            dv(lambda e: e.tensor_scalar(out=pen, in0=gm4, scalar1=BIG, scalar2=-BIG, op0=ALU.mult, op1=ALU.add))
            dv(lambda e: e.tensor_tensor(out=t1[:].rearrange("p t (g e) -> p t g e", g=4),
                                         in0=el.rearrange("p t (g e) -> p t g e", g=4),
                                         in1=pen.unsqueeze(3).to_broadcast([128, NT, 4, 8]), op=ALU.add))
            dv(lambda e: e.tensor_reduce(out=m1, in_=t1[:], axis=AX.X, op=ALU.max))
            dv(lambda e: e.tensor_tensor(out=G[:], in0=t1[:], in1=bc32(m1), op=ALU.is_ge), writes=(gtk, Gtk))
            dv(lambda e: e.scalar_tensor_tensor(out=t1[:], in0=G[:], scalar=-BIG, in1=t1[:], op0=ALU.mult, op1=ALU.add),
               reads=(LGtk, Gtk))
            dv(lambda e: e.tensor_reduce(out=m2, in_=t1[:], axis=AX.X, op=ALU.max))
            dv(lambda e: e.tensor_tensor(out=t2[:], in0=t1[:], in1=bc32(m2), op=ALU.is_ge))
            dv(lambda e: e.tensor_tensor(out=s1, in0=m1, in1=m2, op=ALU.subtract))
            S.op("act", lambda e: e.activation(out=s1, in_=s1, func=AF.Sigmoid), reads=[gtk], writes=[gtk])
            dv(lambda e: e.tensor_scalar(out=s2, in0=s1, scalar1=-1.0, scalar2=1.0, op0=ALU.mult, op1=ALU.add))
            dv(lambda e: e.tensor_tensor(out=s1, in0=s1, in1=gsum, op=ALU.mult))
            dv(lambda e: e.tensor_tensor(out=s2, in0=s2, in1=gsum, op=ALU.mult))
            dv(lambda e: e.tensor_tensor(out=G[:], in0=G[:], in1=bc32(s1), op=ALU.mult), reads=(LGtk, Gtk),
               writes=(gtk, Gtk))
            dv(lambda e: e.tensor_tensor(out=t2[:], in0=t2[:], in1=bc32(s2), op=ALU.mult))
            dv(lambda e: e.tensor_tensor(out=G[:], in0=G[:], in1=t2[:], op=ALU.add), reads=(LGtk, Gtk),
               writes=(gtk, Gtk))
            if cfg.get("dbg_gate"):
                S.dma("sp", P.dram["dbg_gate"].rearrange("(n p) e -> p n e", p=128), G[:], reads=[Gtk],
                      writes=[Tk("dbg_gate")])

            for n in range(NT):
                S.op("act", lambda e, n=n: e.mul(out=X[:, n, :], in_=X[:, n, :], mul=DN_ALPHA), reads=[Xtk[n]],
                     writes=[Xtk[n]])

            NB = T // 512
            items = [(e, b) for e in range(NE) for b in range(NB)]

            def stage1(idx):
                e, b = items[idx]
                j = e % 2
                a = idx % 2
                if b == 1 and e + 1 < NE:
                    load_expert(e + 1)
                if b == 0:
                    S.op("dve", lambda en, j=j: en.tensor_tensor(
                        out=W2[j][:], in0=W2[j][:], in1=MOD[:, 2 * D:3 * D].unsqueeze(1).to_broadcast([128, 4, D]),
                        op=ALU.mult), reads=[W2tk[j], MODtk], writes=[W2tk[j]])
                tsl = slice(b * 512, (b + 1) * 512)
                htks = HTtk[b * 4:(b + 1) * 4]
                for hc in range(4):
                    p1 = (hc % 2) * 2
                    p3 = p1 + 1
                    for kc in range(KC):
                        S.op("pe", lambda en, kc=kc, hc=hc, p1=p1, j=j: en.matmul(
                            PSB[p1][:], W1[j][:, kc, hc * 128:(hc + 1) * 128], HT[:, kc, tsl],
                            start=(kc == 0), stop=(kc == KC - 1)),
                             reads=htks + [W1tk[j]], writes=[PStk[p1]], signal=(kc == KC - 1))
                    for kc in range(KC):
                        S.op("pe", lambda en, kc=kc, hc=hc, p3=p3, j=j: en.matmul(
                            PSB[p3][:], W3[j][:, kc, hc * 128:(hc + 1) * 128], HT[:, kc, tsl],
                            start=(kc == 0), stop=(kc == KC - 1)),
                             reads=htks + [W3tk[j]], writes=[PStk[p3]], signal=(kc == KC - 1))
                    sj = hc % 2
                    S.op("act", lambda en, p1=p1, sj=sj: en.activation(out=SL[sj][:], in_=PSB[p1][:], func=AF.Silu),
                         reads=[PStk[p1]], writes=[SLtk[sj]])
                    S.op("dve", lambda en, p3=p3, sj=sj, hc=hc, a=a: en.tensor_tensor(
                        out=A[a][:, hc, :], in0=PSB[p3][:], in1=SL[sj][:], op=ALU.mult),
                         reads=[PStk[p3], SLtk[sj]], writes=[Atk[a]])

            def stage2(idx):
                e, b = items[idx]
                j = e % 2
                a = idx % 2
                for tt in range(4):
                    n = b * 4 + tt
                    pb = 4 + (tt % 2) * 2
                    for nh in range(2):
                        for hc in range(4):
                            S.op("pe", lambda en, hc=hc, nh=nh, tt=tt, pb=pb: en.matmul(
                                PSB[pb + nh][:], A[a][:, hc, tt * 128:(tt + 1) * 128],
                                W2[j][:, hc, nh * 512:(nh + 1) * 512], start=(hc == 0), stop=(hc == 3)),
                                 reads=[Atk[a], W2tk[j]], writes=[PStk[pb + nh]], signal=(hc == 3))
                    for nh in range(2):
                        S.op("dve", lambda en, nh=nh, pb=pb, n=n, e=e: en.scalar_tensor_tensor(
                            out=X[:, n, nh * 512:(nh + 1) * 512], in0=PSB[pb + nh][:], scalar=G[:, n, e:e + 1],
                            in1=X[:, n, nh * 512:(nh + 1) * 512], op0=ALU.mult, op1=ALU.add),
                             reads=[PStk[pb + nh], Gtk, Xtk[n]], writes=[Xtk[n]])

            ne_run = cfg.get("moe_experts", NE)
            items = [(e, b) for e in range(ne_run) for b in range(NB)]
            stage1(0)
            for idx in range(len(items)):
                if idx + 1 < len(items):
                    stage1(idx + 1)
                stage2(idx)
            S.finish(Xtk + W1tk + W2tk + W3tk + Atk + SLtk + [Gtk, gtk, LGtk, wrtk])

    xs_d = P.dscratch("xs", [T, D])
    Zv = X[:].bitcast(BF16)
    identb = P.sb("identb", [128, 128], BF16)
    S.op("dve", lambda e: e.tensor_copy(out=identb[:], in_=ident[:]), reads=[identtk], writes=[identtk])

    def spill_X():
        tk = Tk("xs")
        for n in range(NT):
            S.dma("sp", xs_d[n * 128:(n + 1) * 128, :], X[:, n, :], reads=[Xtk[n]], writes=[tk])
        S.barrier()

    def mixer_epilogue(w_out_ap, nz):
        with ExitStack() as les:
            WO = P.sb("WO", [128, nz, D], BF16, les)
            WOtk = Tk("WO")
            for c0 in range(0, nz, 8):
                S.dma("pool", WO[:, c0:c0 + 8, :], w_out_ap[c0 * 128:(c0 + 8) * 128, :].rearrange(
                    "(c p) n -> p c n", p=128), writes=[WOtk])
            ZT = [P.sb("ZT%d" % j, [128, nz, 128], BF16, les) for j in range(2)]
            ZTtk = [Tk("ZT0"), Tk("ZT1")]
            XO = [P.sb("XO%d" % j, [128, D], F32, les) for j in range(2)]
            XOtk = [Tk("XO0"), Tk("XO1")]
            for n in range(NT):
                j = n % 2
                S.dma("sp", XO[j][:], xs_d[n * 128:(n + 1) * 128, :], writes=[XOtk[j]])
                for c0 in range(0, nz, 8):
                    pb = (c0 // 8) % 2
                    pv = PSB[pb][:].bitcast(BF16)
                    for c in range(c0, c0 + 8):
                        S.op("pe", lambda e, c=c, c0=c0, pv=pv, n=n: e.transpose(
                            pv[:, (c - c0) * 128:(c - c0 + 1) * 128], Zv[:, n, c * 128:(c + 1) * 128], identb[:]),
                             reads=[Xtk[n], identtk], writes=[PStk[pb]], signal=(c == c0 + 7))
                    S.op("act", lambda e, c0=c0, pv=pv, j=j: e.copy(
                        out=ZT[j][:, c0:c0 + 8, :], in_=pv.rearrange("p (c t) -> p c t", c=8)),
                         reads=[PStk[pb]], writes=[ZTtk[j]])
                for nh in range(2):
                    pb = 2 + (n % 2) * 2 + nh
                    for c in range(nz):
                        S.op("pe", lambda e, c=c, nh=nh, pb=pb, j=j: e.matmul(
                            PSB[pb][:], ZT[j][:, c, :], WO[:, c, nh * 512:(nh + 1) * 512],
                            start=(c == 0), stop=(c == nz - 1)),
                             reads=[ZTtk[j], WOtk], writes=[PStk[pb]], signal=(c == nz - 1))
                    S.op("dve", lambda e, nh=nh, pb=pb, n=n: e.tensor_tensor(
                        out=X[:, n, nh * 512:(nh + 1) * 512], in0=PSB[pb][:],
                        in1=MOD[:, 2 * D + nh * 512:2 * D + (nh + 1) * 512], op=ALU.mult),
                         reads=[PStk[pb], MODtk, Xtk[n]], writes=[Xtk[n]])
                S.op("dve", lambda e, n=n, j=j: e.scalar_tensor_tensor(
                    out=X[:, n, :], in0=XO[j][:], scalar=DN_ALPHA, in1=X[:, n, :], op0=ALU.mult, op1=ALU.add),
                     reads=[XOtk[j], Xtk[n]], writes=[Xtk[n]])
                _layer_norm_tile(P, S, X[:, n, :], Xtk[n], X[:, n, :], Xtk[n], LNG[:], LNB[:], LNtk,
                                 ST[:, j, :], STtk[j])
            S.barrier()

    def head_norm_tile(src_ps, src_tk, width, eps, gB, bB, gbtk, st, sttk, tmp, tmptk):
        S.op("dve", lambda e: e.bn_stats(out=st[:, 0:6], in_=src_ps), reads=[src_tk], writes=[sttk])
        S.op("dve", lambda e: e.bn_aggr(out=st[:, 12:14], in_=st[:, 0:6]), reads=[sttk], writes=[sttk])
        S.op("dve", lambda e: e.tensor_scalar_add(out=st[:, 15:16], in0=st[:, 13:14], scalar1=eps),
             reads=[sttk], writes=[sttk])
        S.op("act", lambda e: e.activation(out=st[:, 15:16], in_=st[:, 15:16], func=AF.Ln), reads=[sttk],
             writes=[sttk])
        S.op("act", lambda e: e.activation(out=st[:, 14:15], in_=st[:, 15:16], func=AF.Exp, scale=-0.5),
             reads=[sttk], writes=[sttk])
        S.op("dve", lambda e: e.tensor_scalar(out=tmp, in0=src_ps, scalar1=st[:, 12:13], scalar2=st[:, 14:15],
                                              op0=ALU.subtract, op1=ALU.mult), reads=[src_tk, sttk],
             writes=[tmptk])
        S.op("pool", lambda e: e.tensor_tensor(out=tmp, in0=tmp, in1=gB, op=ALU.mult), reads=[tmptk, gbtk],
             writes=[tmptk])
        S.op("pool", lambda e: e.tensor_tensor(out=tmp, in0=tmp, in1=bB, op=ALU.add), reads=[tmptk, gbtk],
             writes=[tmptk])

    def retention(j):
        RH, DKh, DVh = 4, 256, 512
        w_in = P.dram["rt_w_in"]
        with ExitStack() as les:
            QT = P.sb("QT", [128, 2, T], BF16, les)
            KT = P.sb("KT", [128, 2, T], BF16, les)
            VH = P.sb("VH", [128, NT, DVh], BF16, les)
            QTtk, KTtk, VHtk = Tk("QT"), Tk("KT"), Tk("VH")
            for h in range(RH):
                with ExitStack() as aes:
                    WQ = P.sb("WQ", [128, KC, DKh], BF16, aes)
                    WK = P.sb("WK", [128, KC, DKh], BF16, aes)
                    WV = P.sb("WV", [128, KC, DVh], BF16, aes)
                    COS = P.sb("COS", [128, T], F32, aes)
                    SIN = P.sb("SIN", [128, T], F32, aes)
                    Wtk, CStk = Tk("Wqkv"), Tk("cs")
                    S.dma("pool", WQ[:], w_in[j, :, h * DKh:(h + 1) * DKh].rearrange("(kc p) n -> p kc n", p=128),
                          writes=[Wtk])
                    S.dma("pool", WK[:], w_in[j, :, D + h * DKh:D + (h + 1) * DKh].rearrange(
                        "(kc p) n -> p kc n", p=128), writes=[Wtk])
                    S.dma("pool", WV[:], w_in[j, :, 2 * D + h * DVh:2 * D + (h + 1) * DVh].rearrange(
                        "(kc p) n -> p kc n", p=128), writes=[Wtk])
                    S.dma("sp", COS[:], P.dram["rt_cos"][:, :], writes=[CStk])
                    S.dma("sp", SIN[:], P.dram["rt_sin"][:, :], writes=[CStk])
                    tA = [P.sb("rtA%d" % q, [128, 512], F32, aes) for q in range(4)]
                    tAtk = [Tk("rtA%d" % q) for q in range(4)]
                    for (Wt, OT, OTtk) in ((WQ, QT, QTtk), (WK, KT, KTtk)):
                        for tb in range(4):
                            tsl = slice(tb * 512, (tb + 1) * 512)
                            htks = HTtk[tb * 4:(tb + 1) * 4]
                            pbs = (4 + (tb % 2) * 2, 5 + (tb % 2) * 2)
                            for dc in range(2):
                                for kc in range(KC):
                                    S.op("pe", lambda e, kc=kc, dc=dc, Wt=Wt: e.matmul(
                                        PSB[pbs[dc]][:], Wt[:, kc, dc * 128:(dc + 1) * 128], HT[:, kc, tsl],
                                        start=(kc == 0), stop=(kc == KC - 1)),
                                         reads=htks + [Wtk], writes=[PStk[pbs[dc]]], signal=(kc == KC - 1))
                            x1, x2 = PSB[pbs[0]][:], PSB[pbs[1]][:]
                            k1, k2 = PStk[pbs[0]], PStk[pbs[1]]
                            S.op("dve", lambda e: e.tensor_tensor(out=tA[0][:], in0=x1, in1=COS[:, tsl], op=ALU.mult),
                                 reads=[k1, CStk], writes=[tAtk[0]])
                            S.op("dve", lambda e: e.tensor_tensor(out=tA[1][:], in0=x2, in1=SIN[:, tsl], op=ALU.mult),
                                 reads=[k2, CStk], writes=[tAtk[1]])
                            S.op("dve", lambda e: e.tensor_tensor(out=tA[2][:], in0=x1, in1=SIN[:, tsl], op=ALU.mult),
                                 reads=[k1, CStk], writes=[tAtk[2]])
                            S.op("dve", lambda e: e.tensor_tensor(out=tA[3][:], in0=x2, in1=COS[:, tsl], op=ALU.mult),
                                 reads=[k2, CStk], writes=[tAtk[3]])
                            S.op("pool", lambda e, OT=OT: e.tensor_tensor(out=OT[:, 0, tsl], in0=tA[0][:], in1=tA[1][:],
                                                                         op=ALU.subtract),
                                 reads=[tAtk[0], tAtk[1]], writes=[OTtk])
                            S.op("pool", lambda e, OT=OT: e.tensor_tensor(out=OT[:, 1, tsl], in0=tA[2][:], in1=tA[3][:],
                                                                         op=ALU.add),
                                 reads=[tAtk[2], tAtk[3]], writes=[OTtk])
                    for n in range(NT):
                        pb = 4 + n % 4
                        for kc in range(KC):
                            S.op("pe", lambda e, kc=kc, n=n, pb=pb: e.matmul(
                                PSB[pb][:], HT[:, kc, n * 128:(n + 1) * 128], WV[:, kc, :],
                                start=(kc == 0), stop=(kc == KC - 1)),
                                 reads=[HTtk[n], Wtk], writes=[PStk[pb]], signal=(kc == KC - 1))
                        S.op("act", lambda e, n=n, pb=pb: e.copy(out=VH[:, n, :], in_=PSB[pb][:]),
                             reads=[PStk[pb]], writes=[VHtk])
                    S.barrier()
                with ExitStack() as bes:
                    TAB = P.sb("TAB", [128, 16, 128], F32, bes)
                    GNG = P.sb("GNG", [128, DVh], F32, bes)
                    GNB = P.sb("GNB", [128, DVh], F32, bes)
                    WG = P.sb("WG", [128, KC, DVh], BF16, bes)
                    TABtk, GNtk, WGtk = Tk("TAB"), Tk("GN"), Tk("WG")
                    S.dma("sp", TAB[:], P.dram["rt_tab"][h], writes=[TABtk])
                    S.dma("sp", GNG[:], P.dram["rt_gn_g"][j:j + 1, h * DVh:(h + 1) * DVh].to_broadcast([128, DVh]),
                          writes=[GNtk])
                    S.dma("sp", GNB[:], P.dram["rt_gn_b"][j:j + 1, h * DVh:(h + 1) * DVh].to_broadcast([128, DVh]),
                          writes=[GNtk])
                    S.dma("pool", WG[:], w_in[j, :, 4 * D + h * DVh:4 * D + (h + 1) * DVh].rearrange(
                        "(kc p) n -> p kc n", p=128), writes=[WGtk])
                    PM = [P.sb("PM%d" % q, [128, 4, 128], BF16, bes) for q in range(2)]
                    PMtk = [Tk("PM0"), Tk("PM1")]
                    SG = [P.sb("SG%d" % q, [128, DVh], F32, bes) for q in range(2)]
                    SGtk = [Tk("SG0"), Tk("SG1")]
                    YN = [P.sb("YN%d" % q, [128, DVh], F32, bes) for q in range(2)]
                    YNtk = [Tk("YN0"), Tk("YN1")]
                    gi = 0
                    for sb in range(NT):
                        ya = 2 + sb % 2
                        for jg in range(0, sb + 1, 4):
                            nj = min(4, sb + 1 - jg)
                            sp = gi % 2
                            gi += 1
                            for q in range(nj):
                                jb = jg + q
                                for dc in range(2):
                                    S.op("pe", lambda e, dc=dc, q=q, jb=jb, sp=sp, sb=sb: e.matmul(
                                        PSB[sp][:, q * 128:(q + 1) * 128], KT[:, dc, jb * 128:(jb + 1) * 128],
                                        QT[:, dc, sb * 128:(sb + 1) * 128], start=(dc == 0), stop=(dc == 1)),
                                         reads=[KTtk, QTtk], writes=[PStk[sp]], signal=(dc == 1 and q == nj - 1))
                            r0 = 15 - sb + jg
                            S.op("dve", lambda e, sp=sp, nj=nj, r0=r0: e.tensor_tensor(
                                out=PM[sp][:, 0:nj, :], in0=PSB[sp][:, 0:nj * 128].rearrange("p (q t) -> p q t", q=nj),
                                in1=TAB[:, r0:r0 + nj, :], op=ALU.mult),
                                 reads=[PStk[sp], TABtk], writes=[PMtk[sp]])
                            for q in range(nj):
                                jb = jg + q
                                S.op("pe", lambda e, q=q, jb=jb, sp=sp, ya=ya, sb=sb: e.matmul(
                                    PSB[ya][:], PM[sp][:, q, :], VH[:, jb, :], start=(jb == 0), stop=(jb == sb)),
                                     reads=[PMtk[sp], VHtk], writes=[PStk[ya]], signal=(jb == sb))
                        gp = 4 + sb % 2
                        for kc in range(KC):
                            S.op("pe", lambda e, kc=kc, sb=sb, gp=gp: e.matmul(
                                PSB[gp][:], HT[:, kc, sb * 128:(sb + 1) * 128], WG[:, kc, :],
                                start=(kc == 0), stop=(kc == KC - 1)),
                                 reads=[HTtk[sb], WGtk], writes=[PStk[gp]], signal=(kc == KC - 1))
                        q2 = sb % 2
                        S.op("act", lambda e, gp=gp, q2=q2: e.activation(out=SG[q2][:], in_=PSB[gp][:], func=AF.Silu),
                             reads=[PStk[gp]], writes=[SGtk[q2]])
                        head_norm_tile(PSB[ya][:], PStk[ya], DVh, 1e-6, GNG[:], GNB[:], GNtk, ST[:, q2, :], STtk[q2],
                                       YN[q2][:], YNtk[q2])
                        S.op("pool", lambda e, q2=q2, sb=sb, h=h: e.tensor_tensor(
                            out=Zv[:, sb, h * DVh:(h + 1) * DVh], in0=YN[q2][:], in1=SG[q2][:], op=ALU.mult),
                             reads=[YNtk[q2], SGtk[q2]], writes=[Xtk[sb]])
                    S.barrier()
        mixer_epilogue(P.dram["rt_w_out"][j], 16)

    fs_d = P.dscratch("fs", [8, T])

    def mlstm(j):
        MH, DKh, DVh = 8, 64, 128
        w_in = P.dram["ml_w_in"]
        with ExitStack() as les:
            QK = P.sb("QK", [128, KC, T], BF16, les)
            QKtk = Tk("QK")
            BJ = P.sb("BJ", [128, NT, MH], F32, les)
            BJtk = Tk("BJ")
            TRI = P.sb("TRI", [128, 128], F32, les)
            ONES = P.sb("ONES", [128, 128], F32, les)
            Ctk = Tk("mlconst")
            S.dma("sp", TRI[:], P.dram["tri_incl"][:, :], writes=[Ctk])
            S.dma("sp", ONES[:], P.dram["ones"][:, :], writes=[Ctk])
            with ExitStack() as aes:
                WQK = P.sb("WQK", [128, KC, D], BF16, aes)
                WGT = P.sb("WGT", [128, KC, 16], BF16, aes)
                CW = P.sb("CW", [128, 4, KC], F32, aes)
                CB = P.sb("CB", [128, KC], F32, aes)
                BG = P.sb("BG", [128, 16], F32, aes)
                Wtk = Tk("mlW")
                for c0 in range(0, KC, 4):
                    S.dma("pool", WQK[:, :, c0 * 128:(c0 + 4) * 128], w_in[j, :, c0 * 128:(c0 + 4) * 128].rearrange(
                        "(kc p) n -> p kc n", p=128), writes=[Wtk])
                S.dma("pool", WGT[:], w_in[j, :, 3 * D:3 * D + 16].rearrange("(kc p) n -> p kc n", p=128),
                      writes=[Wtk])
                for tap in range(4):
                    S.dma("sp", CW[:, tap, :], P.dram["ml_conv_w"][j, tap].rearrange("(c p) -> p c", p=128),
                          writes=[Wtk], allow_slow_non_contiguous=True)
                S.dma("sp", CB[:], P.dram["ml_conv_b"][j].rearrange("(c p) -> p c", p=128), writes=[Wtk],
                      allow_slow_non_contiguous=True)
                S.dma("sp", BG[:], P.dram["ml_b_gate"][j:j + 1, :].to_broadcast([128, 16]), writes=[Wtk])
                RAW = [P.sb("RAW%d" % q, [128, T + 3], F32, aes) for q in range(2)]
                RAWtk = [Tk("RAW0"), Tk("RAW1")]
                ACC = P.sb("ACC", [128, T], F32, aes)
                ACCtk = Tk("ACC")
                for q in range(2):
                    S.op("pool", lambda e, q=q: e.memset(RAW[q][:, 0:3], 0.0), writes=[RAWtk[q]])
                for c in range(KC):
                    rq = c % 2
                    for tb in range(4):
                        pb = 4 + tb
                        tsl = slice(tb * 512, (tb + 1) * 512)
                        for kc in range(KC):
                            S.op("pe", lambda e, kc=kc, c=c, pb=pb, tsl=tsl: e.matmul(
                                PSB[pb][:], WQK[:, kc, c * 128:(c + 1) * 128], HT[:, kc, tsl],
                                start=(kc == 0), stop=(kc == KC - 1)),
                                 reads=HTtk[tb * 4:(tb + 1) * 4] + [Wtk], writes=[PStk[pb]], signal=(kc == KC - 1))
                        S.op("act", lambda e, pb=pb, tb=tb, rq=rq: e.copy(
                            out=RAW[rq][:, 3 + tb * 512:3 + (tb + 1) * 512], in_=PSB[pb][:]),
                             reads=[PStk[pb]], writes=[RAWtk[rq]])
                    S.op("dve", lambda e, c=c, rq=rq: e.tensor_scalar(
                        out=ACC[:], in0=RAW[rq][:, 0:T], scalar1=CW[:, 0, c:c + 1], scalar2=CB[:, c:c + 1],
                        op0=ALU.mult, op1=ALU.add), reads=[RAWtk[rq], Wtk], writes=[ACCtk])
                    for tap in range(1, 4):
                        S.op("dve", lambda e, c=c, rq=rq, tap=tap: e.scalar_tensor_tensor(
                            out=ACC[:], in0=RAW[rq][:, tap:tap + T], scalar=CW[:, tap, c:c + 1], in1=ACC[:],
                            op0=ALU.mult, op1=ALU.add), reads=[RAWtk[rq], Wtk, ACCtk], writes=[ACCtk])
                    S.op("act", lambda e, c=c: e.activation(out=QK[:, c, :], in_=ACC[:], func=AF.Silu),
                         reads=[ACCtk], writes=[QKtk])
                GT = P.sb("GT", [128, NT, 16], F32, aes)
                LF = P.sb("LF", [128, NT, MH], F32, aes)
                FF = P.sb("FF", [128, NT, MH], F32, aes)
                GTtk, LFtk, FFtk = Tk("GT"), Tk("LF"), Tk("FF")
                for n in range(NT):
                    pb = n % 2
                    for kc in range(KC):
                        S.op("pe", lambda e, kc=kc, n=n, pb=pb: e.matmul(
                            PSB[pb][:, 0:16], HT[:, kc, n * 128:(n + 1) * 128], WGT[:, kc, :],
                            start=(kc == 0), stop=(kc == KC - 1)),
                             reads=[HTtk[n], Wtk], writes=[PStk[pb]], signal=(kc == KC - 1))
                    S.op("dve", lambda e, n=n, pb=pb: e.tensor_tensor(out=GT[:, n, :], in0=PSB[pb][:, 0:16], in1=BG[:],
                                                                     op=ALU.add),
                         reads=[PStk[pb], Wtk], writes=[GTtk])
                S.op("act", lambda e: e.activation(out=LF[:], in_=GT[:, :, 8:16], func=AF.Exp, scale=-1.0),
                     reads=[GTtk], writes=[LFtk])
                S.op("act", lambda e: e.activation(out=LF[:], in_=LF[:], func=AF.Ln, bias=1.0),
                     reads=[LFtk], writes=[LFtk])
                S.op("dve", lambda e: e.tensor_scalar_mul(out=LF[:], in0=LF[:], scalar1=-1.0), reads=[LFtk],
                     writes=[LFtk])
                for n in range(NT):
                    pb = 2 + n % 2
                    for m in range(n):
                        S.op("pe", lambda e, m=m, pb=pb: e.matmul(PSB[pb][:, 0:MH], ONES[:], LF[:, m, :],
                                                                 start=(m == 0), stop=False),
                             reads=[LFtk, Ctk], writes=[PStk[pb]], signal=False)
                    S.op("pe", lambda e, n=n, pb=pb: e.matmul(PSB[pb][:, 0:MH], TRI[:], LF[:, n, :],
                                                             start=(n == 0), stop=True),
                         reads=[LFtk, Ctk], writes=[PStk[pb]])
                    S.op("dve", lambda e, n=n, pb=pb: e.tensor_copy(out=FF[:, n, :], in_=PSB[pb][:, 0:MH]),
                         reads=[PStk[pb]], writes=[FFtk])
                S.op("dve", lambda e: e.tensor_tensor(out=BJ[:], in0=GT[:, :, 0:8], in1=FF[:], op=ALU.subtract),
                     reads=[GTtk, FFtk], writes=[BJtk])
                S.op("dve", lambda e: e.tensor_scalar_add(out=BJ[:], in0=BJ[:], scalar1=math.log(DKh ** -0.5)),
                     reads=[BJtk], writes=[BJtk])
                fstk = Tk("fs")
                for hh in range(MH):
                    S.dma("sp", fs_d[hh].rearrange("(n p) -> p n", p=128), FF[:, :, hh], reads=[FFtk], writes=[fstk],
                          allow_slow_non_contiguous=True)
                P.dump("QK", QK[:], [QKtk], BF16)
                P.dump("FF", FF[:], [FFtk])
                S.barrier()
            with ExitStack() as bes:
                GNG = P.sb("GNG", [128, D], F32, bes)
                GNB = P.sb("GNB", [128, D], F32, bes)
                GNtk = Tk("GN")
                S.dma("sp", GNG[:], P.dram["ml_gn_g"][j:j + 1, :].to_broadcast([128, D]), writes=[GNtk])
                S.dma("sp", GNB[:], P.dram["ml_gn_b"][j:j + 1, :].to_broadcast([128, D]), writes=[GNtk])
                FB = [P.sb("FB%d" % q, [128, T], F32, bes) for q in range(2)]
                FBtk = [Tk("FB0"), Tk("FB1")]
                WV = [P.sb("WVh%d" % q, [128, KC, DVh], BF16, bes) for q in range(2)]
                WO = [P.sb("WOh%d" % q, [128, KC, DVh], BF16, bes) for q in range(2)]
                WVtk = [Tk("WV0"), Tk("WV1")]
                V1 = [P.sb("V1%d" % q, [128, NT, DVh + 1], BF16, bes) for q in range(2)]
                V1tk = [Tk("V10"), Tk("V11")]
                EW = [P.sb("EW%d" % q, [128, 4, 128], F32, bes) for q in range(2)]
                EWtk = [Tk("EW0"), Tk("EW1")]
                PM = [P.sb("PMm%d" % q, [128, 4, 128], BF16, bes) for q in range(2)]
                PMtk = [Tk("PM0"), Tk("PM1")]
                SO = [P.sb("SO%d" % q, [128, DVh], F32, bes) for q in range(2)]
                SOtk = [Tk("SO0"), Tk("SO1")]
                HB = [P.sb("HB%d" % q, [128, DVh], F32, bes) for q in range(2)]
                HBtk = [Tk("HB0"), Tk("HB1")]
                DN = [P.sb("DN%d" % q, [128, 2], F32, bes) for q in range(2)]
                DNtk = [Tk("DN0"), Tk("DN1")]
                for q in range(2):
                    S.op("pool", lambda e, q=q: e.memset(V1[q][:, :, DVh:DVh + 1], 1.0), writes=[V1tk[q]])
                gi = 0
                for h in range(MH):
                    hq = h % 2
                    S.dma("sp", FB[hq][:], fs_d[h:h + 1, :].to_broadcast([128, T]), writes=[FBtk[hq]])
                    S.dma("pool", WV[hq][:], w_in[j, :, D + h * DVh:D + (h + 1) * DVh].rearrange(
                        "(kc p) n -> p kc n", p=128), writes=[WVtk[hq]])
                    S.dma("pool", WO[hq][:], w_in[j, :, 2 * D + h * DVh:2 * D + (h + 1) * DVh].rearrange(
                        "(kc p) n -> p kc n", p=128), writes=[WVtk[hq]])
                    for n in range(NT):
                        pb = 4 + n % 2
                        for kc in range(KC):
                            S.op("pe", lambda e, kc=kc, n=n, pb=pb: e.matmul(
                                PSB[pb][:, 0:DVh], HT[:, kc, n * 128:(n + 1) * 128], WV[hq][:, kc, :],
                                start=(kc == 0), stop=(kc == KC - 1)),
                                 reads=[HTtk[n], WVtk[hq]], writes=[PStk[pb]], signal=(kc == KC - 1))
                        S.op("act", lambda e, n=n, pb=pb: e.copy(out=V1[hq][:, n, 0:DVh], in_=PSB[pb][:, 0:DVh]),
                             reads=[PStk[pb]], writes=[V1tk[hq]])
                    prow = slice((h % 2) * 64, (h % 2) * 64 + 64)
                    qc, kcq = h // 2, 4 + h // 2
                    for sb in range(NT):
                        ya = 2 + sb % 2
                        ssl = slice(sb * 128, (sb + 1) * 128)
                        for jg in range(0, sb + 1, 4):
                            nj = min(4, sb + 1 - jg)
                            sp = gi % 2
                            gi += 1
                            for q in range(nj):
                                jb = jg + q
                                S.op("pe", lambda e, q=q, jb=jb, sp=sp: e.matmul(
                                    PSB[sp][:, q * 128:(q + 1) * 128], QK[prow, kcq, jb * 128:(jb + 1) * 128],
                                    QK[prow, qc, ssl], start=True, stop=True),
                                     reads=[QKtk], writes=[PStk[sp]], signal=(q == nj - 1))
                                S.op("act", lambda e, q=q, jb=jb, sp=sp: e.activation(
                                    out=EW[sp][:, q, :], in_=FB[hq][:, ssl], func=AF.Exp, bias=BJ[:, jb, h:h + 1]),
                                     reads=[FBtk[hq], BJtk], writes=[EWtk[sp]])
                                if jb == sb:
                                    S.op("pool", lambda e, q=q, sp=sp: e.tensor_tensor(
                                        out=EW[sp][:, q, :], in0=EW[sp][:, q, :], in1=TRI[:], op=ALU.mult),
                                         reads=[EWtk[sp], Ctk], writes=[EWtk[sp]])
                            S.op("dve", lambda e, sp=sp, nj=nj: e.tensor_tensor(
                                out=PM[sp][:, 0:nj, :], in0=PSB[sp][:, 0:nj * 128].rearrange("p (q t) -> p q t", q=nj),
                                in1=EW[sp][:, 0:nj, :], op=ALU.mult),
                                 reads=[PStk[sp], EWtk[sp]], writes=[PMtk[sp]])
                            for q in range(nj):
                                jb = jg + q
                                S.op("pe", lambda e, q=q, jb=jb, sp=sp, ya=ya: e.matmul(
                                    PSB[ya][:, 0:DVh + 1], PM[sp][:, q, :], V1[hq][:, jb, :],
                                    start=(jb == 0), stop=(jb == sb)),
                                     reads=[PMtk[sp], V1tk[hq]], writes=[PStk[ya]], signal=(jb == sb))
                        gp = 6 + sb % 2
                        for kc in range(KC):
                            S.op("pe", lambda e, kc=kc, gp=gp, ssl=ssl: e.matmul(
                                PSB[gp][:, 0:DVh], HT[:, kc, ssl], WO[hq][:, kc, :],
                                start=(kc == 0), stop=(kc == KC - 1)),
                                 reads=[HTtk[sb], WVtk[hq]], writes=[PStk[gp]], signal=(kc == KC - 1))
                        q2 = sb % 2
                        S.op("act", lambda e, gp=gp, q2=q2: e.activation(out=SO[q2][:], in_=PSB[gp][:, 0:DVh],
                                                                        func=AF.Sigmoid),
                             reads=[PStk[gp]], writes=[SOtk[q2]])
                        S.op("act", lambda e, ya=ya, q2=q2: e.activation(
                            out=DN[q2][:, 0:1], in_=PSB[ya][:, DVh:DVh + 1], func=AF.Abs),
                             reads=[PStk[ya]], writes=[DNtk[q2]])
                        S.op("dve", lambda e, q2=q2: e.tensor_scalar_max(out=DN[q2][:, 0:1], in0=DN[q2][:, 0:1],
                                                                        scalar1=1.0),
                             reads=[DNtk[q2]], writes=[DNtk[q2]])
                        S.op("dve", lambda e, q2=q2: e.reciprocal(out=DN[q2][:, 1:2], in_=DN[q2][:, 0:1]),
                             reads=[DNtk[q2]], writes=[DNtk[q2]])
                        S.op("dve", lambda e, ya=ya, q2=q2: e.tensor_scalar_mul(
                            out=HB[q2][:], in0=PSB[ya][:, 0:DVh], scalar1=DN[q2][:, 1:2]),
                             reads=[PStk[ya], DNtk[q2]], writes=[HBtk[q2]])
                        head_norm_tile(HB[q2][:], HBtk[q2], DVh, 1e-6, GNG[:, h * DVh:(h + 1) * DVh],
                                       GNB[:, h * DVh:(h + 1) * DVh], GNtk, ST[:, q2, :], STtk[q2], HB[q2][:], HBtk[q2])
                        S.op("pool", lambda e, q2=q2, sb=sb, h=h: e.tensor_tensor(
                            out=Zv[:, sb, h * DVh:(h + 1) * DVh], in0=HB[q2][:], in1=SO[q2][:], op=ALU.mult),
                             reads=[HBtk[q2], SOtk[q2]], writes=[Xtk[sb]])
                S.barrier()
        mixer_epilogue(P.dram["ml_w_out"][j], 8)

    RWS = {}
    for nm, shp in (("R", [D, T]), ("K", [D, T]), ("A", [D, T]), ("V", [T, D]), ("VF", [T, D]), ("LW", [T, D]),
                    ("G", [T, D])):
        RWS[nm] = P.dscratch("rw_" + nm, shp)

    def rwkv_stage_a(i, j):
        first = (j == 0)
        dr = P.dram
        with ExitStack() as les:
            XS = P.sb("XS", [128, KC, T], BF16, les)
            XStk = Tk("XS")
            MU = P.sb("MU", [128, 6, KC], F32, les)
            MU1 = P.sb("MU1", [128, 6, KC], F32, les)
            MUtk = Tk("MU")
            for n in range(6):
                S.dma("sp", MU[:, n, :], dr["rw_mu"][j, n].rearrange("(c p) -> p c", p=128), writes=[MUtk],
                      allow_slow_non_contiguous=True)
            S.op("dve", lambda e: e.tensor_scalar(out=MU1[:], in0=MU[:], scalar1=-1.0, scalar2=1.0, op0=ALU.mult,
                                                  op1=ALU.add), reads=[MUtk], writes=[MUtk])
            WB = [P.sb("WBp%d" % q, [128, KC, D], BF16, les) for q in range(2)]
            WBtk = [Tk("WB0"), Tk("WB1")]
            STG = [P.sb("STG%d" % q, [128, 512], F32, les) for q in range(4)]
            STGtk = [Tk("STG%d" % q) for q in range(4)]
            OUTtk = [Tk("rwout%d" % q) for q in range(4)]
            LO = P.sb("LO", [128, T], BF16, les)
            LOtk = Tk("LO")
            L1 = P.sb("L1", [128, KC, 128], BF16, les)
            L2 = P.sb("L2", [128, D], BF16, les)
            Ltk = Tk("Lw")
            BCV = P.sb("BCV", [128, D], F32, les)
            A0 = P.sb("A0", [128, KC], F32, les)
            VFT = P.sb("VFT", [128, 512], F32, les)
            VFTtk = Tk("VFT")
            sctr = [0]

            def make_xs(n):
                for c in range(KC):
                    S.op("dve", lambda e, c=c: e.tensor_scalar_mul(out=XS[:, c, :], in0=HT[:, c, :],
                                                                   scalar1=MU1[:, n, c:c + 1]),
                         reads=HTtk + [MUtk], writes=[XStk])
                    S.op("dve", lambda e, c=c: e.scalar_tensor_tensor(
                        out=XS[:, c, 1:T], in0=HT[:, c, 0:T - 1], scalar=MU[:, n, c:c + 1], in1=XS[:, c, 1:T],
                        op0=ALU.mult, op1=ALU.add), reads=HTtk + [MUtk, XStk], writes=[XStk])

            def stage_out(ps_ap, pstk, dst_ap, func=None, bias=None, pre=None):
                q = sctr[0] % 4
                sctr[0] += 1
                if pre is not None:
                    pre(q)
                elif func is None:
                    S.op("act", lambda e: e.copy(out=STG[q][:], in_=ps_ap), reads=[pstk], writes=[STGtk[q]])
                else:
                    kw = {} if bias is None else {"bias": bias}
                    S.op("act", lambda e: e.activation(out=STG[q][:], in_=ps_ap, func=func, **kw),
                         reads=[pstk, Ltk], writes=[STGtk[q]])
                S.dma("sp", dst_ap, STG[q][:], reads=[STGtk[q]], writes=[OUTtk[q]])

            def proj_fm(Wt, Wtk_, dst, func=None, bias_fn=None, kdim=KC, rhs_fn=None, rtks=None):
                for c in range(KC):
                    for tb in range(4):
                        pb = (c * 4 + tb) % 4
                        tsl = slice(tb * 512, (tb + 1) * 512)
                        if rhs_fn is None:
                            for kc in range(KC):
                                S.op("pe", lambda e, kc=kc: e.matmul(PSB[pb][:], Wt[:, kc, c * 128:(c + 1) * 128],
                                                                     XS[:, kc, tsl], start=(kc == 0),
                                                                     stop=(kc == KC - 1)),
                                     reads=[XStk, Wtk_], writes=[PStk[pb]], signal=(kc == KC - 1))
                        else:
                            rhs_fn(pb, c, tsl)
                        stage_out(PSB[pb][:], PStk[pb], dst[c * 128:(c + 1) * 128, tsl], func,
                                  None if bias_fn is None else bias_fn(c))

            def proj_tm(lhs_fn, ltks, Wt, Wtk_, dst, nk, post=None):
                for n in range(NT):
                    for nh in range(2):
                        pb = 4 + (n * 2 + nh) % 4
                        for kc in range(nk):
                            S.op("pe", lambda e, kc=kc: e.matmul(PSB[pb][:], lhs_fn(kc, n), Wt(kc, nh),
                                                                 start=(kc == 0), stop=(kc == nk - 1)),
                                 reads=ltks + [Wtk_], writes=[PStk[pb]], signal=(kc == nk - 1))
                        dsl = dst[n * 128:(n + 1) * 128, nh * 512:(nh + 1) * 512]
                        if post is None:
                            stage_out(PSB[pb][:], PStk[pb], dsl)
                        else:
                            stage_out(PSB[pb][:], PStk[pb], dsl, pre=lambda q, pb=pb, n=n, nh=nh: post(q, pb, n, nh))

            def load_w(q, ap):
                for c0 in range(0, KC, 4):
                    S.dma("pool", WB[q][:, :, c0 * 128:(c0 + 4) * 128],
                          ap[:, c0 * 128:(c0 + 4) * 128].rearrange("(kc p) n -> p kc n", p=128), writes=[WBtk[q]])

            def lora1(w1_ap, r, func):
                S.dma("pool", L1[:, :, 0:r], w1_ap.rearrange("(kc p) n -> p kc n", p=128), writes=[Ltk])
                for tb in range(4):
                    pb = tb % 4
                    tsl = slice(tb * 512, (tb + 1) * 512)
                    for kc in range(KC):
                        S.op("pe", lambda e, kc=kc: e.matmul(PSB[pb][0:r, :], L1[:, kc, 0:r], XS[:, kc, tsl],
                                                             start=(kc == 0), stop=(kc == KC - 1)),
                             reads=[XStk, Ltk], writes=[PStk[pb]], signal=(kc == KC - 1))
                    if func is None:
                        S.op("act", lambda e: e.copy(out=LO[0:r, tsl], in_=PSB[pb][0:r, :]), reads=[PStk[pb]],
                             writes=[LOtk])
                    else:
                        S.op("act", lambda e: e.activation(out=LO[0:r, tsl], in_=PSB[pb][0:r, :], func=func),
                             reads=[PStk[pb]], writes=[LOtk])

            load_w(0, dr["rw_w_rkv"][j, 0])
            load_w(1, dr["rw_w_rkv"][j, 1])
            make_xs(0)
            proj_fm(WB[0], WBtk[0], RWS["R"])
            make_xs(1)
            proj_fm(WB[1], WBtk[1], RWS["K"])
            load_w(0, dr["rw_w_rkv"][j, 2])
            make_xs(2)
            if first:
                proj_tm(lambda kc, n: XS[:, kc, n * 128:(n + 1) * 128], [XStk],
                        lambda kc, nh: WB[0][:, kc, nh * 512:(nh + 1) * 512], WBtk[0], RWS["VF"], KC)
            else:
                lora1(dr["rw_v1"][j - 1], 32, None)
                S.dma("pool", L2[0:32, :], dr["rw_v2"][j - 1], writes=[Ltk])
                S.dma("sp", BCV[:], dr["rw_v0"][j - 1:j, :].to_broadcast([128, D]), writes=[Ltk])
                SGV = P.sb("SGV", [128, 512], F32, les)
                SGVtk = Tk("SGV")

                def vpost(q, pb, n, nh):
                    csl = slice(nh * 512, (nh + 1) * 512)
                    gp = (pb - 4 + 2) % 4
                    S.op("pe", lambda e: e.matmul(PSB[gp][:], LO[0:32, n * 128:(n + 1) * 128], L2[0:32, csl],
                                                  start=True, stop=True), reads=[LOtk, Ltk], writes=[PStk[gp]])
                    S.op("dve", lambda e: e.tensor_tensor(out=SGV[:], in0=PSB[gp][:], in1=BCV[:, csl], op=ALU.add),
                         reads=[PStk[gp], Ltk], writes=[SGVtk])
                    S.op("act", lambda e: e.activation(out=SGV[:], in_=SGV[:], func=AF.Sigmoid), reads=[SGVtk],
                         writes=[SGVtk])
                    S.dma("sp", VFT[:], RWS["VF"][n * 128:(n + 1) * 128, csl], writes=[VFTtk])
                    S.op("dve", lambda e: e.tensor_tensor(out=VFT[:], in0=VFT[:], in1=PSB[pb][:], op=ALU.subtract),
                         reads=[VFTtk, PStk[pb]], writes=[VFTtk])
                    S.op("dve", lambda e: e.tensor_tensor(out=VFT[:], in0=VFT[:], in1=SGV[:], op=ALU.mult),
                         reads=[VFTtk, SGVtk], writes=[VFTtk])
                    S.op("dve", lambda e: e.tensor_tensor(out=STG[q][:], in0=VFT[:], in1=PSB[pb][:], op=ALU.add),
                         reads=[VFTtk, PStk[pb]], writes=[STGtk[q]])

                proj_tm(lambda kc, n: XS[:, kc, n * 128:(n + 1) * 128], [XStk],
                        lambda kc, nh: WB[0][:, kc, nh * 512:(nh + 1) * 512], WBtk[0], RWS["V"], KC, post=vpost)
            make_xs(3)
            lora1(dr["rw_w1"][j], 64, AF.Tanh)
            S.dma("pool", L2[0:64, :], dr["rw_w2"][j], writes=[Ltk])
            S.dma("sp", BCV[:], dr["rw_w0"][j:j + 1, :].to_broadcast([128, D]), writes=[Ltk])

            def wpost(q, pb, n, nh):
                csl = slice(nh * 512, (nh + 1) * 512)
                S.op("dve", lambda e: e.tensor_tensor(out=STG[q][:], in0=PSB[pb][:], in1=BCV[:, csl], op=ALU.add),
                     reads=[PStk[pb], Ltk], writes=[STGtk[q]])
                S.op("act", lambda e: e.activation(out=STG[q][:], in_=STG[q][:], func=AF.Sigmoid),
                     reads=[STGtk[q]], writes=[STGtk[q]])

            proj_tm(lambda kc, n: LO[0:64, n * 128:(n + 1) * 128], [LOtk],
                    lambda kc, nh: L2[0:64, nh * 512:(nh + 1) * 512], Ltk, RWS["LW"], 1, post=wpost)
            make_xs(4)
            lora1(dr["rw_a1"][j], 64, None)
            S.dma("pool", L2[0:64, :], dr["rw_a2"][j], writes=[Ltk])
            S.dma("sp", A0[:], dr["rw_a0"][j].rearrange("(c p) -> p c", p=128), writes=[Ltk],
                  allow_slow_non_contiguous=True)

            def a_rhs(pb, c, tsl):
                S.op("pe", lambda e: e.matmul(PSB[pb][:], L2[0:64, c * 128:(c + 1) * 128], LO[0:64, tsl],
                                              start=True, stop=True), reads=[LOtk, Ltk], writes=[PStk[pb]])

            proj_fm(None, None, RWS["A"], func=AF.Sigmoid, bias_fn=lambda c: A0[:, c:c + 1], rhs_fn=a_rhs)
            make_xs(5)
            lora1(dr["rw_g1"][j], 128, AF.Sigmoid)
            S.dma("pool", L2[:, :], dr["rw_g2"][j], writes=[Ltk])
            proj_tm(lambda kc, n: LO[:, n * 128:(n + 1) * 128], [LOtk],
                    lambda kc, nh: L2[:, nh * 512:(nh + 1) * 512], Ltk, RWS["G"], 1)
            S.barrier()

    def rwkv_stage_b(i, j):
        first = (j == 0)
        dr = P.dram
        RW_EPS = 64e-5
        with ExitStack() as les:
            HTf = HT[:].bitcast(F32)
            FB_ = [HTf[:, 2 * q:2 * q + 2, :].rearrange("p a b -> p (a b)") for q in range(4)]
            FB_ += [P.sb("rwF%d" % q, [128, T], F32, les)[:] for q in range(3)]
            Rb, Kb, Ab, KPb, B4, B5, B6 = FB_
            Ftk = [Tk("rwFB%d" % q) for q in range(7)]
            Rtk, Ktk, Atk_, KPtk, B4tk, B5tk, B6tk = Ftk
            AR = P.sb("AR", [128, NT, 2, 128], F32, les)
            ARtk = Tk("AR")
            XF = X[:, :, 512:1024]
            VTM, SIGT, BHT, KHT = (XF[:, :, q * 128:(q + 1) * 128] for q in range(4))
            VTMtk, SIGTtk, BHTtk, KHTtk = Tk("VTM"), Tk("SIGT"), Tk("BHT"), Tk("KHT")
            CST = {}
            ctk = Tk("rwconst")
            for nm, w in (("rw_tri_i", 128), ("rw_tri_e", 128), ("rw_mask4", 512), ("rw_maskl", 128),
                          ("rw_blk", 128), ("rw_sel2", 2)):
                CST[nm] = P.sb(nm, [128, w], F32, les)
                S.dma("sp", CST[nm][:], dr[nm][:, :], writes=[ctk])
            PRM = P.sb("PRM", [128, 4, KC], F32, les)
            for q, nm in enumerate(("rw_kk", "rw_ka", "rw_rk")):
                S.dma("sp", PRM[:, q, :], dr[nm][j].rearrange("(c p) -> p c", p=128), writes=[ctk],
                      allow_slow_non_contiguous=True)
            S.op("dve", lambda e: e.tensor_scalar(out=PRM[:, 3, :], in0=PRM[:, 1, :], scalar1=-1.0, scalar2=1.0,
                                                  op0=ALU.mult, op1=ALU.add), reads=[ctk], writes=[ctk])
            GNG = P.sb("rwGNG", [128, 128], F32, les)
            GNB = P.sb("rwGNB", [128, 128], F32, les)
            gntk = Tk("rwgn")
            PL = P.sb("PL", [128, 32], F32, les)
            PLtk = Tk("PL")
            CBON = P.sb("CBON", [128, NT, 2], F32, les)
            CBtk = Tk("CBON")
            SS = P.sb("SS", [128, 128], F32, les)
            SStk = Tk("SS")
            INN = P.sb("INN", [128, 128], F32, les)
            US = P.sb("US", [128, 128], F32, les)
            YS = P.sb("YS", [128, 128], F32, les)
            INNtk, UStk, YStk = Tk("INN"), Tk("US"), Tk("YS")
            MM = [[P.sb("MM%d%d" % (a, b), [128, 512], F32, les) for b in range(2)] for a in range(4)]
            MMtk = [[Tk("MM%d%d" % (a, b)) for b in range(2)] for a in range(4)]
            TT = [[P.sb("TT%d%d" % (a, b), [128, 128], F32, les) for b in range(2)] for a in range(4)]
            TTtk = [[Tk("TT%d%d" % (a, b)) for b in range(2)] for a in range(4)]
            NPc_ = [[P.sb("NP%d%d" % (a, q), [128, 128], F32, les) for q in range(2)] for a in range(4)]
            NNc_ = [[P.sb("NN%d%d" % (a, q), [128, 128], F32, les) for q in range(2)] for a in range(4)]
            NPk_ = [[Tk("NP") for q in range(2)] for a in range(4)]
            NNk_ = [[Tk("NN") for q in range(2)] for a in range(4)]
            RG = [[PStk[2 + a]] * 4 for a in range(4)]
            CK = [PStk[6], PStk[6], PStk[6], PStk[7]]
            GT_ = [P.sb("rwGT%d" % q, [128, 128], F32, les) for q in range(2)]
            GTtk = [Tk("rwGT0"), Tk("rwGT1")]
            YN = [P.sb("rwYN%d" % q, [128, 64], F32, les) for q in range(2)]
            YNtk = [Tk("rwYN0"), Tk("rwYN1")]
            S.op("pool", lambda e: e.memset(INN[:], 0.0), writes=[INNtk])
            S.op("pool", lambda e: e.memset(US[:], 0.0), writes=[UStk])
            v_src = RWS["VF"] if first else RWS["V"]
            v3 = lambda ap: ap.rearrange("p (n t) -> p n t", t=128)

            for c in range(KC):
                fsl = slice(c * 128, (c + 1) * 128)
                S.dma("sp", Rb, RWS["R"][fsl, :], writes=[Rtk])
                S.dma("sp", Kb, RWS["K"][fsl, :], writes=[Ktk])
                S.dma("sp", Ab, RWS["A"][fsl, :], writes=[Atk_])
                S.dma("sp", SIGT, RWS["LW"][:, fsl].rearrange("(n p) f -> p n f", p=128), writes=[SIGTtk])
                S.dma("sp", VTM, v_src[:, fsl].rearrange("(n p) f -> p n f", p=128), writes=[VTMtk])
                S.dma("sp", GNG[:], dr["rw_gn_g"][j:j + 1, fsl].to_broadcast([128, 128]), writes=[gntk])
                S.dma("sp", GNB[:], dr["rw_gn_b"][j:j + 1, fsl].to_broadcast([128, 128]), writes=[gntk])
                S.op("pool", lambda e: e.memset(SS[:], 0.0), writes=[SStk])
                kkp, kap, rkp, ka1 = (PRM[:, q, c:c + 1] for q in range(4))
                S.op("dve", lambda e: e.tensor_scalar(out=KPb, in0=Ab, scalar1=kap, scalar2=ka1, op0=ALU.mult,
                                                      op1=ALU.add), reads=[Atk_, ctk], writes=[KPtk])
                S.op("dve", lambda e: e.tensor_tensor(out=KPb, in0=KPb, in1=Kb, op=ALU.mult), reads=[KPtk, Ktk],
                     writes=[KPtk])
                S.op("dve", lambda e: e.scalar_tensor_tensor(out=B4, in0=Rb, scalar=rkp, in1=KPb, op0=ALU.mult,
                                                             op1=ALU.mult), reads=[Rtk, KPtk, ctk], writes=[B4tk])
                for n in range(NT):
                    pb = n % 2
                    S.op("pe", lambda e, n=n, pb=pb: e.matmul(PSB[pb][:, 0:2], B4[:, n * 128:(n + 1) * 128],
                                                             CST["rw_sel2"][:], start=True, stop=True),
                         reads=[B4tk, ctk], writes=[PStk[pb]])
                    S.op("act", lambda e, n=n, pb=pb: e.copy(out=CBON[:, n, :], in_=PSB[pb][:, 0:2]),
                         reads=[PStk[pb]], writes=[CBtk])
                S.op("dve", lambda e: e.tensor_scalar_mul(out=Kb, in0=Kb, scalar1=kkp), reads=[Ktk, ctk],
                     writes=[Ktk])
                S.op("dve", lambda e: e.tensor_tensor(out=B4, in0=Kb, in1=Kb, op=ALU.mult), reads=[Ktk, B4tk],
                     writes=[B4tk])
                for tb in range(4):
                    pb = tb % 2
                    tsl = slice(tb * 512, (tb + 1) * 512)
                    S.op("pe", lambda e, pb=pb, tsl=tsl: e.matmul(PSB[pb][:], CST["rw_blk"][:], B4[:, tsl],
                                                                 start=True, stop=True),
                         reads=[B4tk, ctk], writes=[PStk[pb]])
                    S.op("act", lambda e, pb=pb, tsl=tsl: e.activation(out=B5[:, tsl], in_=PSB[pb][:], func=AF.Sqrt),
                         reads=[PStk[pb]], writes=[B5tk])
                S.op("dve", lambda e: e.tensor_scalar_max(out=B5, in0=B5, scalar1=1e-12), reads=[B5tk], writes=[B5tk])
                S.op("dve", lambda e: e.reciprocal(out=B5, in_=B5), reads=[B5tk], writes=[B5tk])
                S.op("dve", lambda e: e.tensor_tensor(out=Kb, in0=Kb, in1=B5, op=ALU.mult), reads=[Ktk, B5tk],
                     writes=[Ktk])
                S.op("dve", lambda e: e.tensor_tensor(out=Ab, in0=Ab, in1=Kb, op=ALU.mult), reads=[Atk_, Ktk],
                     writes=[Atk_])
                for n in range(NT):
                    pb = n % 2
                    nsl = slice(n * 128, (n + 1) * 128)
                    S.op("pe", lambda e, n=n, pb=pb: e.matmul(PSB[pb][:, 0:128], SIGT[:, n, :], CST["rw_tri_i"][:],
                                                             start=True, stop=True),
                         reads=[SIGTtk, ctk], writes=[PStk[pb]], signal=False)
                    S.op("pe", lambda e, n=n, pb=pb: e.matmul(PSB[pb][:, 128:256], SIGT[:, n, :], CST["rw_tri_e"][:],
                                                             start=True, stop=True),
                         reads=[SIGTtk, ctk], writes=[PStk[pb]])
                    S.op("act", lambda e, pb=pb, nsl=nsl: e.activation(out=B4[:, nsl], in_=PSB[pb][:, 0:128],
                                                                      func=AF.Exp),
                         reads=[PStk[pb]], writes=[B4tk])
                    S.op("act", lambda e, pb=pb, nsl=nsl: e.activation(out=B5[:, nsl], in_=PSB[pb][:, 0:128],
                                                                      func=AF.Exp, scale=-1.0),
                         reads=[PStk[pb]], writes=[B5tk])
                    S.op("act", lambda e, pb=pb, nsl=nsl: e.activation(out=B6[:, nsl], in_=PSB[pb][:, 128:256],
                                                                      func=AF.Exp),
                         reads=[PStk[pb]], writes=[B6tk])
                S.op("dve", lambda e: e.tensor_tensor(out=AR[:, :, 1, :], in0=v3(Rb), in1=v3(B4), op=ALU.mult),
                     reads=[Rtk, B4tk], writes=[ARtk])
                S.op("dve", lambda e: e.scalar_tensor_tensor(out=AR[:, :, 0, :], in0=v3(Kb), scalar=-1.0, in1=v3(B6),
                                                             op0=ALU.mult, op1=ALU.mult),
                     reads=[Ktk, B6tk, ARtk], writes=[ARtk])
                S.op("dve", lambda e: e.tensor_tensor(out=KPb, in0=KPb, in1=B5, op=ALU.mult), reads=[KPtk, B5tk],
                     writes=[KPtk])
                S.op("dve", lambda e: e.tensor_tensor(out=Ab, in0=Ab, in1=B5, op=ALU.mult), reads=[Atk_, B5tk],
                     writes=[Atk_])
                ch3 = lambda ap: ap.rearrange("p (c l) -> p c l", l=64)
                S.op("dve", lambda e: e.tensor_copy(out=PL[:], in_=ch3(B4)[:, :, 63]), reads=[B4tk], writes=[PLtk])
                plb = PL[:].unsqueeze(2).to_broadcast([128, 32, 64])
                S.op("dve", lambda e: e.tensor_tensor(out=ch3(B5), in0=ch3(Ab), in1=plb, op=ALU.mult),
                     reads=[Atk_, PLtk, B5tk], writes=[B5tk])
                S.op("dve", lambda e: e.tensor_tensor(out=ch3(B6), in0=ch3(KPb), in1=plb, op=ALU.mult),
                     reads=[KPtk, PLtk, B6tk], writes=[B6tk])
                for n in range(NT):
                    nsl = slice(n * 128, (n + 1) * 128)
                    for (src, stk, dst, dtk, pb) in ((B5, B5tk, BHT, BHTtk, 0), (B6, B6tk, KHT, KHTtk, 1)):
                        S.op("pe", lambda e, src=src, pb=pb, nsl=nsl: e.transpose(PSB[pb][:, 0:128], src[:, nsl],
                                                                                   ident[:]),
                             reads=[stk, identtk], writes=[PStk[pb]])
                        S.op("act", lambda e, dst=dst, pb=pb, n=n: e.copy(out=dst[:, n, :], in_=PSB[pb][:, 0:128]),
                             reads=[PStk[pb]], writes=[dtk])

                def precompute_gen(tiles):
                    chains = [(n, h2) for n in tiles for h2 in range(2)]
                    st_ = {}
                    for ci, (n, h2) in enumerate(chains):
                        ts_ = n % 4
                        nsl = slice(n * 128, (n + 1) * 128)
                        prow = slice(h2 * 64, h2 * 64 + 64)
                        mb = ci % 2
                        S.op("pe", lambda e: e.matmul(PSB[mb][:, 0:256], Ab[prow, nsl],
                                                      AR[prow, n, :, :].rearrange("p a t -> p (a t)"),
                                                      start=True, stop=True),
                             reads=[Atk_, ARtk], writes=[PStk[mb]], signal=False)
                        S.op("pe", lambda e: e.matmul(PSB[mb][:, 256:512], KPb[prow, nsl],
                                                      AR[prow, n, :, :].rearrange("p a t -> p (a t)"),
                                                      start=True, stop=True),
                             reads=[KPtk, ARtk], writes=[PStk[mb]])
                        S.op("dve", lambda e: e.tensor_tensor(out=MM[ts_][h2][:], in0=PSB[mb][:],
                                                              in1=CST["rw_mask4"][:], op=ALU.mult),
                             reads=[PStk[mb], ctk], writes=[MMtk[ts_][h2]])
                        yield
                    for ci, (n, h2) in enumerate(chains):
                        ts_ = n % 4
                        nsl = slice(n * 128, (n + 1) * 128)
                        prow = slice(h2 * 64, h2 * 64 + 64)
                        ib = 2 + ci
                        rg = RG[ci]
                        S.op("pe", lambda e: e.matmul(PSB[ib][:, 0:128], AR[prow, n, 0, :], Ab[prow, nsl],
                                                      start=True, stop=True),
                             reads=[Atk_, ARtk], writes=[rg[0]])
                        S.op("dve", lambda e: e.tensor_tensor(out=NNc_[ci][0][:], in0=PSB[ib][:, 0:128],
                                                              in1=CST["rw_maskl"][:], op=ALU.mult),
                             reads=[rg[0], ctk], writes=[NNk_[ci][0]])
                        S.op("dve", lambda e: e.tensor_tensor(out=TT[ts_][h2][:], in0=MM[ts_][h2][:, 0:128],
                                                              in1=ident[:], op=ALU.add),
                             reads=[MMtk[ts_][h2], identtk], writes=[TTtk[ts_][h2]])
                        st_[ci] = [MM[ts_][h2][:, 0:128], MMtk[ts_][h2], NNc_[ci][0][:], NNk_[ci][0]]
                        yield
                    for k in range(5):
                        q = (k + 1) % 2
                        for ci, (n, h2) in enumerate(chains):
                            ib = 2 + ci
                            np_ap, np_tk, nn_ap, nn_tk = st_[ci]
                            S.op("pe", lambda e: e.matmul(PSB[ib][:, 128:256], np_ap, nn_ap, start=True, stop=True),
                                 reads=[np_tk, nn_tk], writes=[RG[ci][1]])
                            if k < 4:
                                S.op("pe", lambda e: e.matmul(PSB[ib][:, 256:384], nn_ap, np_ap, start=True,
                                                              stop=True),
                                     reads=[np_tk, nn_tk], writes=[RG[ci][2]])
                        yield
                        for ci, (n, h2) in enumerate(chains):
                            ib = 2 + ci
                            S.op("act", lambda e: e.copy(out=NNc_[ci][q][:], in_=PSB[ib][:, 128:256]),
                                 reads=[RG[ci][1]], writes=[NNk_[ci][q]])
                            st_[ci][2], st_[ci][3] = NNc_[ci][q][:], NNk_[ci][q]
                            if k < 4:
                                S.op("act", lambda e: e.copy(out=NPc_[ci][q][:], in_=PSB[ib][:, 256:384]),
                                     reads=[RG[ci][2]], writes=[NPk_[ci][q]])
                                st_[ci][0], st_[ci][1] = NPc_[ci][q][:], NPk_[ci][q]
                        yield
                        for ci, (n, h2) in enumerate(chains):
                            ts_ = n % 4
                            ib = 2 + ci
                            S.op("pe", lambda e: e.matmul(PSB[ib][:, 384:512], st_[ci][2], TT[ts_][h2][:],
                                                          start=True, stop=True),
                                 reads=[st_[ci][3], TTtk[ts_][h2]], writes=[RG[ci][3]])
                        yield
                        for ci, (n, h2) in enumerate(chains):
                            ts_ = n % 4
                            ib = 2 + ci
                            S.op("dve", lambda e: e.tensor_tensor(out=TT[ts_][h2][:], in0=PSB[ib][:, 384:512],
                                                                  in1=TT[ts_][h2][:], op=ALU.add),
                                 reads=[RG[ci][3], TTtk[ts_][h2]], writes=[TTtk[ts_][h2]])
                        yield

                def chain_gen(n):
                    ts_ = n % 4
                    b6, b7 = PSB[6], PSB[7]
                    k6a, k6u, k6y, k7 = CK
                    for par in range(2):
                        tp = slice(par * 64, par * 64 + 64)
                        tc = slice(par * 64, par * 64 + 64)
                        chn = 2 * n + par
                        S.op("pe", lambda e: e.matmul(b6[tp, 0:128], AR[:, n, 0, tc], SS[:], start=True, stop=False),
                             reads=[ARtk, SStk], writes=[k6a], signal=False)
                        for h2 in range(2):
                            ic = slice(h2 * 64, h2 * 64 + 64)
                            S.op("pe", lambda e, h2=h2, ic=ic: e.matmul(
                                b6[tp, ic], MM[ts_][h2][:, 256 + par * 64:256 + par * 64 + 64], VTM[:, n, ic],
                                start=False, stop=(h2 == 1)),
                                 reads=[MMtk[ts_][h2], VTMtk], writes=[k6a], signal=(h2 == 1))
                        yield
                        S.op("act", lambda e: e.copy(out=INN[tp, :], in_=b6[tp, 0:128]), reads=[k6a], writes=[INNtk])
                        yield
                        for h2 in range(2):
                            ic = slice(h2 * 64, h2 * 64 + 64)
                            S.op("pe", lambda e, h2=h2, ic=ic: e.matmul(
                                b6[tp, 128 + h2 * 64:128 + h2 * 64 + 64], TT[ts_][h2][:, tc], INN[:, ic],
                                start=True, stop=True),
                                 reads=[TTtk[ts_][h2], INNtk], writes=[k6u], signal=(h2 == 1))
                        yield
                        S.op("dve", lambda e: e.tensor_copy(out=US[tp, :], in_=b6[tp, 128:256]), reads=[k6u],
                             writes=[UStk])
                        yield
                        S.op("pe", lambda e: e.matmul(b6[tp, 256:384], AR[:, n, 1, tc], SS[:], start=True, stop=False),
                             reads=[ARtk, SStk], writes=[k6y], signal=False)
                        for h2 in range(2):
                            ic = slice(h2 * 64, h2 * 64 + 64)
                            oc = slice(256 + h2 * 64, 256 + h2 * 64 + 64)
                            S.op("pe", lambda e, h2=h2, ic=ic, oc=oc: e.matmul(
                                b6[tp, oc], MM[ts_][h2][:, 128 + par * 64:128 + par * 64 + 64], US[:, ic],
                                start=False, stop=False),
                                 reads=[MMtk[ts_][h2], UStk], writes=[k6y], signal=False)
                            S.op("pe", lambda e, h2=h2, ic=ic, oc=oc: e.matmul(
                                b6[tp, oc], MM[ts_][h2][:, 384 + par * 64:384 + par * 64 + 64], VTM[:, n, ic],
                                start=False, stop=(h2 == 1)),
                                 reads=[MMtk[ts_][h2], VTMtk], writes=[k6y], signal=(h2 == 1))
                        S.op("pe", lambda e: e.matmul(b7[:, 0:128], BHT[tp, n, :], US[tp, :], start=True, stop=False),
                             reads=[BHTtk, UStk], writes=[k7], signal=False)
                        S.op("pe", lambda e: e.matmul(b7[:, 0:128], KHT[tp, n, :], VTM[tp, n, :], start=False,
                                                      stop=True),
                             reads=[KHTtk, VTMtk], writes=[k7])
                        yield
                        S.op("act", lambda e: e.copy(out=YS[tp, :], in_=b6[tp, 256:384]), reads=[k6y], writes=[YStk])
                        for h2 in range(2):
                            pr = slice(h2 * 64, h2 * 64 + 64)
                            S.op("dve", lambda e, pr=pr: e.scalar_tensor_tensor(
                                out=SS[pr, pr], in0=SS[pr, pr], scalar=PL[pr, chn:chn + 1], in1=b7[pr, pr],
                                op0=ALU.mult, op1=ALU.add), reads=[SStk, PLtk, k7], writes=[SStk])
                        yield

                def post(n):
                    gq = n % 2
                    S.dma("sp", GT_[gq][:], RWS["G"][n * 128:(n + 1) * 128, fsl], writes=[GTtk[gq]])
                    for h2 in range(2):
                        ic = slice(h2 * 64, h2 * 64 + 64)
                        gc = slice(c * 128 + h2 * 64, c * 128 + h2 * 64 + 64)
                        head_norm_tile(YS[:, ic], YStk, 64, RW_EPS, GNG[:, ic], GNB[:, ic], gntk, ST[:, h2, :],
                                       STtk[h2], YN[h2][:], YNtk[h2])
                        S.op("dve", lambda e, h2=h2, ic=ic: e.scalar_tensor_tensor(
                            out=YN[h2][:], in0=VTM[:, n, ic], scalar=CBON[:, n, h2:h2 + 1], in1=YN[h2][:],
                            op0=ALU.mult, op1=ALU.add), reads=[VTMtk, CBtk, YNtk[h2]], writes=[YNtk[h2]])
                        S.op("pool", lambda e, h2=h2, ic=ic, gc=gc: e.tensor_tensor(
                            out=Zv[:, n, gc], in0=YN[h2][:], in1=GT_[gq][:, ic], op=ALU.mult),
                             reads=[YNtk[h2], GTtk[gq]], writes=[Xtk[n]])

                def run_merged(gens):
                    gens = [g for g in gens if g is not None]
                    while gens:
                        for g in list(gens):
                            try:
                                next(g)
                            except StopIteration:
                                gens.remove(g)

                def tile_work(tiles):
                    for n in tiles:
                        yield from chain_gen(n)
                        post(n)
                        yield

                run_merged([precompute_gen([0, 1])])
                for g in range(NT // 2):
                    nxt = precompute_gen([2 * g + 2, 2 * g + 3]) if g + 1 < NT // 2 else None
                    run_merged([nxt, tile_work([2 * g, 2 * g + 1])])
                if cfg.get("rw_pairs") and c + 1 >= cfg["rw_pairs"]:
                    break
            S.barrier()
        mixer_epilogue(dr["rw_w_out"][j], 8)

    if cfg.get("dbg_gate"):
        P.dout("dbg_gate", [T, NE])
    for i in layers:
        if 0 in subs:
            kind, jj = i % 3, i // 3
            ada_mod(i, 0)
            modulate_transpose()
            spill_X()
            if kind == 2:
                retention(jj)
            elif kind == 1:
                mlstm(jj)
            else:
                rwkv_stage_a(i, jj)
                if cfg.get("only") == "rwa":
                    break
                rwkv_stage_b(i, jj)
        if 1 in subs:
            ada_mod(i, 1)
            if cfg.get("only") == "ada":
                break
            modulate_transpose()
            moe(i)
            deepnorm_from_X()

    otk = Tk("out")
    for n in range(NT):
        S.dma("sp", out_d[n * 128:(n + 1) * 128, :], X[:, n, :], reads=[Xtk[n]], writes=[otk])
    S.barrier()
    es.close()
    return P


_CONSTS = {}


def _consts():
    if _CONSTS:
        return _CONSTS
    half = 128
    inv = 10000.0 ** -(np.arange(half, dtype=np.float64) / (half - 1))
    ang = (np.arange(T, dtype=np.float32)[:, None] * inv.astype(np.float32)[None, :]).astype(np.float32)
    _CONSTS["rt_cos"] = np.ascontiguousarray(np.cos(ang).T.astype(np.float32))
    _CONSTS["rt_sin"] = np.ascontiguousarray(np.sin(ang).T.astype(np.float32))
    tab = np.zeros((4, 128, 16, 128), np.float64)
    jj = np.arange(128)[:, None]
    ss = np.arange(128)[None, :]
    for h in range(4):
        lg = np.log1p(-2.0 ** (-5.0 - h))
        for r in range(16):
            dl = 15 - r
            ex = 128 * dl + ss - jj
            v = np.exp(lg * np.maximum(ex, 0)) / 16.0
            tab[h, :, r, :] = np.where(ex >= 0, v, 0.0)
    _CONSTS["rt_tab"] = tab.astype(np.float32)
    _CONSTS["tri_incl"] = np.triu(np.ones((128, 128), np.float32))
    _CONSTS["ones"] = np.ones((128, 128), np.float32)
    idx = np.arange(128)
    same = (idx[:, None] // 64) == (idx[None, :] // 64)
    up_i = same & (idx[:, None] <= idx[None, :])
    up_s = same & (idx[:, None] < idx[None, :])
    lo_s = same & (idx[:, None] > idx[None, :])
    cdec = -math.exp(-0.5)
    _CONSTS["rw_tri_i"] = (up_i * cdec).astype(np.float32)
    _CONSTS["rw_tri_e"] = (up_s * cdec).astype(np.float32)
    _CONSTS["rw_mask4"] = np.concatenate([up_s, up_i, up_s, up_i], axis=1).astype(np.float32)
    _CONSTS["rw_maskl"] = lo_s.astype(np.float32)
    _CONSTS["rw_blk"] = same.astype(np.float32)
    sel = np.zeros((128, 2), np.float32)
    sel[:64, 0] = 1.0
    sel[64:, 1] = 1.0
    _CONSTS["rw_sel2"] = sel
    return _CONSTS


def prep_inputs(inputs, b):
    f = lambda a: np.ascontiguousarray(a, dtype=np.float32)
    m = {
        "x": f(inputs["x"][b]),
        "c": f(inputs["c"][b:b + 1]),
        "ada_w": f(inputs["ada_w"]),
        "ada_b": f(inputs["ada_b"]),
        "ln_g": f(inputs["ln_g"]),
        "ln_b": f(inputs["ln_b"]),
        "moe_wr": f(np.concatenate([inputs["moe_w_grp"], inputs["moe_w_exp"]], axis=-1)),
        "moe_br": f(np.concatenate([inputs["moe_b_grp"], inputs["moe_b_exp"]], axis=-1)),
        "moe_w1": f(inputs["moe_w1"]),
        "moe_w3": f(inputs["moe_w3"]),
        "moe_w2": f(inputs["moe_w2"]),
        "ident": np.eye(128, dtype=np.float32),
    }
    for k in ("rt_w_in", "rt_gn_g", "rt_gn_b", "rt_w_out", "ml_w_in", "ml_b_gate", "ml_conv_w", "ml_conv_b",
              "ml_gn_g", "ml_gn_b", "ml_w_out"):
        m[k] = f(inputs[k])
    for k in ("rw_mu", "rw_w_rkv", "rw_w0", "rw_w1", "rw_w2", "rw_a0", "rw_a1", "rw_a2", "rw_v0", "rw_v1", "rw_v2",
              "rw_g1", "rw_g2", "rw_kk", "rw_ka", "rw_gn_g", "rw_gn_b", "rw_w_out"):
        m[k] = f(inputs[k])
    m["rw_rk"] = f(inputs["rw_rk"].reshape(2, D))
    m.update(_consts())
    return m


def kernel(**inputs):
    P = build({})
    in_maps = [prep_inputs(inputs, b) for b in range(8)]
    res = run_bass_kernel_spmd(P.nc, in_maps, core_ids=list(range(8)))
    return np.stack([np.asarray(r["out"]) for r in res.results], axis=0).astype(np.float32)
```

```python
import math
from contextlib import ExitStack
import numpy as np
import concourse.bass as bass
import concourse.mybir as mybir
from concourse.bass_utils import run_bass_kernel_spmd

F32 = mybir.dt.float32
BF16 = mybir.dt.bfloat16
AF = mybir.ActivationFunctionType
ALU = mybir.AluOpType
AX = mybir.AxisListType

D = 1024
T = 2048
NT = T // 128
KC = D // 128
DEPTH = 4
DN_ALPHA = (2 * DEPTH) ** 0.25
LN_EPS = 1e-5
NE = 32
HID = 512
BIG = 1.0e4


class Tk:
    __slots__ = ("name", "wr", "rd", "dsem")

    def __init__(self, name):
        self.name = name
        self.wr = None
        self.rd = {}
        self.dsem = None


class Sync:
    def __init__(self, nc, es):
        self.nc = nc
        self.es = es
        self.eng = {"pe": nc.tensor, "dve": nc.vector, "act": nc.scalar, "pool": nc.gpsimd, "sp": nc.sync}
        self.sem = {}
        self.cnt = {}
        for k in ("pe", "dve", "act", "pool"):
            self.sem[k] = es.enter_context(nc.semaphore("s_" + k))
            self.cnt[k] = 0
        self.free = {"sw": [], "hw": []}
        self.owners = []
        self.nd = 0
        self.seen = {e: {} for e in self.eng}
        self.nwait = 0
        self.ninst = 0

    def _dsem(self, tk, q):
        kind = "sw" if q == "pool" else "hw"
        if tk.dsem is None:
            tk.dsem = {}
        if kind not in tk.dsem:
            if self.free[kind]:
                tk.dsem[kind] = self.free[kind].pop()
            else:
                key = "d%s%d" % (kind, self.nd)
                self.nd += 1
                self.sem[key] = self.es.enter_context(self.nc.semaphore(key))
                self.cnt[key] = 0
                tk.dsem[kind] = key
            self.owners.append((tk, kind))
        return tk.dsem[kind]

    def _wait(self, e, ev):
        key, val = ev
        if self.seen[e].get(key, 0) >= val:
            return
        self.seen[e][key] = val
        self.eng[e].wait_ge(self.sem[key], val)
        self.nwait += 1

    def _deps(self, e, reads, writes):
        evs = {}

        def add(ev):
            if ev is None:
                return
            k, v = ev
            if e == "pe" and k == "pe":
                return
            if evs.get(k, 0) < v:
                evs[k] = v

        for t in reads:
            add(t.wr)
        for t in writes:
            add(t.wr)
            for k, v in t.rd.items():
                add((k, v))
        for k, v in evs.items():
            self._wait(e, (k, v))

    def _post(self, ev, reads, writes):
        k, v = ev
        for t in reads:
            if t.rd.get(k, 0) < v:
                t.rd[k] = v
        for t in writes:
            t.wr = ev
            t.rd = {}

    def op(self, e, fn, reads=(), writes=(), signal=True):
        self._deps(e, reads, writes)
        ins = fn(self.eng[e])
        self.ninst += 1
        ev = (e, self.cnt[e] + 1)
        if signal:
            ins.then_inc(self.sem[e], 1)
            self.cnt[e] += 1
        self._post(ev, reads, writes)
        return ins

    def dma(self, q, out, in_, reads=(), writes=(), **kw):
        self._deps(q, reads, writes)
        key = self._dsem(writes[0], q)
        ins = self.eng[q].dma_start(out=out, in_=in_, **kw)
        ins.then_inc(self.sem[key], 16)
        self.cnt[key] += 16
        self.ninst += 1
        self._post((key, self.cnt[key]), reads, writes)
        return ins

    def barrier(self):
        for e in self.eng:
            for key, val in self.cnt.items():
                if val > 0:
                    self._wait(e, (key, val))
        for tk, kind in self.owners:
            self.free[kind].append(tk.dsem.pop(kind))
        self.owners = []

    def finish(self, tks):
        self.barrier()


class Prog:
    def __init__(self, cfg):
        self.cfg = cfg
        self.nc = bass.Bass("TRN2", target_bir_lowering=False)
        self.es = ExitStack()
        self.S = Sync(self.nc, self.es)
        self.dram = {}
        self.psn = 0

    def din(self, name, shape, dt=F32):
        ap = self.nc.dram_tensor(name, list(shape), dt, kind="ExternalInput").ap()
        self.dram[name] = ap
        return ap

    def dout(self, name, shape, dt=F32):
        ap = self.nc.dram_tensor(name, list(shape), dt, kind="ExternalOutput").ap()
        self.dram[name] = ap
        return ap

    def dscratch(self, name, shape, dt=F32):
        kind = "ExternalOutput" if self.cfg.get("dbg_scratch") else "Internal"
        ap = self.nc.dram_tensor(name, list(shape), dt, kind=kind).ap()
        self.dram[name] = ap
        return ap

    def dump(self, name, ap, tks, dt=F32):
        if name not in self.cfg.get("dumps", ()):
            return
        d = self.dout("dump_" + name, list(ap.shape), dt)
        self.S.dma("sp", d, ap, reads=list(tks), writes=[Tk("dump_" + name)])

    def sb(self, name, shape, dt=F32, es=None):
        self.psn += 1
        return (es or self.es).enter_context(self.nc.sbuf_tensor("sb%d_%s" % (self.psn, name), list(shape), dt))

    def ps(self, name, shape, dt=F32, es=None):
        self.psn += 1
        return (es or self.es).enter_context(self.nc.psum_tensor("ps%d_%s" % (self.psn, name), list(shape), dt))


def _layer_norm_tile(P, S, src, src_tk, dst, dst_tk, gB, bB, gb_tk, st, st_tk, tmp=None, tmp_tk=None):
    nc = P.nc
    for c in range(2):
        S.op("dve", lambda e, c=c: e.bn_stats(out=st[:, c * 6:(c + 1) * 6], in_=src[:, c * 512:(c + 1) * 512]),
             reads=[src_tk], writes=[st_tk])
    S.op("dve", lambda e: e.bn_aggr(out=st[:, 12:14], in_=st[:, 0:12]), reads=[st_tk], writes=[st_tk])
    S.op("dve", lambda e: e.tensor_scalar_add(out=st[:, 15:16], in0=st[:, 13:14], scalar1=LN_EPS),
         reads=[st_tk], writes=[st_tk])
    S.op("act", lambda e: e.activation(out=st[:, 15:16], in_=st[:, 15:16], func=AF.Ln), reads=[st_tk],
         writes=[st_tk])
    S.op("act", lambda e: e.activation(out=st[:, 14:15], in_=st[:, 15:16], func=AF.Exp, scale=-0.5), reads=[st_tk],
         writes=[st_tk])
    S.op("dve", lambda e: e.tensor_scalar(out=dst, in0=src, scalar1=st[:, 12:13], scalar2=st[:, 14:15],
                                          op0=ALU.subtract, op1=ALU.mult), reads=[src_tk, st_tk], writes=[dst_tk])
    S.op("pool", lambda e: e.tensor_tensor(out=dst, in0=dst, in1=gB, op=ALU.mult), reads=[dst_tk, gb_tk],
         writes=[dst_tk])
    S.op("pool", lambda e: e.tensor_tensor(out=dst, in0=dst, in1=bB, op=ALU.add), reads=[dst_tk, gb_tk],
         writes=[dst_tk])


def build(cfg):
    P = Prog(cfg)
    nc, S, es = P.nc, P.S, P.es
    layers = cfg.get("layers", list(range(DEPTH)))
    subs = cfg.get("subs", (0, 1))

    x_d = P.din("x", [T, D])
    c_d = P.din("c", [1, D])
    ada_w = P.din("ada_w", [DEPTH, 2, D, 3 * D])
    ada_b = P.din("ada_b", [DEPTH, 2, 3 * D])
    ln_g = P.din("ln_g", [DEPTH, 2, D])
    ln_b = P.din("ln_b", [DEPTH, 2, D])
    moe_wr = P.din("moe_wr", [DEPTH, D, 36])
    moe_br = P.din("moe_br", [DEPTH, 36])
    moe_w1 = P.din("moe_w1", [DEPTH, NE, D, HID])
    moe_w3 = P.din("moe_w3", [DEPTH, NE, D, HID])
    moe_w2 = P.din("moe_w2", [DEPTH, NE, HID, D])
    ident_d = P.din("ident", [128, 128])
    P.din("rt_w_in", [1, D, 6 * D])
    P.din("rt_gn_g", [1, 2 * D])
    P.din("rt_gn_b", [1, 2 * D])
    P.din("rt_w_out", [1, 2 * D, D])
    for nm, shp in (("rw_mu", [2, 6, D]), ("rw_w_rkv", [2, 3, D, D]), ("rw_w0", [2, D]), ("rw_w1", [2, D, 64]),
                    ("rw_w2", [2, 64, D]), ("rw_a0", [2, D]), ("rw_a1", [2, D, 64]), ("rw_a2", [2, 64, D]),
                    ("rw_v0", [1, D]), ("rw_v1", [1, D, 32]), ("rw_v2", [1, 32, D]), ("rw_g1", [2, D, 128]),
                    ("rw_g2", [2, 128, D]), ("rw_kk", [2, D]), ("rw_ka", [2, D]), ("rw_rk", [2, D]),
                    ("rw_gn_g", [2, D]), ("rw_gn_b", [2, D]), ("rw_w_out", [2, D, D])):
        P.din(nm, shp)
    for nm, w in (("rw_tri_i", 128), ("rw_tri_e", 128), ("rw_mask4", 512), ("rw_maskl", 128), ("rw_blk", 128),
                  ("rw_sel2", 2)):
        P.din(nm, [128, w])
    P.din("ml_w_in", [1, D, 3 * D + 16])
    P.din("ml_b_gate", [1, 16])
    P.din("ml_conv_w", [1, 4, D])
    P.din("ml_conv_b", [1, D])
    P.din("ml_gn_g", [1, D])
    P.din("ml_gn_b", [1, D])
    P.din("ml_w_out", [1, D, D])
    P.din("tri_incl", [128, 128])
    P.din("ones", [128, 128])
    P.din("rt_cos", [128, T])
    P.din("rt_sin", [128, T])
    P.din("rt_tab", [4, 128, 16, 128])
    out_d = P.dout("out", [T, D])

    X = P.sb("X", [128, NT, D], F32)
    Xtk = [Tk("X%d" % n) for n in range(NT)]
    HT = P.sb("HT", [128, KC, T], BF16)
    HTtk = [Tk("HT%d" % n) for n in range(NT)]
    MOD = P.sb("MOD", [128, 3 * D], F32)
    MODtk = Tk("MOD")
    LNG = P.sb("LNG", [128, D], F32)
    LNB = P.sb("LNB", [128, D], F32)
    LNtk = Tk("LNGB")
    ident = P.sb("ident", [128, 128], F32)
    identtk = Tk("ident")
    csb = P.sb("csb", [128, KC, 128], BF16)
    csbtk = Tk("csb")
    ST = P.sb("ST", [128, 2, 16], F32)
    STtk = [Tk("ST0"), Tk("ST1")]

    S.dma("sp", ident[:], ident_d[:, :], writes=[identtk])
    for n in range(NT):
        S.dma("sp", X[:, n, :], x_d[n * 128:(n + 1) * 128, :], writes=[Xtk[n]])

    with ExitStack() as les:
        c_sb = P.sb("c_sb", [128, KC], F32, les)
        ctk = Tk("c")
        S.dma("sp", c_sb[:], c_d[0, :].rearrange("(k p) -> p k", p=128), writes=[ctk],
              allow_slow_non_contiguous=True)
        S.op("act", lambda e: e.activation(out=c_sb[:], in_=c_sb[:], func=AF.Silu), reads=[ctk], writes=[ctk])
        for k in range(KC):
            S.op("dve", lambda e, k=k: e.tensor_copy(out=csb[:, k, :], in_=c_sb[:, k:k + 1].to_broadcast([128, 128])),
                 reads=[ctk], writes=[csbtk])
        S.finish([csbtk])

    PSB = [P.ps("psb%d" % i, [128, 512], F32) for i in range(8)]
    PStk = [Tk("ps%d" % i) for i in range(8)]

    def ada_mod(i, k):
        with ExitStack() as les:
            wb = [P.sb("adaw%d" % j, [128, KC, 512], BF16, les) for j in range(2)]
            wtk = [Tk("adaw0"), Tk("adaw1")]
            bb = P.sb("adab", [128, 3 * D], F32, les)
            btk = Tk("adab")
            S.dma("sp", bb[:], ada_b[i, k:k + 1, :].to_broadcast([128, 3 * D]), writes=[btk])
            S.dma("sp", LNG[:], ln_g[i, k:k + 1, :].to_broadcast([128, D]), writes=[LNtk])
            S.dma("sp", LNB[:], ln_b[i, k:k + 1, :].to_broadcast([128, D]), writes=[LNtk])
            for nchunk in range(6):
                j = nchunk % 2
                S.dma("pool", wb[j][:], ada_w[i, k, :, nchunk * 512:(nchunk + 1) * 512].rearrange(
                    "(kc p) n -> p kc n", p=128), writes=[wtk[j]])
                pb = nchunk % 2
                for kc in range(KC):
                    S.op("pe", lambda e, kc=kc, j=j, pb=pb: e.matmul(PSB[pb][:], csb[:, kc, :], wb[j][:, kc, :],
                                                                   start=(kc == 0), stop=(kc == KC - 1)),
                         reads=[csbtk, wtk[j]], writes=[PStk[pb]], signal=(kc == KC - 1))
                sl = slice(nchunk * 512, (nchunk + 1) * 512)
                S.op("dve", lambda e, pb=pb, sl=sl: e.tensor_tensor(out=MOD[:, sl], in0=PSB[pb][:], in1=bb[:, sl],
                                                                   op=ALU.add),
                     reads=[PStk[pb], btk], writes=[MODtk])
            S.op("dve", lambda e: e.tensor_scalar_add(out=MOD[:, D:3 * D], in0=MOD[:, D:3 * D], scalar1=1.0),
                 reads=[MODtk], writes=[MODtk])
            P.dump("csb", csb[:], [csbtk], BF16)
            P.dump("wb", wb[1][:], [wtk[1]], BF16)
            P.dump("bb", bb[:], [btk])
            P.dump("MOD", MOD[:], [MODtk])
            S.finish([MODtk])

    def modulate_transpose():
        with ExitStack() as les:
            hb = [P.sb("hmod%d" % j, [128, D], F32, les) for j in range(2)]
            htk = [Tk("hmod0"), Tk("hmod1")]
            for n in range(NT):
                j = n % 2
                S.op("dve", lambda e, n=n, j=j: e.tensor_tensor(out=hb[j][:], in0=X[:, n, :], in1=MOD[:, D:2 * D],
                                                               op=ALU.mult),
                     reads=[Xtk[n], MODtk], writes=[htk[j]])
                S.op("pool", lambda e, j=j: e.tensor_tensor(out=hb[j][:], in0=hb[j][:], in1=MOD[:, 0:D], op=ALU.add),
                     reads=[htk[j], MODtk], writes=[htk[j]])
                for half in range(2):
                    pb = (2 * n + half) % 4
                    for q in range(4):
                        kc = half * 4 + q
                        S.op("pe", lambda e, kc=kc, q=q, pb=pb, j=j: e.transpose(
                            PSB[pb][:, q * 128:(q + 1) * 128], hb[j][:, kc * 128:(kc + 1) * 128], ident[:]),
                             reads=[htk[j], identtk], writes=[PStk[pb]], signal=(q == 3))
                    S.op("act", lambda e, half=half, pb=pb, n=n: e.copy(
                        out=HT[:, half * 4:(half + 1) * 4, n * 128:(n + 1) * 128],
                        in_=PSB[pb][:].rearrange("p (q t) -> p q t", q=4)),
                         reads=[PStk[pb]], writes=[HTtk[n]])
            P.dump("HT", HT[:], HTtk, BF16)
            S.finish(HTtk)

    def deepnorm_from_X():
        for n in range(NT):
            j = n % 2
            _layer_norm_tile(P, S, X[:, n, :], Xtk[n], X[:, n, :], Xtk[n], LNG[:], LNB[:], LNtk, ST[:, j, :], STtk[j])

    def moe(i):
        with ExitStack() as les:
            wr = P.sb("wr", [128, KC, 36], BF16, les)
            wrtk = Tk("wr")
            brb = P.sb("brb", [128, 36], F32, les)
            LG = P.sb("LG", [128, NT, 36], F32, les)
            LGtk = Tk("LG")
            G = P.sb("G", [128, NT, NE], F32, les)
            Gtk = Tk("G")
            t1 = P.sb("t1", [128, NT, NE], F32, les)
            t2 = P.sb("t2", [128, NT, NE], F32, les)
            sm = P.sb("sm", [128, 8, NT], F32, les)
            gtk = Tk("gating")
            W1 = [P.sb("W1_%d" % j, [128, KC, HID], BF16, les) for j in range(2)]
            W3 = [P.sb("W3_%d" % j, [128, KC, HID], BF16, les) for j in range(2)]
            W2 = [P.sb("W2_%d" % j, [128, 4, D], BF16, les) for j in range(2)]
            W1tk = [Tk("W1a"), Tk("W1b")]
            W3tk = [Tk("W3a"), Tk("W3b")]
            W2tk = [Tk("W2a"), Tk("W2b")]
            A = [P.sb("A_%d" % j, [128, 4, 512], BF16, les) for j in range(2)]
            Atk = [Tk("Aa"), Tk("Ab")]
            SL = [P.sb("SL_%d" % j, [128, 512], BF16, les) for j in range(2)]
            SLtk = [Tk("SLa"), Tk("SLb")]

            def load_expert(e):
                j = e % 2
                S.dma("pool", W1[j][:], moe_w1[i, e].rearrange("(kc p) n -> p kc n", p=128), writes=[W1tk[j]])
                S.dma("pool", W3[j][:], moe_w3[i, e].rearrange("(kc p) n -> p kc n", p=128), writes=[W3tk[j]])
                S.dma("pool", W2[j][:], moe_w2[i, e].rearrange("(kc p) n -> p kc n", p=128), writes=[W2tk[j]])

            S.dma("pool", wr[:], moe_wr[i].rearrange("(kc p) n -> p kc n", p=128), writes=[wrtk])
            S.dma("sp", brb[:], moe_br[i:i + 1, :].to_broadcast([128, 36]), writes=[wrtk])
            load_expert(0)

            for n in range(NT):
                pb = n % 2
                for kc in range(KC):
                    S.op("pe", lambda e, kc=kc, n=n, pb=pb: e.matmul(PSB[pb][:, 0:36], HT[:, kc, n * 128:(n + 1) * 128],
                                                                   wr[:, kc, :], start=(kc == 0), stop=(kc == KC - 1)),
                         reads=[HTtk[n], wrtk], writes=[PStk[pb]], signal=(kc == KC - 1))
                S.op("dve", lambda e, n=n, pb=pb: e.tensor_tensor(out=LG[:, n, :], in0=PSB[pb][:, 0:36], in1=brb[:],
                                                                 op=ALU.add),
                     reads=[PStk[pb], wrtk], writes=[LGtk])
            P.dump("LG", LG[:], [LGtk])
            gl = LG[:, :, 0:4]
            el = LG[:, :, 4:36]
            gmax, gsum, m1, m2, s1, s2 = (sm[:, q, :] for q in range(6))
            gm4 = t2[:, :, 0:4]
            pen = t2[:, :, 4:8]
            ex4 = t2[:, :, 8:12]

            def dv(fn, reads=(LGtk,), writes=(gtk,)):
                S.op("dve", fn, reads=list(reads) + [gtk], writes=list(writes))

            bc4 = lambda a: a.unsqueeze(2).to_broadcast([128, NT, 4])
            bc32 = lambda a: a.unsqueeze(2).to_broadcast([128, NT, NE])
            dv(lambda e: e.tensor_reduce(out=gmax, in_=gl, axis=AX.X, op=ALU.max))
            dv(lambda e: e.tensor_tensor(out=gm4, in0=gl, in1=bc4(gmax), op=ALU.is_ge))
            dv(lambda e: e.tensor_tensor(out=ex4, in0=gl, in1=bc4(gmax), op=ALU.subtract))
            S.op("act", lambda e: e.activation(out=ex4, in_=ex4, func=AF.Exp), reads=[gtk], writes=[gtk])
            dv(lambda e: e.tensor_reduce(out=gsum, in_=ex4, axis=AX.X, op=ALU.add))
            dv(lambda e: e.reciprocal(out=gsum, in_=gsum))
            dv(lambda e: e.tensor_scalar(out=pen, in0=gm4, scalar1=BIG, scalar2=-BIG, op0=ALU.mult, op1=ALU.add))
            dv(lambda e: e.tensor_tensor(out=t1[:].rearrange("p t (g e) -> p t g e", g=4),
                                         in0=el.rearrange("p t (g e) -> p t g e", g=4),
                                         in1=pen.unsqueeze(3).to_broadcast([128, NT, 4, 8]), op=ALU.add))
            dv(lambda e: e.tensor_reduce(out=m1, in_=t1[:], axis=AX.X, op=ALU.max))
            dv(lambda e: e.tensor_tensor(out=G[:], in0=t1[:], in1=bc32(m1), op=ALU.is_ge), writes=(gtk, Gtk))
            dv(lambda e: e.scalar_tensor_tensor(out=t1[:], in0=G[:], scalar=-BIG, in1=t1[:], op0=ALU.mult, op1=ALU.add),
               reads=(LGtk, Gtk))
            dv(lambda e: e.tensor_reduce(out=m2, in_=t1[:], axis=AX.X, op=ALU.max))
            dv(lambda e: e.tensor_tensor(out=t2[:], in0=t1[:], in1=bc32(m2), op=ALU.is_ge))
            dv(lambda e: e.tensor_tensor(out=s1, in0=m1, in1=m2, op=ALU.subtract))
            S.op("act", lambda e: e.activation(out=s1, in_=s1, func=AF.Sigmoid), reads=[gtk], writes=[gtk])
            dv(lambda e: e.tensor_scalar(out=s2, in0=s1, scalar1=-1.0, scalar2=1.0, op0=ALU.mult, op1=ALU.add))
            dv(lambda e: e.tensor_tensor(out=s1, in0=s1, in1=gsum, op=ALU.mult))
            dv(lambda e: e.tensor_tensor(out=s2, in0=s2, in1=gsum, op=ALU.mult))
            dv(lambda e: e.tensor_tensor(out=G[:], in0=G[:], in1=bc32(s1), op=ALU.mult), reads=(LGtk, Gtk),
               writes=(gtk, Gtk))
            dv(lambda e: e.tensor_tensor(out=t2[:], in0=t2[:], in1=bc32(s2), op=ALU.mult))
            dv(lambda e: e.tensor_tensor(out=G[:], in0=G[:], in1=t2[:], op=ALU.add), reads=(LGtk, Gtk),
               writes=(gtk, Gtk))
            if cfg.get("dbg_gate"):
                S.dma("sp", P.dram["dbg_gate"].rearrange("(n p) e -> p n e", p=128), G[:], reads=[Gtk],
                      writes=[Tk("dbg_gate")])

            for n in range(NT):
                S.op("act", lambda e, n=n: e.mul(out=X[:, n, :], in_=X[:, n, :], mul=DN_ALPHA), reads=[Xtk[n]],
                     writes=[Xtk[n]])

            NB = T // 512
            items = [(e, b) for e in range(NE) for b in range(NB)]

            def stage1(idx):
                e, b = items[idx]
                j = e % 2
                a = idx % 2
                if b == 1 and e + 1 < NE:
                    load_expert(e + 1)
                if b == 0:
                    S.op("dve", lambda en, j=j: en.tensor_tensor(
                        out=W2[j][:], in0=W2[j][:], in1=MOD[:, 2 * D:3 * D].unsqueeze(1).to_broadcast([128, 4, D]),
                        op=ALU.mult), reads=[W2tk[j], MODtk], writes=[W2tk[j]])
                tsl = slice(b * 512, (b + 1) * 512)
                htks = HTtk[b * 4:(b + 1) * 4]
                for hc in range(4):
                    p1 = (hc % 2) * 2
                    p3 = p1 + 1
                    for kc in range(KC):
                        S.op("pe", lambda en, kc=kc, hc=hc, p1=p1, j=j: en.matmul(
                            PSB[p1][:], W1[j][:, kc, hc * 128:(hc + 1) * 128], HT[:, kc, tsl],
                            start=(kc == 0), stop=(kc == KC - 1)),
                             reads=htks + [W1tk[j]], writes=[PStk[p1]], signal=(kc == KC - 1))
                    for kc in range(KC):
                        S.op("pe", lambda en, kc=kc, hc=hc, p3=p3, j=j: en.matmul(
                            PSB[p3][:], W3[j][:, kc, hc * 128:(hc + 1) * 128], HT[:, kc, tsl],
                            start=(kc == 0), stop=(kc == KC - 1)),
                             reads=htks + [W3tk[j]], writes=[PStk[p3]], signal=(kc == KC - 1))
                    sj = hc % 2
                    S.op("act", lambda en, p1=p1, sj=sj: en.activation(out=SL[sj][:], in_=PSB[p1][:], func=AF.Silu),
                         reads=[PStk[p1]], writes=[SLtk[sj]])
                    S.op("dve", lambda en, p3=p3, sj=sj, hc=hc, a=a: en.tensor_tensor(
                        out=A[a][:, hc, :], in0=PSB[p3][:], in1=SL[sj][:], op=ALU.mult),
                         reads=[PStk[p3], SLtk[sj]], writes=[Atk[a]])

            def stage2(idx):
                e, b = items[idx]
                j = e % 2
                a = idx % 2
                for tt in range(4):
                    n = b * 4 + tt
                    pb = 4 + (tt % 2) * 2
                    for nh in range(2):
                        for hc in range(4):
                            S.op("pe", lambda en, hc=hc, nh=nh, tt=tt, pb=pb: en.matmul(
                                PSB[pb + nh][:], A[a][:, hc, tt * 128:(tt + 1) * 128],
                                W2[j][:, hc, nh * 512:(nh + 1) * 512], start=(hc == 0), stop=(hc == 3)),
                                 reads=[Atk[a], W2tk[j]], writes=[PStk[pb + nh]], signal=(hc == 3))
                    for nh in range(2):
                        S.op("dve", lambda en, nh=nh, pb=pb, n=n, e=e: en.scalar_tensor_tensor(
                            out=X[:, n, nh * 512:(nh + 1) * 512], in0=PSB[pb + nh][:], scalar=G[:, n, e:e + 1],
                            in1=X[:, n, nh * 512:(nh + 1) * 512], op0=ALU.mult, op1=ALU.add),
                             reads=[PStk[pb + nh], Gtk, Xtk[n]], writes=[Xtk[n]])

            ne_run = cfg.get("moe_experts", NE)
            items = [(e, b) for e in range(ne_run) for b in range(NB)]
            stage1(0)
            for idx in range(len(items)):
                if idx + 1 < len(items):
                    stage1(idx + 1)
                stage2(idx)
            S.finish(Xtk + W1tk + W2tk + W3tk + Atk + SLtk + [Gtk, gtk, LGtk, wrtk])

    xs_d = P.dscratch("xs", [T, D])
    Zv = X[:].bitcast(BF16)
    identb = P.sb("identb", [128, 128], BF16)
    S.op("dve", lambda e: e.tensor_copy(out=identb[:], in_=ident[:]), reads=[identtk], writes=[identtk])

    def spill_X():
        tk = Tk("xs")
        for n in range(NT):
            S.dma("sp", xs_d[n * 128:(n + 1) * 128, :], X[:, n, :], reads=[Xtk[n]], writes=[tk])
        S.barrier()

    def mixer_epilogue(w_out_ap, nz):
        with ExitStack() as les:
            WO = P.sb("WO", [128, nz, D], BF16, les)
            WOtk = Tk("WO")
            for c0 in range(0, nz, 8):
                S.dma("pool", WO[:, c0:c0 + 8, :], w_out_ap[c0 * 128:(c0 + 8) * 128, :].rearrange(
                    "(c p) n -> p c n", p=128), writes=[WOtk])
            ZT = [P.sb("ZT%d" % j, [128, nz, 128], BF16, les) for j in range(2)]
            ZTtk = [Tk("ZT0"), Tk("ZT1")]
            XO = [P.sb("XO%d" % j, [128, D], F32, les) for j in range(2)]
            XOtk = [Tk("XO0"), Tk("XO1")]
            for n in range(NT):
                j = n % 2
                S.dma("sp", XO[j][:], xs_d[n * 128:(n + 1) * 128, :], writes=[XOtk[j]])
                for c0 in range(0, nz, 8):
                    pb = (c0 // 8) % 2
                    pv = PSB[pb][:].bitcast(BF16)
                    for c in range(c0, c0 + 8):
                        S.op("pe", lambda e, c=c, c0=c0, pv=pv, n=n: e.transpose(
                            pv[:, (c - c0) * 128:(c - c0 + 1) * 128], Zv[:, n, c * 128:(c + 1) * 128], identb[:]),
                             reads=[Xtk[n], identtk], writes=[PStk[pb]], signal=(c == c0 + 7))
                    S.op("act", lambda e, c0=c0, pv=pv, j=j: e.copy(
                        out=ZT[j][:, c0:c0 + 8, :], in_=pv.rearrange("p (c t) -> p c t", c=8)),
                         reads=[PStk[pb]], writes=[ZTtk[j]])
                for nh in range(2):
                    pb = 2 + (n % 2) * 2 + nh
                    for c in range(nz):
                        S.op("pe", lambda e, c=c, nh=nh, pb=pb, j=j: e.matmul(
                            PSB[pb][:], ZT[j][:, c, :], WO[:, c, nh * 512:(nh + 1) * 512],
                            start=(c == 0), stop=(c == nz - 1)),
                             reads=[ZTtk[j], WOtk], writes=[PStk[pb]], signal=(c == nz - 1))
                    S.op("dve", lambda e, nh=nh, pb=pb, n=n: e.tensor_tensor(
                        out=X[:, n, nh * 512:(nh + 1) * 512], in0=PSB[pb][:],
                        in1=MOD[:, 2 * D + nh * 512:2 * D + (nh + 1) * 512], op=ALU.mult),
                         reads=[PStk[pb], MODtk, Xtk[n]], writes=[Xtk[n]])
                S.op("dve", lambda e, n=n, j=j: e.scalar_tensor_tensor(
                    out=X[:, n, :], in0=XO[j][:], scalar=DN_ALPHA, in1=X[:, n, :], op0=ALU.mult, op1=ALU.add),
                     reads=[XOtk[j], Xtk[n]], writes=[Xtk[n]])
                _layer_norm_tile(P, S, X[:, n, :], Xtk[n], X[:, n, :], Xtk[n], LNG[:], LNB[:], LNtk,
                                 ST[:, j, :], STtk[j])
            S.barrier()

    def head_norm_tile(src_ps, src_tk, width, eps, gB, bB, gbtk, st, sttk, tmp, tmptk):
        S.op("dve", lambda e: e.bn_stats(out=st[:, 0:6], in_=src_ps), reads=[src_tk], writes=[sttk])
        S.op("dve", lambda e: e.bn_aggr(out=st[:, 12:14], in_=st[:, 0:6]), reads=[sttk], writes=[sttk])
        S.op("dve", lambda e: e.tensor_scalar_add(out=st[:, 15:16], in0=st[:, 13:14], scalar1=eps),
             reads=[sttk], writes=[sttk])
        S.op("act", lambda e: e.activation(out=st[:, 15:16], in_=st[:, 15:16], func=AF.Ln), reads=[sttk],
             writes=[sttk])
        S.op("act", lambda e: e.activation(out=st[:, 14:15], in_=st[:, 15:16], func=AF.Exp, scale=-0.5),
             reads=[sttk], writes=[sttk])
        S.op("dve", lambda e: e.tensor_scalar(out=tmp, in0=src_ps, scalar1=st[:, 12:13], scalar2=st[:, 14:15],
                                              op0=ALU.subtract, op1=ALU.mult), reads=[src_tk, sttk],
             writes=[tmptk])
        S.op("pool", lambda e: e.tensor_tensor(out=tmp, in0=tmp, in1=gB, op=ALU.mult), reads=[tmptk, gbtk],
             writes=[tmptk])
        S.op("pool", lambda e: e.tensor_tensor(out=tmp, in0=tmp, in1=bB, op=ALU.add), reads=[tmptk, gbtk],
             writes=[tmptk])

    def retention(j):
        RH, DKh, DVh = 4, 256, 512
        w_in = P.dram["rt_w_in"]
        with ExitStack() as les:
            QT = P.sb("QT", [128, 2, T], BF16, les)
            KT = P.sb("KT", [128, 2, T], BF16, les)
            VH = P.sb("VH", [128, NT, DVh], BF16, les)
            QTtk, KTtk, VHtk = Tk("QT"), Tk("KT"), Tk("VH")
            for h in range(RH):
                with ExitStack() as aes:
                    WQ = P.sb("WQ", [128, KC, DKh], BF16, aes)
                    WK = P.sb("WK", [128, KC, DKh], BF16, aes)
                    WV = P.sb("WV", [128, KC, DVh], BF16, aes)
                    COS = P.sb("COS", [128, T], F32, aes)
                    SIN = P.sb("SIN", [128, T], F32, aes)
                    Wtk, CStk = Tk("Wqkv"), Tk("cs")
                    S.dma("pool", WQ[:], w_in[j, :, h * DKh:(h + 1) * DKh].rearrange("(kc p) n -> p kc n", p=128),
                          writes=[Wtk])
                    S.dma("pool", WK[:], w_in[j, :, D + h * DKh:D + (h + 1) * DKh].rearrange(
                        "(kc p) n -> p kc n", p=128), writes=[Wtk])
                    S.dma("pool", WV[:], w_in[j, :, 2 * D + h * DVh:2 * D + (h + 1) * DVh].rearrange(
                        "(kc p) n -> p kc n", p=128), writes=[Wtk])
                    S.dma("sp", COS[:], P.dram["rt_cos"][:, :], writes=[CStk])
                    S.dma("sp", SIN[:], P.dram["rt_sin"][:, :], writes=[CStk])
                    tA = [P.sb("rtA%d" % q, [128, 512], F32, aes) for q in range(4)]
                    tAtk = [Tk("rtA%d" % q) for q in range(4)]
                    for (Wt, OT, OTtk) in ((WQ, QT, QTtk), (WK, KT, KTtk)):
                        for tb in range(4):
                            tsl = slice(tb * 512, (tb + 1) * 512)
                            htks = HTtk[tb * 4:(tb + 1) * 4]
                            pbs = (4 + (tb % 2) * 2, 5 + (tb % 2) * 2)
                            for dc in range(2):
                                for kc in range(KC):
                                    S.op("pe", lambda e, kc=kc, dc=dc, Wt=Wt: e.matmul(
                                        PSB[pbs[dc]][:], Wt[:, kc, dc * 128:(dc + 1) * 128], HT[:, kc, tsl],
                                        start=(kc == 0), stop=(kc == KC - 1)),
                                         reads=htks + [Wtk], writes=[PStk[pbs[dc]]], signal=(kc == KC - 1))
                            x1, x2 = PSB[pbs[0]][:], PSB[pbs[1]][:]
                            k1, k2 = PStk[pbs[0]], PStk[pbs[1]]
                            S.op("dve", lambda e: e.tensor_tensor(out=tA[0][:], in0=x1, in1=COS[:, tsl], op=ALU.mult),
                                 reads=[k1, CStk], writes=[tAtk[0]])
                            S.op("dve", lambda e: e.tensor_tensor(out=tA[1][:], in0=x2, in1=SIN[:, tsl], op=ALU.mult),
                                 reads=[k2, CStk], writes=[tAtk[1]])
                            S.op("dve", lambda e: e.tensor_tensor(out=tA[2][:], in0=x1, in1=SIN[:, tsl], op=ALU.mult),
                                 reads=[k1, CStk], writes=[tAtk[2]])
                            S.op("dve", lambda e: e.tensor_tensor(out=tA[3][:], in0=x2, in1=COS[:, tsl], op=ALU.mult),
                                 reads=[k2, CStk], writes=[tAtk[3]])
                            S.op("pool", lambda e, OT=OT: e.tensor_tensor(out=OT[:, 0, tsl], in0=tA[0][:], in1=tA[1][:],
                                                                         op=ALU.subtract),
                                 reads=[tAtk[0], tAtk[1]], writes=[OTtk])
                            S.op("pool", lambda e, OT=OT: e.tensor_tensor(out=OT[:, 1, tsl], in0=tA[2][:], in1=tA[3][:],
                                                                         op=ALU.add),
                                 reads=[tAtk[2], tAtk[3]], writes=[OTtk])
                    for n in range(NT):
                        pb = 4 + n % 4
                        for kc in range(KC):
                            S.op("pe", lambda e, kc=kc, n=n, pb=pb: e.matmul(
                                PSB[pb][:], HT[:, kc, n * 128:(n + 1) * 128], WV[:, kc, :],
                                start=(kc == 0), stop=(kc == KC - 1)),
                                 reads=[HTtk[n], Wtk], writes=[PStk[pb]], signal=(kc == KC - 1))
                        S.op("act", lambda e, n=n, pb=pb: e.copy(out=VH[:, n, :], in_=PSB[pb][:]),
                             reads=[PStk[pb]], writes=[VHtk])
                    S.barrier()
                with ExitStack() as bes:
                    TAB = P.sb("TAB", [128, 16, 128], F32, bes)
                    GNG = P.sb("GNG", [128, DVh], F32, bes)
                    GNB = P.sb("GNB", [128, DVh], F32, bes)
                    WG = P.sb("WG", [128, KC, DVh], BF16, bes)
                    TABtk, GNtk, WGtk = Tk("TAB"), Tk("GN"), Tk("WG")
                    S.dma("sp", TAB[:], P.dram["rt_tab"][h], writes=[TABtk])
                    S.dma("sp", GNG[:], P.dram["rt_gn_g"][j:j + 1, h * DVh:(h + 1) * DVh].to_broadcast([128, DVh]),
                          writes=[GNtk])
                    S.dma("sp", GNB[:], P.dram["rt_gn_b"][j:j + 1, h * DVh:(h + 1) * DVh].to_broadcast([128, DVh]),
                          writes=[GNtk])
                    S.dma("pool", WG[:], w_in[j, :, 4 * D + h * DVh:4 * D + (h + 1) * DVh].rearrange(
                        "(kc p) n -> p kc n", p=128), writes=[WGtk])
                    PM = [P.sb("PM%d" % q, [128, 4, 128], BF16, bes) for q in range(2)]
                    PMtk = [Tk("PM0"), Tk("PM1")]
                    SG = [P.sb("SG%d" % q, [128, DVh], F32, bes) for q in range(2)]
                    SGtk = [Tk("SG0"), Tk("SG1")]
                    YN = [P.sb("YN%d" % q, [128, DVh], F32, bes) for q in range(2)]
                    YNtk = [Tk("YN0"), Tk("YN1")]
                    gi = 0
                    for sb in range(NT):
                        ya = 2 + sb % 2
                        for jg in range(0, sb + 1, 4):
                            nj = min(4, sb + 1 - jg)
                            sp = gi % 2
                            gi += 1
                            for q in range(nj):
                                jb = jg + q
                                for dc in range(2):
                                    S.op("pe", lambda e, dc=dc, q=q, jb=jb, sp=sp, sb=sb: e.matmul(
                                        PSB[sp][:, q * 128:(q + 1) * 128], KT[:, dc, jb * 128:(jb + 1) * 128],
                                        QT[:, dc, sb * 128:(sb + 1) * 128], start=(dc == 0), stop=(dc == 1)),
                                         reads=[KTtk, QTtk], writes=[PStk[sp]], signal=(dc == 1 and q == nj - 1))
                            r0 = 15 - sb + jg
                            S.op("dve", lambda e, sp=sp, nj=nj, r0=r0: e.tensor_tensor(
                                out=PM[sp][:, 0:nj, :], in0=PSB[sp][:, 0:nj * 128].rearrange("p (q t) -> p q t", q=nj),
                                in1=TAB[:, r0:r0 + nj, :], op=ALU.mult),
                                 reads=[PStk[sp], TABtk], writes=[PMtk[sp]])
                            for q in range(nj):
                                jb = jg + q
                                S.op("pe", lambda e, q=q, jb=jb, sp=sp, ya=ya, sb=sb: e.matmul(
                                    PSB[ya][:], PM[sp][:, q, :], VH[:, jb, :], start=(jb == 0), stop=(jb == sb)),
                                     reads=[PMtk[sp], VHtk], writes=[PStk[ya]], signal=(jb == sb))
                        gp = 4 + sb % 2
                        for kc in range(KC):
                            S.op("pe", lambda e, kc=kc, sb=sb, gp=gp: e.matmul(
                                PSB[gp][:], HT[:, kc, sb * 128:(sb + 1) * 128], WG[:, kc, :],
                                start=(kc == 0), stop=(kc == KC - 1)),
                                 reads=[HTtk[sb], WGtk], writes=[PStk[gp]], signal=(kc == KC - 1))
                        q2 = sb % 2
                        S.op("act", lambda e, gp=gp, q2=q2: e.activation(out=SG[q2][:], in_=PSB[gp][:], func=AF.Silu),
                             reads=[PStk[gp]], writes=[SGtk[q2]])
                        head_norm_tile(PSB[ya][:], PStk[ya], DVh, 1e-6, GNG[:], GNB[:], GNtk, ST[:, q2, :], STtk[q2],
                                       YN[q2][:], YNtk[q2])
                        S.op("pool", lambda e, q2=q2, sb=sb, h=h: e.tensor_tensor(
                            out=Zv[:, sb, h * DVh:(h + 1) * DVh], in0=YN[q2][:], in1=SG[q2][:], op=ALU.mult),
                             reads=[YNtk[q2], SGtk[q2]], writes=[Xtk[sb]])
                    S.barrier()
        mixer_epilogue(P.dram["rt_w_out"][j], 16)

    fs_d = P.dscratch("fs", [8, T])

    def mlstm(j):
        MH, DKh, DVh = 8, 64, 128
        w_in = P.dram["ml_w_in"]
        with ExitStack() as les:
            QK = P.sb("QK", [128, KC, T], BF16, les)
            QKtk = Tk("QK")
            BJ = P.sb("BJ", [128, NT, MH], F32, les)
            BJtk = Tk("BJ")
            WW = P.sb("WW", [128, NT, MH], F32, les)
            CARRY = P.sb("CARRY", [128, NT, MH], F32, les)
            RS = P.sb("RS", [128, NT, MH], F32, les)
            LNDK = P.sb("LNDK", [128, 1], F32, les)
            WWtk, CAtk, RStk = Tk("WW"), Tk("CARRY"), Tk("RS")
            TRI = P.sb("TRI", [128, 128], F32, les)
            ONES = P.sb("ONES", [128, 128], F32, les)
            Ctk = Tk("mlconst")
            S.dma("sp", TRI[:], P.dram["tri_incl"][:, :], writes=[Ctk])
            S.dma("sp", ONES[:], P.dram["ones"][:, :], writes=[Ctk])
            S.op("pool", lambda e: e.memset(LNDK[:], math.log(DKh ** -0.5)), reads=[Ctk], writes=[Ctk])
            with ExitStack() as aes:
                WQK = P.sb("WQK", [128, KC, D], BF16, aes)
                WGT = P.sb("WGT", [128, KC, 16], BF16, aes)
                CW = P.sb("CW", [128, 4, KC], F32, aes)
                CB = P.sb("CB", [128, KC], F32, aes)
                BG = P.sb("BG", [128, 16], F32, aes)
                Wtk = Tk("mlW")
                for c0 in range(0, KC, 4):
                    S.dma("pool", WQK[:, :, c0 * 128:(c0 + 4) * 128], w_in[j, :, c0 * 128:(c0 + 4) * 128].rearrange(
                        "(kc p) n -> p kc n", p=128), writes=[Wtk])
                S.dma("pool", WGT[:], w_in[j, :, 3 * D:3 * D + 16].rearrange("(kc p) n -> p kc n", p=128),
                      writes=[Wtk])
                for tap in range(4):
                    S.dma("sp", CW[:, tap, :], P.dram["ml_conv_w"][j, tap].rearrange("(c p) -> p c", p=128),
                          writes=[Wtk], allow_slow_non_contiguous=True)
                S.dma("sp", CB[:], P.dram["ml_conv_b"][j].rearrange("(c p) -> p c", p=128), writes=[Wtk],
                      allow_slow_non_contiguous=True)
                S.dma("sp", BG[:], P.dram["ml_b_gate"][j:j + 1, :].to_broadcast([128, 16]), writes=[Wtk])
                RAW = [P.sb("RAW%d" % q, [128, T + 3], F32, aes) for q in range(2)]
                RAWtk = [Tk("RAW0"), Tk("RAW1")]
                ACC = P.sb("ACC", [128, T], F32, aes)
                ACCtk = Tk("ACC")
                for q in range(2):
                    S.op("pool", lambda e, q=q: e.memset(RAW[q][:, 0:3], 0.0), writes=[RAWtk[q]])
                for c in range(KC):
                    rq = c % 2
                    for tb in range(4):
                        pb = 4 + tb
                        tsl = slice(tb * 512, (tb + 1) * 512)
                        for kc in range(KC):
                            S.op("pe", lambda e, kc=kc, c=c, pb=pb, tsl=tsl: e.matmul(
                                PSB[pb][:], WQK[:, kc, c * 128:(c + 1) * 128], HT[:, kc, tsl],
                                start=(kc == 0), stop=(kc == KC - 1)),
                                 reads=HTtk[tb * 4:(tb + 1) * 4] + [Wtk], writes=[PStk[pb]], signal=(kc == KC - 1))
                        S.op("act", lambda e, pb=pb, tb=tb, rq=rq: e.copy(
                            out=RAW[rq][:, 3 + tb * 512:3 + (tb + 1) * 512], in_=PSB[pb][:]),
                             reads=[PStk[pb]], writes=[RAWtk[rq]])
                    S.op("dve", lambda e, c=c, rq=rq: e.tensor_scalar(
                        out=ACC[:], in0=RAW[rq][:, 0:T], scalar1=CW[:, 0, c:c + 1], scalar2=CB[:, c:c + 1],
                        op0=ALU.mult, op1=ALU.add), reads=[RAWtk[rq], Wtk], writes=[ACCtk])
                    for tap in range(1, 4):
                        S.op("dve", lambda e, c=c, rq=rq, tap=tap: e.scalar_tensor_tensor(
                            out=ACC[:], in0=RAW[rq][:, tap:tap + T], scalar=CW[:, tap, c:c + 1], in1=ACC[:],
                            op0=ALU.mult, op1=ALU.add), reads=[RAWtk[rq], Wtk, ACCtk], writes=[ACCtk])
                    S.op("act", lambda e, c=c: e.activation(out=QK[:, c, :], in_=ACC[:], func=AF.Silu),
                         reads=[ACCtk], writes=[QKtk])
                GT = P.sb("GT", [128, NT, 16], F32, aes)
                LF = P.sb("LF", [128, NT, MH], F32, aes)
                FF = P.sb("FF", [128, NT, MH], F32, aes)
                GTtk, LFtk, FFtk = Tk("GT"), Tk("LF"), Tk("FF")
                for n in range(NT):
                    pb = n % 2
                    for kc in range(KC):
                        S.op("pe", lambda e, kc=kc, n=n, pb=pb: e.matmul(
                            PSB[pb][:, 0:16], HT[:, kc, n * 128:(n + 1) * 128], WGT[:, kc, :],
                            start=(kc == 0), stop=(kc == KC - 1)),
                             reads=[HTtk[n], Wtk], writes=[PStk[pb]], signal=(kc == KC - 1))
                    S.op("dve", lambda e, n=n, pb=pb: e.tensor_tensor(out=GT[:, n, :], in0=PSB[pb][:, 0:16], in1=BG[:],
                                                                     op=ALU.add),
                         reads=[PStk[pb], Wtk], writes=[GTtk])
                S.op("act", lambda e: e.activation(out=LF[:], in_=GT[:, :, 8:16], func=AF.Exp, scale=-1.0),
                     reads=[GTtk], writes=[LFtk])
                S.op("act", lambda e: e.activation(out=LF[:], in_=LF[:], func=AF.Ln, bias=1.0),
                     reads=[LFtk], writes=[LFtk])
                S.op("dve", lambda e: e.tensor_scalar_mul(out=LF[:], in0=LF[:], scalar1=-1.0), reads=[LFtk],
                     writes=[LFtk])
                for n in range(NT):
                    S.op("pe", lambda e, n=n: e.matmul(PSB[2][:, n * 16:n * 16 + 8], ONES[:], LF[:, n, :],
                                                       start=True, stop=True),
                         reads=[LFtk, Ctk], writes=[PStk[2]], signal=False)
                    S.op("pe", lambda e, n=n: e.matmul(PSB[2][:, n * 16 + 8:n * 16 + 16], TRI[:], LF[:, n, :],
                                                       start=True, stop=True),
                         reads=[LFtk, Ctk], writes=[PStk[2]], signal=(n == NT - 1))
                cw = PSB[2][:, 0:NT * 16].rearrange("p (n c) -> p n c", c=16)
                S.op("dve", lambda e: e.tensor_copy(out=FF[:], in_=cw[:, :, 0:8]), reads=[PStk[2]], writes=[FFtk])
                S.op("dve", lambda e: e.tensor_copy(out=WW[:], in_=cw[:, :, 8:16]), reads=[PStk[2]], writes=[WWtk])
                S.op("pool", lambda e: e.memset(CARRY[:, 0, :], 0.0), writes=[CAtk])
                for n in range(1, NT):
                    S.op("dve", lambda e, n=n: e.tensor_tensor(out=CARRY[:, n, :], in0=CARRY[:, n - 1, :],
                                                               in1=FF[:, n - 1, :], op=ALU.add),
                         reads=[FFtk, CAtk], writes=[CAtk])
                S.op("act", lambda e: e.activation(out=RS[:], in_=WW[:], func=AF.Exp), reads=[WWtk], writes=[RStk])
                S.op("dve", lambda e: e.tensor_tensor(out=BJ[:], in0=GT[:, :, 0:8], in1=WW[:], op=ALU.subtract),
                     reads=[GTtk, WWtk], writes=[BJtk])
                S.op("act", lambda e: e.activation(out=BJ[:], in_=BJ[:], func=AF.Exp,
                                                   bias=LNDK[:, 0:1]), reads=[BJtk, Ctk], writes=[BJtk])
                P.dump("QK", QK[:], [QKtk], BF16)
                S.barrier()
            with ExitStack() as bes:
                GNG = P.sb("GNG", [128, D], F32, bes)
                GNB = P.sb("GNB", [128, D], F32, bes)
                GNtk = Tk("GN")
                S.dma("sp", GNG[:], P.dram["ml_gn_g"][j:j + 1, :].to_broadcast([128, D]), writes=[GNtk])
                S.dma("sp", GNB[:], P.dram["ml_gn_b"][j:j + 1, :].to_broadcast([128, D]), writes=[GNtk])
                WV = [P.sb("WVh%d" % q, [128, KC, DVh], BF16, bes) for q in range(2)]
                WO = [P.sb("WOh%d" % q, [128, KC, DVh], BF16, bes) for q in range(2)]
                WVtk = [Tk("WV0"), Tk("WV1")]
                V1 = [P.sb("V1%d" % q, [128, NT, DVh + 1], BF16, bes) for q in range(2)]
                V1tk = [Tk("V10"), Tk("V11")]
                GC = [P.sb("GC%d" % q, [128, NT], F32, bes) for q in range(2)]
                GCtk = [Tk("GC0"), Tk("GC1")]
                PM = [P.sb("PMm%d" % q, [128, 4, 128], BF16, bes) for q in range(2)]
                PMtk = [Tk("PM0"), Tk("PM1")]
                SO = [P.sb("SO%d" % q, [128, DVh], F32, bes) for q in range(2)]
                SOtk = [Tk("SO0"), Tk("SO1")]
                HB = [P.sb("HB%d" % q, [128, DVh], F32, bes) for q in range(2)]
                HBtk = [Tk("HB0"), Tk("HB1")]
                DN = [P.sb("DN%d" % q, [128, 2], F32, bes) for q in range(2)]
                DNtk = [Tk("DN0"), Tk("DN1")]
                gi = 0
                for h in range(MH):
                    hq = h % 2
                    S.dma("pool", WV[hq][:], w_in[j, :, D + h * DVh:D + (h + 1) * DVh].rearrange(
                        "(kc p) n -> p kc n", p=128), writes=[WVtk[hq]])
                    S.dma("pool", WO[hq][:], w_in[j, :, 2 * D + h * DVh:2 * D + (h + 1) * DVh].rearrange(
                        "(kc p) n -> p kc n", p=128), writes=[WVtk[hq]])
                    for n in range(NT):
                        pb = 4 + n % 2
                        for kc in range(KC):
                            S.op("pe", lambda e, kc=kc, n=n, pb=pb: e.matmul(
                                PSB[pb][:, 0:DVh], HT[:, kc, n * 128:(n + 1) * 128], WV[hq][:, kc, :],
                                start=(kc == 0), stop=(kc == KC - 1)),
                                 reads=[HTtk[n], WVtk[hq]], writes=[PStk[pb]], signal=(kc == KC - 1))
                        S.op("act", lambda e, n=n, pb=pb: e.activation(
                            out=V1[hq][:, n, 0:DVh], in_=PSB[pb][:, 0:DVh], func=AF.Copy, scale=BJ[:, n, h:h + 1]),
                             reads=[PStk[pb], BJtk], writes=[V1tk[hq]])
                    S.op("dve", lambda e: e.tensor_copy(out=V1[hq][:, :, DVh], in_=BJ[:, :, h]),
                         reads=[BJtk, V1tk[hq]], writes=[V1tk[hq]])
                    prow = slice((h % 2) * 64, (h % 2) * 64 + 64)
                    qc, kcq = h // 2, 4 + h // 2
                    for sb in range(NT):
                        ya = 2 + sb % 2
                        q2 = sb % 2
                        ssl = slice(sb * 128, (sb + 1) * 128)
                        S.op("act", lambda e: e.activation(out=GC[q2][:, 0:sb + 1], in_=CARRY[:, 0:sb + 1, h],
                                                           func=AF.Exp, scale=-1.0, bias=CARRY[:, sb, h:h + 1]),
                             reads=[CAtk], writes=[GCtk[q2]])
                        for jg in range(0, sb + 1, 4):
                            nj = min(4, sb + 1 - jg)
                            sp = gi % 2
                            gi += 1
                            for q in range(nj):
                                jb = jg + q
                                S.op("pe", lambda e, q=q, jb=jb, sp=sp: e.matmul(
                                    PSB[sp][:, q * 128:(q + 1) * 128], QK[prow, kcq, jb * 128:(jb + 1) * 128],
                                    QK[prow, qc, ssl], start=True, stop=True),
                                     reads=[QKtk], writes=[PStk[sp]], signal=(q == nj - 1))
                            for q in range(nj):
                                jb = jg + q
                                if jb == sb:
                                    S.op("dve", lambda e, q=q, sp=sp: e.tensor_tensor(
                                        out=PM[sp][:, q, :], in0=PSB[sp][:, q * 128:(q + 1) * 128], in1=TRI[:],
                                        op=ALU.mult), reads=[PStk[sp], Ctk], writes=[PMtk[sp]])
                                else:
                                    S.op("dve", lambda e, q=q, sp=sp, jb=jb: e.tensor_scalar_mul(
                                        out=PM[sp][:, q, :], in0=PSB[sp][:, q * 128:(q + 1) * 128],
                                        scalar1=GC[q2][:, jb:jb + 1]),
                                         reads=[PStk[sp], GCtk[q2]], writes=[PMtk[sp]])
                            for q in range(nj):
                                jb = jg + q
                                S.op("pe", lambda e, q=q, jb=jb, sp=sp, ya=ya: e.matmul(
                                    PSB[ya][:, 0:DVh + 1], PM[sp][:, q, :], V1[hq][:, jb, :],
                                    start=(jb == 0), stop=(jb == sb)),
                                     reads=[PMtk[sp], V1tk[hq]], writes=[PStk[ya]], signal=(jb == sb))
                        gp = 6 + sb % 2
                        for kc in range(KC):
                            S.op("pe", lambda e, kc=kc, gp=gp, ssl=ssl: e.matmul(
                                PSB[gp][:, 0:DVh], HT[:, kc, ssl], WO[hq][:, kc, :],
                                start=(kc == 0), stop=(kc == KC - 1)),
                                 reads=[HTtk[sb], WVtk[hq]], writes=[PStk[gp]], signal=(kc == KC - 1))
                        S.op("act", lambda e, gp=gp, q2=q2: e.activation(out=SO[q2][:], in_=PSB[gp][:, 0:DVh],
                                                                        func=AF.Exp, scale=-1.0),
                             reads=[PStk[gp]], writes=[SOtk[q2]])
                        S.op("pool", lambda e, q2=q2: e.tensor_scalar_add(out=SO[q2][:], in0=SO[q2][:], scalar1=1.0),
                             reads=[SOtk[q2]], writes=[SOtk[q2]])
                        S.op("dve", lambda e, q2=q2: e.reciprocal(out=SO[q2][:], in_=SO[q2][:]),
                             reads=[SOtk[q2]], writes=[SOtk[q2]])
                        S.op("dve", lambda e, ya=ya, q2=q2: e.tensor_tensor(
                            out=DN[q2][:, 0:1], in0=PSB[ya][:, DVh:DVh + 1], in1=RS[:, sb, h:h + 1], op=ALU.mult),
                             reads=[PStk[ya], RStk], writes=[DNtk[q2]])
                        S.op("act", lambda e, q2=q2: e.activation(out=DN[q2][:, 0:1], in_=DN[q2][:, 0:1],
                                                                 func=AF.Abs), reads=[DNtk[q2]], writes=[DNtk[q2]])
                        S.op("dve", lambda e, q2=q2: e.tensor_scalar_max(out=DN[q2][:, 0:1], in0=DN[q2][:, 0:1],
                                                                        scalar1=1.0),
                             reads=[DNtk[q2]], writes=[DNtk[q2]])
                        S.op("dve", lambda e, q2=q2: e.reciprocal(out=DN[q2][:, 1:2], in_=DN[q2][:, 0:1]),
                             reads=[DNtk[q2]], writes=[DNtk[q2]])
                        S.op("dve", lambda e, q2=q2: e.tensor_tensor(out=DN[q2][:, 1:2], in0=DN[q2][:, 1:2],
                                                                    in1=RS[:, sb, h:h + 1], op=ALU.mult),
                             reads=[DNtk[q2], RStk], writes=[DNtk[q2]])
                        S.op("dve", lambda e, ya=ya, q2=q2: e.tensor_scalar_mul(
                            out=HB[q2][:], in0=PSB[ya][:, 0:DVh], scalar1=DN[q2][:, 1:2]),
                             reads=[PStk[ya], DNtk[q2]], writes=[HBtk[q2]])
                        head_norm_tile(HB[q2][:], HBtk[q2], DVh, 1e-6, GNG[:, h * DVh:(h + 1) * DVh],
                                       GNB[:, h * DVh:(h + 1) * DVh], GNtk, ST[:, q2, :], STtk[q2], HB[q2][:], HBtk[q2])
                        S.op("pool", lambda e, q2=q2, sb=sb, h=h: e.tensor_tensor(
                            out=Zv[:, sb, h * DVh:(h + 1) * DVh], in0=HB[q2][:], in1=SO[q2][:], op=ALU.mult),
                             reads=[HBtk[q2], SOtk[q2]], writes=[Xtk[sb]])
                S.barrier()
        mixer_epilogue(P.dram["ml_w_out"][j], 8)

    RWS = {}
    for nm, shp in (("R", [D, T]), ("K", [D, T]), ("A", [D, T]), ("V", [T, D]), ("VF", [T, D]), ("LW", [T, D]),
                    ("G", [T, D])):
        RWS[nm] = P.dscratch("rw_" + nm, shp)

    def rwkv_stage_a(i, j):
        first = (j == 0)
        dr = P.dram
        with ExitStack() as les:
            XS = P.sb("XS", [128, KC, T], BF16, les)
            XStk = Tk("XS")
            MU = P.sb("MU", [128, 6, KC], F32, les)
            MU1 = P.sb("MU1", [128, 6, KC], F32, les)
            MUtk = Tk("MU")
            for n in range(6):
                S.dma("sp", MU[:, n, :], dr["rw_mu"][j, n].rearrange("(c p) -> p c", p=128), writes=[MUtk],
                      allow_slow_non_contiguous=True)
            S.op("dve", lambda e: e.tensor_scalar(out=MU1[:], in0=MU[:], scalar1=-1.0, scalar2=1.0, op0=ALU.mult,
                                                  op1=ALU.add), reads=[MUtk], writes=[MUtk])
            WB = [P.sb("WBp%d" % q, [128, KC, D], BF16, les) for q in range(2)]
            WBtk = [Tk("WB0"), Tk("WB1")]
            STG = [P.sb("STG%d" % q, [128, 512], F32, les) for q in range(4)]
            STGtk = [Tk("STG%d" % q) for q in range(4)]
            OUTtk = [Tk("rwout%d" % q) for q in range(4)]
            LO = P.sb("LO", [128, T], BF16, les)
            LOtk = Tk("LO")
            L1 = P.sb("L1", [128, KC, 128], BF16, les)
            L2 = P.sb("L2", [128, D], BF16, les)
            Ltk = Tk("Lw")
            BCV = P.sb("BCV", [128, D], F32, les)
            A0 = P.sb("A0", [128, KC], F32, les)
            VFT = P.sb("VFT", [128, 512], F32, les)
            VFTtk = Tk("VFT")
            sctr = [0]

            def make_xs(n):
                for c in range(KC):
                    S.op("dve", lambda e, c=c: e.tensor_scalar_mul(out=XS[:, c, :], in0=HT[:, c, :],
                                                                   scalar1=MU1[:, n, c:c + 1]),
                         reads=HTtk + [MUtk], writes=[XStk])
                    S.op("dve", lambda e, c=c: e.scalar_tensor_tensor(
                        out=XS[:, c, 1:T], in0=HT[:, c, 0:T - 1], scalar=MU[:, n, c:c + 1], in1=XS[:, c, 1:T],
                        op0=ALU.mult, op1=ALU.add), reads=HTtk + [MUtk, XStk], writes=[XStk])

            def stage_out(ps_ap, pstk, dst_ap, func=None, bias=None, pre=None):
                q = sctr[0] % 4
                sctr[0] += 1
                if pre is not None:
                    pre(q)
                elif func is None:
                    S.op("act", lambda e: e.copy(out=STG[q][:], in_=ps_ap), reads=[pstk], writes=[STGtk[q]])
                else:
                    kw = {} if bias is None else {"bias": bias}
                    S.op("act", lambda e: e.activation(out=STG[q][:], in_=ps_ap, func=func, **kw),
                         reads=[pstk, Ltk], writes=[STGtk[q]])
                S.dma("sp", dst_ap, STG[q][:], reads=[STGtk[q]], writes=[OUTtk[q]])

            def proj_fm(Wt, Wtk_, dst, func=None, bias_fn=None, kdim=KC, rhs_fn=None, rtks=None):
                for c in range(KC):
                    for tb in range(4):
                        pb = (c * 4 + tb) % 4
                        tsl = slice(tb * 512, (tb + 1) * 512)
                        if rhs_fn is None:
                            for kc in range(KC):
                                S.op("pe", lambda e, kc=kc: e.matmul(PSB[pb][:], Wt[:, kc, c * 128:(c + 1) * 128],
                                                                     XS[:, kc, tsl], start=(kc == 0),
                                                                     stop=(kc == KC - 1)),
                                     reads=[XStk, Wtk_], writes=[PStk[pb]], signal=(kc == KC - 1))
                        else:
                            rhs_fn(pb, c, tsl)
                        stage_out(PSB[pb][:], PStk[pb], dst[c * 128:(c + 1) * 128, tsl], func,
                                  None if bias_fn is None else bias_fn(c))

            def proj_tm(lhs_fn, ltks, Wt, Wtk_, dst, nk, post=None):
                for n in range(NT):
                    for nh in range(2):
                        pb = 4 + (n * 2 + nh) % 4
                        for kc in range(nk):
                            S.op("pe", lambda e, kc=kc: e.matmul(PSB[pb][:], lhs_fn(kc, n), Wt(kc, nh),
                                                                 start=(kc == 0), stop=(kc == nk - 1)),
                                 reads=ltks + [Wtk_], writes=[PStk[pb]], signal=(kc == nk - 1))
                        dsl = dst[n * 128:(n + 1) * 128, nh * 512:(nh + 1) * 512]
                        if post is None:
                            stage_out(PSB[pb][:], PStk[pb], dsl)
                        else:
                            stage_out(PSB[pb][:], PStk[pb], dsl, pre=lambda q, pb=pb, n=n, nh=nh: post(q, pb, n, nh))

            def load_w(q, ap):
                for c0 in range(0, KC, 4):
                    S.dma("pool", WB[q][:, :, c0 * 128:(c0 + 4) * 128],
                          ap[:, c0 * 128:(c0 + 4) * 128].rearrange("(kc p) n -> p kc n", p=128), writes=[WBtk[q]])

            def lora1(w1_ap, r, func):
                S.dma("pool", L1[:, :, 0:r], w1_ap.rearrange("(kc p) n -> p kc n", p=128), writes=[Ltk])
                for tb in range(4):
                    pb = tb % 4
                    tsl = slice(tb * 512, (tb + 1) * 512)
                    for kc in range(KC):
                        S.op("pe", lambda e, kc=kc: e.matmul(PSB[pb][0:r, :], L1[:, kc, 0:r], XS[:, kc, tsl],
                                                             start=(kc == 0), stop=(kc == KC - 1)),
                             reads=[XStk, Ltk], writes=[PStk[pb]], signal=(kc == KC - 1))
                    if func is None:
                        S.op("act", lambda e: e.copy(out=LO[0:r, tsl], in_=PSB[pb][0:r, :]), reads=[PStk[pb]],
                             writes=[LOtk])
                    else:
                        S.op("act", lambda e: e.activation(out=LO[0:r, tsl], in_=PSB[pb][0:r, :], func=func),
                             reads=[PStk[pb]], writes=[LOtk])

            load_w(0, dr["rw_w_rkv"][j, 0])
            load_w(1, dr["rw_w_rkv"][j, 1])
            make_xs(0)
            proj_fm(WB[0], WBtk[0], RWS["R"])
            make_xs(1)
            proj_fm(WB[1], WBtk[1], RWS["K"])
            load_w(0, dr["rw_w_rkv"][j, 2])
            make_xs(2)
            if first:
                proj_tm(lambda kc, n: XS[:, kc, n * 128:(n + 1) * 128], [XStk],
                        lambda kc, nh: WB[0][:, kc, nh * 512:(nh + 1) * 512], WBtk[0], RWS["VF"], KC)
            else:
                lora1(dr["rw_v1"][j - 1], 32, None)
                S.dma("pool", L2[0:32, :], dr["rw_v2"][j - 1], writes=[Ltk])
                S.dma("sp", BCV[:], dr["rw_v0"][j - 1:j, :].to_broadcast([128, D]), writes=[Ltk])
                SGV = P.sb("SGV", [128, 512], F32, les)
                SGVtk = Tk("SGV")

                def vpost(q, pb, n, nh):
                    csl = slice(nh * 512, (nh + 1) * 512)
                    gp = (pb - 4 + 2) % 4
                    S.op("pe", lambda e: e.matmul(PSB[gp][:], LO[0:32, n * 128:(n + 1) * 128], L2[0:32, csl],
                                                  start=True, stop=True), reads=[LOtk, Ltk], writes=[PStk[gp]])
                    S.op("dve", lambda e: e.tensor_tensor(out=SGV[:], in0=PSB[gp][:], in1=BCV[:, csl], op=ALU.add),
                         reads=[PStk[gp], Ltk], writes=[SGVtk])
                    S.op("act", lambda e: e.activation(out=SGV[:], in_=SGV[:], func=AF.Sigmoid), reads=[SGVtk],
                         writes=[SGVtk])
                    S.dma("sp", VFT[:], RWS["VF"][n * 128:(n + 1) * 128, csl], writes=[VFTtk])
                    S.op("dve", lambda e: e.tensor_tensor(out=VFT[:], in0=VFT[:], in1=PSB[pb][:], op=ALU.subtract),
                         reads=[VFTtk, PStk[pb]], writes=[VFTtk])
                    S.op("dve", lambda e: e.tensor_tensor(out=VFT[:], in0=VFT[:], in1=SGV[:], op=ALU.mult),
                         reads=[VFTtk, SGVtk], writes=[VFTtk])
                    S.op("dve", lambda e: e.tensor_tensor(out=STG[q][:], in0=VFT[:], in1=PSB[pb][:], op=ALU.add),
                         reads=[VFTtk, PStk[pb]], writes=[STGtk[q]])

                proj_tm(lambda kc, n: XS[:, kc, n * 128:(n + 1) * 128], [XStk],
                        lambda kc, nh: WB[0][:, kc, nh * 512:(nh + 1) * 512], WBtk[0], RWS["V"], KC, post=vpost)
            make_xs(3)
            lora1(dr["rw_w1"][j], 64, AF.Tanh)
            S.dma("pool", L2[0:64, :], dr["rw_w2"][j], writes=[Ltk])
            S.dma("sp", BCV[:], dr["rw_w0"][j:j + 1, :].to_broadcast([128, D]), writes=[Ltk])

            def wpost(q, pb, n, nh):
                csl = slice(nh * 512, (nh + 1) * 512)
                S.op("dve", lambda e: e.tensor_tensor(out=STG[q][:], in0=PSB[pb][:], in1=BCV[:, csl], op=ALU.add),
                     reads=[PStk[pb], Ltk], writes=[STGtk[q]])
                S.op("act", lambda e: e.activation(out=STG[q][:], in_=STG[q][:], func=AF.Sigmoid),
                     reads=[STGtk[q]], writes=[STGtk[q]])

            proj_tm(lambda kc, n: LO[0:64, n * 128:(n + 1) * 128], [LOtk],
                    lambda kc, nh: L2[0:64, nh * 512:(nh + 1) * 512], Ltk, RWS["LW"], 1, post=wpost)
            make_xs(4)
            lora1(dr["rw_a1"][j], 64, None)
            S.dma("pool", L2[0:64, :], dr["rw_a2"][j], writes=[Ltk])
            S.dma("sp", A0[:], dr["rw_a0"][j].rearrange("(c p) -> p c", p=128), writes=[Ltk],
                  allow_slow_non_contiguous=True)

            def a_rhs(pb, c, tsl):
                S.op("pe", lambda e: e.matmul(PSB[pb][:], L2[0:64, c * 128:(c + 1) * 128], LO[0:64, tsl],
                                              start=True, stop=True), reads=[LOtk, Ltk], writes=[PStk[pb]])

            proj_fm(None, None, RWS["A"], func=AF.Sigmoid, bias_fn=lambda c: A0[:, c:c + 1], rhs_fn=a_rhs)
            make_xs(5)
            lora1(dr["rw_g1"][j], 128, AF.Sigmoid)
            S.dma("pool", L2[:, :], dr["rw_g2"][j], writes=[Ltk])
            proj_tm(lambda kc, n: LO[:, n * 128:(n + 1) * 128], [LOtk],
                    lambda kc, nh: L2[:, nh * 512:(nh + 1) * 512], Ltk, RWS["G"], 1)
            S.barrier()

    def rwkv_stage_b(i, j):
        first = (j == 0)
        dr = P.dram
        RW_EPS = 64e-5
        with ExitStack() as les:
            HTf = HT[:].bitcast(F32)
            FB_ = [HTf[:, 2 * q:2 * q + 2, :].rearrange("p a b -> p (a b)") for q in range(4)]
            FB_ += [P.sb("rwF%d" % q, [128, T], F32, les)[:] for q in range(3)]
            Rb, Kb, Ab, KPb, B4, B5, B6 = FB_
            Ftk = [Tk("rwFB%d" % q) for q in range(7)]
            Rtk, Ktk, Atk_, KPtk, B4tk, B5tk, B6tk = Ftk
            AR = P.sb("AR", [128, NT, 2, 128], F32, les)
            ARtk = Tk("AR")
            XF = X[:, :, 512:1024]
            VTM, SIGT, BHT, KHT = (XF[:, :, q * 128:(q + 1) * 128] for q in range(4))
            VTMtk, SIGTtk, BHTtk, KHTtk = Tk("VTM"), Tk("SIGT"), Tk("BHT"), Tk("KHT")
            CST = {}
            ctk = Tk("rwconst")
            for nm, w in (("rw_tri_i", 128), ("rw_tri_e", 128), ("rw_mask4", 512), ("rw_maskl", 128),
                          ("rw_blk", 128), ("rw_sel2", 2)):
                CST[nm] = P.sb(nm, [128, w], F32, les)
                S.dma("sp", CST[nm][:], dr[nm][:, :], writes=[ctk])
            PRM = P.sb("PRM", [128, 4, KC], F32, les)
            for q, nm in enumerate(("rw_kk", "rw_ka", "rw_rk")):
                S.dma("sp", PRM[:, q, :], dr[nm][j].rearrange("(c p) -> p c", p=128), writes=[ctk],
                      allow_slow_non_contiguous=True)
            S.op("dve", lambda e: e.tensor_scalar(out=PRM[:, 3, :], in0=PRM[:, 1, :], scalar1=-1.0, scalar2=1.0,
                                                  op0=ALU.mult, op1=ALU.add), reads=[ctk], writes=[ctk])
            GNG = P.sb("rwGNG", [128, 128], F32, les)
            GNB = P.sb("rwGNB", [128, 128], F32, les)
            gntk = Tk("rwgn")
            PL = P.sb("PL", [128, 32], F32, les)
            PLtk = Tk("PL")
            CBON = P.sb("CBON", [128, NT, 2], F32, les)
            CBtk = Tk("CBON")
            SS = P.sb("SS", [128, 128], F32, les)
            SStk = Tk("SS")
            INN = P.sb("INN", [128, 128], F32, les)
            US = P.sb("US", [128, 128], F32, les)
            YS = P.sb("YS", [128, 128], F32, les)
            INNtk, UStk, YStk = Tk("INN"), Tk("US"), Tk("YS")
            MM = [[P.sb("MM%d%d" % (a, b), [128, 512], F32, les) for b in range(2)] for a in range(4)]
            MMtk = [[Tk("MM%d%d" % (a, b)) for b in range(2)] for a in range(4)]
            TT = [[P.sb("TT%d%d" % (a, b), [128, 128], F32, les) for b in range(2)] for a in range(4)]
            TTtk = [[Tk("TT%d%d" % (a, b)) for b in range(2)] for a in range(4)]
            NPc_ = [[P.sb("NP%d%d" % (a, q), [128, 128], F32, les) for q in range(2)] for a in range(4)]
            NNc_ = [[P.sb("NN%d%d" % (a, q), [128, 128], F32, les) for q in range(2)] for a in range(4)]
            NPk_ = [[Tk("NP") for q in range(2)] for a in range(4)]
            NNk_ = [[Tk("NN") for q in range(2)] for a in range(4)]
            RG = [[PStk[2 + a]] * 4 for a in range(4)]
            CK = [PStk[6], PStk[6], PStk[6], PStk[7]]
            GT_ = [P.sb("rwGT%d" % q, [128, 128], F32, les) for q in range(2)]
            GTtk = [Tk("rwGT0"), Tk("rwGT1")]
            YN = [P.sb("rwYN%d" % q, [128, 64], F32, les) for q in range(2)]
            YNtk = [Tk("rwYN0"), Tk("rwYN1")]
            S.op("pool", lambda e: e.memset(INN[:], 0.0), writes=[INNtk])
            S.op("pool", lambda e: e.memset(US[:], 0.0), writes=[UStk])
            v_src = RWS["VF"] if first else RWS["V"]
            v3 = lambda ap: ap.rearrange("p (n t) -> p n t", t=128)

            for c in range(KC):
                fsl = slice(c * 128, (c + 1) * 128)
                S.dma("sp", Rb, RWS["R"][fsl, :], writes=[Rtk])
                S.dma("sp", Kb, RWS["K"][fsl, :], writes=[Ktk])
                S.dma("sp", Ab, RWS["A"][fsl, :], writes=[Atk_])
                S.dma("sp", SIGT, RWS["LW"][:, fsl].rearrange("(n p) f -> p n f", p=128), writes=[SIGTtk])
                S.dma("sp", VTM, v_src[:, fsl].rearrange("(n p) f -> p n f", p=128), writes=[VTMtk])
                S.dma("sp", GNG[:], dr["rw_gn_g"][j:j + 1, fsl].to_broadcast([128, 128]), writes=[gntk])
                S.dma("sp", GNB[:], dr["rw_gn_b"][j:j + 1, fsl].to_broadcast([128, 128]), writes=[gntk])
                S.op("pool", lambda e: e.memset(SS[:], 0.0), writes=[SStk])
                kkp, kap, rkp, ka1 = (PRM[:, q, c:c + 1] for q in range(4))
                S.op("dve", lambda e: e.tensor_scalar(out=KPb, in0=Ab, scalar1=kap, scalar2=ka1, op0=ALU.mult,
                                                      op1=ALU.add), reads=[Atk_, ctk], writes=[KPtk])
                S.op("dve", lambda e: e.tensor_tensor(out=KPb, in0=KPb, in1=Kb, op=ALU.mult), reads=[KPtk, Ktk],
                     writes=[KPtk])
                S.op("dve", lambda e: e.scalar_tensor_tensor(out=B4, in0=Rb, scalar=rkp, in1=KPb, op0=ALU.mult,
                                                             op1=ALU.mult), reads=[Rtk, KPtk, ctk], writes=[B4tk])
                for n in range(NT):
                    pb = n % 2
                    S.op("pe", lambda e, n=n, pb=pb: e.matmul(PSB[pb][:, 0:2], B4[:, n * 128:(n + 1) * 128],
                                                             CST["rw_sel2"][:], start=True, stop=True),
                         reads=[B4tk, ctk], writes=[PStk[pb]])
                    S.op("act", lambda e, n=n, pb=pb: e.copy(out=CBON[:, n, :], in_=PSB[pb][:, 0:2]),
                         reads=[PStk[pb]], writes=[CBtk])
                S.op("dve", lambda e: e.tensor_scalar_mul(out=Kb, in0=Kb, scalar1=kkp), reads=[Ktk, ctk],
                     writes=[Ktk])
                S.op("dve", lambda e: e.tensor_tensor(out=B4, in0=Kb, in1=Kb, op=ALU.mult), reads=[Ktk, B4tk],
                     writes=[B4tk])
                for tb in range(4):
                    pb = tb % 2
                    tsl = slice(tb * 512, (tb + 1) * 512)
                    S.op("pe", lambda e, pb=pb, tsl=tsl: e.matmul(PSB[pb][:], CST["rw_blk"][:], B4[:, tsl],
                                                                 start=True, stop=True),
                         reads=[B4tk, ctk], writes=[PStk[pb]])
                    S.op("act", lambda e, pb=pb, tsl=tsl: e.activation(out=B5[:, tsl], in_=PSB[pb][:], func=AF.Sqrt),
                         reads=[PStk[pb]], writes=[B5tk])
                S.op("dve", lambda e: e.tensor_scalar_max(out=B5, in0=B5, scalar1=1e-12), reads=[B5tk], writes=[B5tk])
                S.op("dve", lambda e: e.reciprocal(out=B5, in_=B5), reads=[B5tk], writes=[B5tk])
                S.op("dve", lambda e: e.tensor_tensor(out=Kb, in0=Kb, in1=B5, op=ALU.mult), reads=[Ktk, B5tk],
                     writes=[Ktk])
                S.op("dve", lambda e: e.tensor_tensor(out=Ab, in0=Ab, in1=Kb, op=ALU.mult), reads=[Atk_, Ktk],
                     writes=[Atk_])
                for n in range(NT):
                    pb = n % 2
                    nsl = slice(n * 128, (n + 1) * 128)
                    S.op("pe", lambda e, n=n, pb=pb: e.matmul(PSB[pb][:, 0:128], SIGT[:, n, :], CST["rw_tri_i"][:],
                                                             start=True, stop=True),
                         reads=[SIGTtk, ctk], writes=[PStk[pb]], signal=False)
                    S.op("pe", lambda e, n=n, pb=pb: e.matmul(PSB[pb][:, 128:256], SIGT[:, n, :], CST["rw_tri_e"][:],
                                                             start=True, stop=True),
                         reads=[SIGTtk, ctk], writes=[PStk[pb]])
                    S.op("act", lambda e, pb=pb, nsl=nsl: e.activation(out=B4[:, nsl], in_=PSB[pb][:, 0:128],
                                                                      func=AF.Exp),
                         reads=[PStk[pb]], writes=[B4tk])
                    S.op("act", lambda e, pb=pb, nsl=nsl: e.activation(out=B5[:, nsl], in_=PSB[pb][:, 0:128],
                                                                      func=AF.Exp, scale=-1.0),
                         reads=[PStk[pb]], writes=[B5tk])
                    S.op("act", lambda e, pb=pb, nsl=nsl: e.activation(out=B6[:, nsl], in_=PSB[pb][:, 128:256],
                                                                      func=AF.Exp),
                         reads=[PStk[pb]], writes=[B6tk])
                S.op("dve", lambda e: e.tensor_tensor(out=AR[:, :, 1, :], in0=v3(Rb), in1=v3(B4), op=ALU.mult),
                     reads=[Rtk, B4tk], writes=[ARtk])
                S.op("dve", lambda e: e.scalar_tensor_tensor(out=AR[:, :, 0, :], in0=v3(Kb), scalar=-1.0, in1=v3(B6),
                                                             op0=ALU.mult, op1=ALU.mult),
                     reads=[Ktk, B6tk, ARtk], writes=[ARtk])
                S.op("dve", lambda e: e.tensor_tensor(out=KPb, in0=KPb, in1=B5, op=ALU.mult), reads=[KPtk, B5tk],
                     writes=[KPtk])
                S.op("dve", lambda e: e.tensor_tensor(out=Ab, in0=Ab, in1=B5, op=ALU.mult), reads=[Atk_, B5tk],
                     writes=[Atk_])
                ch3 = lambda ap: ap.rearrange("p (c l) -> p c l", l=64)
                S.op("dve", lambda e: e.tensor_copy(out=PL[:], in_=ch3(B4)[:, :, 63]), reads=[B4tk], writes=[PLtk])
                plb = PL[:].unsqueeze(2).to_broadcast([128, 32, 64])
                S.op("dve", lambda e: e.tensor_tensor(out=ch3(B5), in0=ch3(Ab), in1=plb, op=ALU.mult),
                     reads=[Atk_, PLtk, B5tk], writes=[B5tk])
                S.op("dve", lambda e: e.tensor_tensor(out=ch3(B6), in0=ch3(KPb), in1=plb, op=ALU.mult),
                     reads=[KPtk, PLtk, B6tk], writes=[B6tk])
                for n in range(NT):
                    nsl = slice(n * 128, (n + 1) * 128)
                    for (src, stk, dst, dtk, pb) in ((B5, B5tk, BHT, BHTtk, 0), (B6, B6tk, KHT, KHTtk, 1)):
                        S.op("pe", lambda e, src=src, pb=pb, nsl=nsl: e.transpose(PSB[pb][:, 0:128], src[:, nsl],
                                                                                   ident[:]),
                             reads=[stk, identtk], writes=[PStk[pb]])
                        S.op("act", lambda e, dst=dst, pb=pb, n=n: e.copy(out=dst[:, n, :], in_=PSB[pb][:, 0:128]),
                             reads=[PStk[pb]], writes=[dtk])

                def precompute_gen(tiles):
                    chains = [(n, h2) for n in tiles for h2 in range(2)]
                    st_ = {}
                    for ci, (n, h2) in enumerate(chains):
                        ts_ = n % 4
                        nsl = slice(n * 128, (n + 1) * 128)
                        prow = slice(h2 * 64, h2 * 64 + 64)
                        mb = ci % 2
                        S.op("pe", lambda e: e.matmul(PSB[mb][:, 0:256], Ab[prow, nsl],
                                                      AR[prow, n, :, :].rearrange("p a t -> p (a t)"),
                                                      start=True, stop=True),
                             reads=[Atk_, ARtk], writes=[PStk[mb]], signal=False)
                        S.op("pe", lambda e: e.matmul(PSB[mb][:, 256:512], KPb[prow, nsl],
                                                      AR[prow, n, :, :].rearrange("p a t -> p (a t)"),
                                                      start=True, stop=True),
                             reads=[KPtk, ARtk], writes=[PStk[mb]])
                        S.op("dve", lambda e: e.tensor_tensor(out=MM[ts_][h2][:], in0=PSB[mb][:],
                                                              in1=CST["rw_mask4"][:], op=ALU.mult),
                             reads=[PStk[mb], ctk], writes=[MMtk[ts_][h2]])
                        yield
                    for ci, (n, h2) in enumerate(chains):
                        ts_ = n % 4
                        nsl = slice(n * 128, (n + 1) * 128)
                        prow = slice(h2 * 64, h2 * 64 + 64)
                        ib = 2 + ci
                        rg = RG[ci]
                        S.op("pe", lambda e: e.matmul(PSB[ib][:, 0:128], AR[prow, n, 0, :], Ab[prow, nsl],
                                                      start=True, stop=True),
                             reads=[Atk_, ARtk], writes=[rg[0]])
                        S.op("dve", lambda e: e.tensor_tensor(out=NNc_[ci][0][:], in0=PSB[ib][:, 0:128],
                                                              in1=CST["rw_maskl"][:], op=ALU.mult),
                             reads=[rg[0], ctk], writes=[NNk_[ci][0]])
                        S.op("dve", lambda e: e.tensor_tensor(out=TT[ts_][h2][:], in0=MM[ts_][h2][:, 0:128],
                                                              in1=ident[:], op=ALU.add),
                             reads=[MMtk[ts_][h2], identtk], writes=[TTtk[ts_][h2]])
                        st_[ci] = [MM[ts_][h2][:, 0:128], MMtk[ts_][h2], NNc_[ci][0][:], NNk_[ci][0]]
                        yield
                    for k in range(5):
                        q = (k + 1) % 2
                        for ci, (n, h2) in enumerate(chains):
                            ib = 2 + ci
                            np_ap, np_tk, nn_ap, nn_tk = st_[ci]
                            S.op("pe", lambda e: e.matmul(PSB[ib][:, 128:256], np_ap, nn_ap, start=True, stop=True),
                                 reads=[np_tk, nn_tk], writes=[RG[ci][1]])
                            if k < 4:
                                S.op("pe", lambda e: e.matmul(PSB[ib][:, 256:384], nn_ap, np_ap, start=True,
                                                              stop=True),
                                     reads=[np_tk, nn_tk], writes=[RG[ci][2]])
                        yield
                        for ci, (n, h2) in enumerate(chains):
                            ib = 2 + ci
                            S.op("act", lambda e: e.copy(out=NNc_[ci][q][:], in_=PSB[ib][:, 128:256]),
                                 reads=[RG[ci][1]], writes=[NNk_[ci][q]])
                            st_[ci][2], st_[ci][3] = NNc_[ci][q][:], NNk_[ci][q]
                            if k < 4:
                                S.op("act", lambda e: e.copy(out=NPc_[ci][q][:], in_=PSB[ib][:, 256:384]),
                                     reads=[RG[ci][2]], writes=[NPk_[ci][q]])
                                st_[ci][0], st_[ci][1] = NPc_[ci][q][:], NPk_[ci][q]
                        yield
                        for ci, (n, h2) in enumerate(chains):
                            ts_ = n % 4
                            ib = 2 + ci
                            S.op("pe", lambda e: e.matmul(PSB[ib][:, 384:512], st_[ci][2], TT[ts_][h2][:],
                                                          start=True, stop=True),
                                 reads=[st_[ci][3], TTtk[ts_][h2]], writes=[RG[ci][3]])
                        yield
                        for ci, (n, h2) in enumerate(chains):
                            ts_ = n % 4
                            ib = 2 + ci
                            S.op("dve", lambda e: e.tensor_tensor(out=TT[ts_][h2][:], in0=PSB[ib][:, 384:512],
                                                                  in1=TT[ts_][h2][:], op=ALU.add),
                                 reads=[RG[ci][3], TTtk[ts_][h2]], writes=[TTtk[ts_][h2]])
                        yield

                def chain_gen(n):
                    ts_ = n % 4
                    b6, b7 = PSB[6], PSB[7]
                    k6a, k6u, k6y, k7 = CK
                    for par in range(2):
                        tp = slice(par * 64, par * 64 + 64)
                        tc = slice(par * 64, par * 64 + 64)
                        chn = 2 * n + par
                        S.op("pe", lambda e: e.matmul(b6[tp, 0:128], AR[:, n, 0, tc], SS[:], start=True, stop=False),
                             reads=[ARtk, SStk], writes=[k6a], signal=False)
                        for h2 in range(2):
                            ic = slice(h2 * 64, h2 * 64 + 64)
                            S.op("pe", lambda e, h2=h2, ic=ic: e.matmul(
                                b6[tp, ic], MM[ts_][h2][:, 256 + par * 64:256 + par * 64 + 64], VTM[:, n, ic],
                                start=False, stop=(h2 == 1)),
                                 reads=[MMtk[ts_][h2], VTMtk], writes=[k6a], signal=(h2 == 1))
                        yield
                        S.op("act", lambda e: e.copy(out=INN[tp, :], in_=b6[tp, 0:128]), reads=[k6a], writes=[INNtk])
                        yield
                        for h2 in range(2):
                            ic = slice(h2 * 64, h2 * 64 + 64)
                            S.op("pe", lambda e, h2=h2, ic=ic: e.matmul(
                                b6[tp, 128 + h2 * 64:128 + h2 * 64 + 64], TT[ts_][h2][:, tc], INN[:, ic],
                                start=True, stop=True),
                                 reads=[TTtk[ts_][h2], INNtk], writes=[k6u], signal=(h2 == 1))
                        yield
                        S.op("dve", lambda e: e.tensor_copy(out=US[tp, :], in_=b6[tp, 128:256]), reads=[k6u],
                             writes=[UStk])
                        yield
                        S.op("pe", lambda e: e.matmul(b6[tp, 256:384], AR[:, n, 1, tc], SS[:], start=True, stop=False),
                             reads=[ARtk, SStk], writes=[k6y], signal=False)
                        for h2 in range(2):
                            ic = slice(h2 * 64, h2 * 64 + 64)
                            oc = slice(256 + h2 * 64, 256 + h2 * 64 + 64)
                            S.op("pe", lambda e, h2=h2, ic=ic, oc=oc: e.matmul(
                                b6[tp, oc], MM[ts_][h2][:, 128 + par * 64:128 + par * 64 + 64], US[:, ic],
                                start=False, stop=False),
                                 reads=[MMtk[ts_][h2], UStk], writes=[k6y], signal=False)
                            S.op("pe", lambda e, h2=h2, ic=ic, oc=oc: e.matmul(
                                b6[tp, oc], MM[ts_][h2][:, 384 + par * 64:384 + par * 64 + 64], VTM[:, n, ic],
                                start=False, stop=(h2 == 1)),
                                 reads=[MMtk[ts_][h2], VTMtk], writes=[k6y], signal=(h2 == 1))
                        S.op("pe", lambda e: e.matmul(b7[:, 0:128], BHT[tp, n, :], US[tp, :], start=True, stop=False),
                             reads=[BHTtk, UStk], writes=[k7], signal=False)
                        S.op("pe", lambda e: e.matmul(b7[:, 0:128], KHT[tp, n, :], VTM[tp, n, :], start=False,
                                                      stop=True),
                             reads=[KHTtk, VTMtk], writes=[k7])
                        yield
                        S.op("act", lambda e: e.copy(out=YS[tp, :], in_=b6[tp, 256:384]), reads=[k6y], writes=[YStk])
                        for h2 in range(2):
                            pr = slice(h2 * 64, h2 * 64 + 64)
                            S.op("dve", lambda e, pr=pr: e.scalar_tensor_tensor(
                                out=SS[pr, pr], in0=SS[pr, pr], scalar=PL[pr, chn:chn + 1], in1=b7[pr, pr],
                                op0=ALU.mult, op1=ALU.add), reads=[SStk, PLtk, k7], writes=[SStk])
                        yield

                def post(n):
                    gq = n % 2
                    S.dma("sp", GT_[gq][:], RWS["G"][n * 128:(n + 1) * 128, fsl], writes=[GTtk[gq]])
                    for h2 in range(2):
                        ic = slice(h2 * 64, h2 * 64 + 64)
                        gc = slice(c * 128 + h2 * 64, c * 128 + h2 * 64 + 64)
                        head_norm_tile(YS[:, ic], YStk, 64, RW_EPS, GNG[:, ic], GNB[:, ic], gntk, ST[:, h2, :],
                                       STtk[h2], YN[h2][:], YNtk[h2])
                        S.op("dve", lambda e, h2=h2, ic=ic: e.scalar_tensor_tensor(
                            out=YN[h2][:], in0=VTM[:, n, ic], scalar=CBON[:, n, h2:h2 + 1], in1=YN[h2][:],
                            op0=ALU.mult, op1=ALU.add), reads=[VTMtk, CBtk, YNtk[h2]], writes=[YNtk[h2]])
                        S.op("pool", lambda e, h2=h2, ic=ic, gc=gc: e.tensor_tensor(
                            out=Zv[:, n, gc], in0=YN[h2][:], in1=GT_[gq][:, ic], op=ALU.mult),
                             reads=[YNtk[h2], GTtk[gq]], writes=[Xtk[n]])

                def run_merged(gens):
                    gens = [g for g in gens if g is not None]
                    while gens:
                        for g in list(gens):
                            try:
                                next(g)
                            except StopIteration:
                                gens.remove(g)

                def tile_work(tiles):
                    for n in tiles:
                        yield from chain_gen(n)
                        post(n)
                        yield

                run_merged([precompute_gen([0, 1])])
                for g in range(NT // 2):
                    nxt = precompute_gen([2 * g + 2, 2 * g + 3]) if g + 1 < NT // 2 else None
                    run_merged([nxt, tile_work([2 * g, 2 * g + 1])])
                if cfg.get("rw_pairs") and c + 1 >= cfg["rw_pairs"]:
                    break
            S.barrier()
        mixer_epilogue(dr["rw_w_out"][j], 8)

    if cfg.get("dbg_gate"):
        P.dout("dbg_gate", [T, NE])
    for i in layers:
        if 0 in subs:
            kind, jj = i % 3, i // 3
            ada_mod(i, 0)
            modulate_transpose()
            spill_X()
            if kind == 2:
                retention(jj)
            elif kind == 1:
                mlstm(jj)
            else:
                rwkv_stage_a(i, jj)
                if cfg.get("only") == "rwa":
                    break
                rwkv_stage_b(i, jj)
        if 1 in subs:
            ada_mod(i, 1)
            if cfg.get("only") == "ada":
                break
            modulate_transpose()
            moe(i)
            deepnorm_from_X()

    otk = Tk("out")
    for n in range(NT):
        S.dma("sp", out_d[n * 128:(n + 1) * 128, :], X[:, n, :], reads=[Xtk[n]], writes=[otk])
    S.barrier()
    es.close()
    return P


_CONSTS = {}


def _consts():
    if _CONSTS:
        return _CONSTS
    half = 128
    inv = 10000.0 ** -(np.arange(half, dtype=np.float64) / (half - 1))
    ang = (np.arange(T, dtype=np.float32)[:, None] * inv.astype(np.float32)[None, :]).astype(np.float32)
    _CONSTS["rt_cos"] = np.ascontiguousarray(np.cos(ang).T.astype(np.float32))
    _CONSTS["rt_sin"] = np.ascontiguousarray(np.sin(ang).T.astype(np.float32))
    tab = np.zeros((4, 128, 16, 128), np.float64)
    jj = np.arange(128)[:, None]
    ss = np.arange(128)[None, :]
    for h in range(4):
        lg = np.log1p(-2.0 ** (-5.0 - h))
        for r in range(16):
            dl = 15 - r
            ex = 128 * dl + ss - jj
            v = np.exp(lg * np.maximum(ex, 0)) / 16.0
            tab[h, :, r, :] = np.where(ex >= 0, v, 0.0)
    _CONSTS["rt_tab"] = tab.astype(np.float32)
    _CONSTS["tri_incl"] = np.triu(np.ones((128, 128), np.float32))
    _CONSTS["ones"] = np.ones((128, 128), np.float32)
    idx = np.arange(128)
    same = (idx[:, None] // 64) == (idx[None, :] // 64)
    up_i = same & (idx[:, None] <= idx[None, :])
    up_s = same & (idx[:, None] < idx[None, :])
    lo_s = same & (idx[:, None] > idx[None, :])
    cdec = -math.exp(-0.5)
    _CONSTS["rw_tri_i"] = (up_i * cdec).astype(np.float32)
    _CONSTS["rw_tri_e"] = (up_s * cdec).astype(np.float32)
    _CONSTS["rw_mask4"] = np.concatenate([up_s, up_i, up_s, up_i], axis=1).astype(np.float32)
    _CONSTS["rw_maskl"] = lo_s.astype(np.float32)
    _CONSTS["rw_blk"] = same.astype(np.float32)
    sel = np.zeros((128, 2), np.float32)
    sel[:64, 0] = 1.0
    sel[64:, 1] = 1.0
    _CONSTS["rw_sel2"] = sel
    return _CONSTS


def prep_inputs(inputs, b):
    f = lambda a: np.ascontiguousarray(a, dtype=np.float32)
    m = {
        "x": f(inputs["x"][b]),
        "c": f(inputs["c"][b:b + 1]),
        "ada_w": f(inputs["ada_w"]),
        "ada_b": f(inputs["ada_b"]),
        "ln_g": f(inputs["ln_g"]),
        "ln_b": f(inputs["ln_b"]),
        "moe_wr": f(np.concatenate([inputs["moe_w_grp"], inputs["moe_w_exp"]], axis=-1)),
        "moe_br": f(np.concatenate([inputs["moe_b_grp"], inputs["moe_b_exp"]], axis=-1)),
        "moe_w1": f(inputs["moe_w1"]),
        "moe_w3": f(inputs["moe_w3"]),
        "moe_w2": f(inputs["moe_w2"]),
        "ident": np.eye(128, dtype=np.float32),
    }
    for k in ("rt_w_in", "rt_gn_g", "rt_gn_b", "rt_w_out", "ml_w_in", "ml_b_gate", "ml_conv_w", "ml_conv_b",
              "ml_gn_g", "ml_gn_b", "ml_w_out"):
        m[k] = f(inputs[k])
    for k in ("rw_mu", "rw_w_rkv", "rw_w0", "rw_w1", "rw_w2", "rw_a0", "rw_a1", "rw_a2", "rw_v0", "rw_v1", "rw_v2",
              "rw_g1", "rw_g2", "rw_kk", "rw_ka", "rw_gn_g", "rw_gn_b", "rw_w_out"):
        m[k] = f(inputs[k])
    m["rw_rk"] = f(inputs["rw_rk"].reshape(2, D))
    m.update(_consts())
    return m


def kernel(**inputs):
    P = build({})
    in_maps = [prep_inputs(inputs, b) for b in range(8)]
    res = run_bass_kernel_spmd(P.nc, in_maps, core_ids=list(range(8)))
    return np.stack([np.asarray(r["out"]) for r in res.results], axis=0).astype(np.float32)
```
